# Optimizing a Trainium2 kernel written in Bass

```python
import jax, jax.numpy as jnp
from jax import lax
import numpy as np

D_MODEL = 1024
BATCH = 4
SEQ = 8192
DEPTH = 1

HEAD_DIM = 64
SWA_HEADS = 8
SWA_KV_HEADS = 2
SB_HEADS = 8
SWA_WIDTH = SWA_HEADS * HEAD_DIM
SWA_KV_WIDTH = SWA_KV_HEADS * HEAD_DIM
SB_WIDTH = SB_HEADS * HEAD_DIM
MIX_WIDTH = SWA_WIDTH + SB_WIDTH
IN_WIDTH = SWA_WIDTH + 2 * SWA_KV_WIDTH + 3 * SB_WIDTH
WINDOW = 128
SB_BLOCK = 128
ROPE_THETA = 10000.0
N_EXPERTS = 256
TOP_K = 8
N_GROUPS = 8
TOPK_GROUPS = 4
EXPERT_FF = 256
ROUTED_SCALE = 2.5
EXPERT_BLOCK = 128
LN_EPS = 1e-5
DEEPNORM_ALPHA = (2 * DEPTH) ** 0.25
DEEPNORM_BETA = (8 * DEPTH) ** -0.25

kernel_name = "hymba_swa_stickbreak_moe_deepnorm_adaln"


def _layer_norm(x, gain=None, bias=None):
    xf = x.astype(jnp.float32)
    mu = jnp.mean(xf, axis=-1, keepdims=True)
    var = jnp.mean(jnp.square(xf - mu), axis=-1, keepdims=True)
    y = (xf - mu) * lax.rsqrt(var + LN_EPS)
    if gain is not None:
        y = y * gain.astype(jnp.float32) + bias.astype(jnp.float32)
    return y.astype(x.dtype)


def _rms_norm(x, gain):
    xf = x.astype(jnp.float32)
    y = xf * lax.rsqrt(jnp.mean(jnp.square(xf), axis=-1, keepdims=True) + LN_EPS)
    return (y * gain.astype(jnp.float32)).astype(x.dtype)


def _rope(x, positions):
    half = HEAD_DIM // 2
    inv_freq = ROPE_THETA ** (-jnp.arange(half, dtype=jnp.float32) * 2.0 / HEAD_DIM)
    ang = positions.astype(jnp.float32)[..., None] * inv_freq
    cos = jnp.cos(ang)[:, :, None, :]
    sin = jnp.sin(ang)[:, :, None, :]
    xf = x.astype(jnp.float32)
    x1, x2 = xf[..., :half], xf[..., half:]
    return jnp.concatenate([x1 * cos - x2 * sin, x2 * cos + x1 * sin], axis=-1).astype(x.dtype)


def _sliding_window_attention(q, k, v, sinks):
    b, s = q.shape[0], q.shape[1]
    nb = s // WINDOW
    g = SWA_HEADS // SWA_KV_HEADS
    qb = q.reshape(b, nb, WINDOW, SWA_KV_HEADS, g, HEAD_DIM)

    def band(t):
        tp = jnp.pad(t, ((0, 0), (WINDOW, 0), (0, 0), (0, 0)))
        tp = tp.reshape(b, nb + 1, WINDOW, SWA_KV_HEADS, HEAD_DIM)
        return jnp.concatenate([tp[:, :-1], tp[:, 1:]], axis=2)

    kb, vb = band(k), band(v)
    scores = jnp.einsum('bnikgd,bnjkd->bnkgij', qb, kb,
                        preferred_element_type=jnp.float32) * (HEAD_DIM ** -0.5)
    qi = jnp.arange(WINDOW)[:, None]
    kj = jnp.arange(2 * WINDOW)[None, :]
    dist = qi + WINDOW - kj
    in_band = (dist >= 0) & (dist < WINDOW)
    key_pos = jnp.arange(nb)[:, None] * WINDOW - WINDOW + jnp.arange(2 * WINDOW)[None, :]
    mask = in_band[None, :, :] & (key_pos >= 0)[:, None, :]
    mask = mask[None, :, None, None]
    sink = sinks.astype(jnp.float32).reshape(1, 1, SWA_KV_HEADS, g, 1, 1)
    scores = jnp.where(mask, scores, -jnp.inf)
    m = jnp.maximum(jnp.max(scores, axis=-1, keepdims=True), sink)
    p = jnp.exp(scores - m)
    p = p / (jnp.sum(p, axis=-1, keepdims=True) + jnp.exp(sink - m))
    o = jnp.einsum('bnkgij,bnjkd->bnikgd', p.astype(v.dtype), vb)
    return o.reshape(b, s, SWA_WIDTH)


def _stick_breaking_attention(q, k, v):
    b, s = q.shape[0], q.shape[1]
    nb = s // SB_BLOCK
    qb = q.reshape(b, nb, SB_BLOCK, SB_HEADS, HEAD_DIM).transpose(1, 0, 3, 2, 4)
    kt = k.transpose(0, 2, 1, 3)
    vt = v.transpose(0, 2, 1, 3)
    key_pos = jnp.arange(s)

    def block(args):
        q_blk, n = args
        z = jnp.einsum('bhid,bhsd->bhis', q_blk, kt,
                       preferred_element_type=jnp.float32) * (HEAD_DIM ** -0.5)
        q_pos = n * SB_BLOCK + jnp.arange(SB_BLOCK)
        before = key_pos[None, :] < q_pos[:, None]
        log_keep = jnp.where(before, jax.nn.log_sigmoid(-z), 0.0)
        later = lax.cumsum(log_keep, axis=3, reverse=True) - log_keep
        w = jnp.where(before, jnp.exp(jax.nn.log_sigmoid(z) + later), 0.0)
        return jnp.einsum('bhis,bhsd->bhid', w.astype(v.dtype), vt)

    o = lax.map(block, (qb, jnp.arange(nb)))
    return o.transpose(1, 0, 3, 2, 4).reshape(b, s, SB_WIDTH)


def _mixer(u, positions, w_in, sinks, g_swa, g_sb, w_out):
    b, s, _ = u.shape
    h = jnp.matmul(u, w_in)
    o1 = SWA_WIDTH
    o2 = o1 + SWA_KV_WIDTH
    o3 = o2 + SWA_KV_WIDTH
    o4 = o3 + SB_WIDTH
    o5 = o4 + SB_WIDTH
    qa = _rope(h[..., :o1].reshape(b, s, SWA_HEADS, HEAD_DIM), positions)
    ka = _rope(h[..., o1:o2].reshape(b, s, SWA_KV_HEADS, HEAD_DIM), positions)
    va = h[..., o2:o3].reshape(b, s, SWA_KV_HEADS, HEAD_DIM)
    qs = h[..., o3:o4].reshape(b, s, SB_HEADS, HEAD_DIM)
    ks = h[..., o4:o5].reshape(b, s, SB_HEADS, HEAD_DIM)
    vs = h[..., o5:].reshape(b, s, SB_HEADS, HEAD_DIM)
    oa = _rms_norm(_sliding_window_attention(qa, ka, va, sinks), g_swa)
    ob = _rms_norm(_stick_breaking_attention(qs, ks, vs), g_sb)
    return jnp.matmul(jnp.concatenate([oa, ob], axis=-1), w_out)


def _swiglu(x, w_gate, w_up, w_down):
    return jnp.matmul(jax.nn.silu(jnp.matmul(x, w_gate)) * jnp.matmul(x, w_up), w_down)


def _moe(u, w_router, e_bias, w1, w3, w2, ws1, ws3, ws2):
    b, s, d = u.shape
    n = b * s
    uf = u.reshape(n, d)
    scores = jax.nn.sigmoid(jnp.matmul(uf, w_router, preferred_element_type=jnp.float32))
    biased = scores + e_bias.astype(jnp.float32)
    grouped = biased.reshape(n, N_GROUPS, N_EXPERTS // N_GROUPS)
    group_score = jnp.sum(lax.top_k(grouped, 2)[0], axis=-1)
    _, g_idx = lax.top_k(group_score, TOPK_GROUPS)
    g_mask = jnp.sum(jax.nn.one_hot(g_idx, N_GROUPS, dtype=jnp.float32), axis=1) > 0
    e_mask = jnp.repeat(g_mask, N_EXPERTS // N_GROUPS, axis=1)
    _, e_idx = lax.top_k(jnp.where(e_mask, biased, -jnp.inf), TOP_K)
    gate = jnp.take_along_axis(scores, e_idx, axis=1)
    gate = gate / jnp.sum(gate, axis=-1, keepdims=True) * ROUTED_SCALE

    nk = n * TOP_K
    e_flat = e_idx.reshape(nk)
    tok_flat = jnp.repeat(jnp.arange(n, dtype=jnp.int32), TOP_K)
    w_flat = gate.reshape(nk)
    order = jnp.argsort(e_flat, stable=True)
    e_sorted = e_flat[order]
    counts = jnp.bincount(e_flat, length=N_EXPERTS)
    padded = (counts + EXPERT_BLOCK - 1) // EXPERT_BLOCK * EXPERT_BLOCK
    start = jnp.cumsum(counts) - counts
    pend = jnp.cumsum(padded)
    pstart = pend - padded
    dest = pstart[e_sorted] + (jnp.arange(nk) - start[e_sorted])
    n_rows = nk + N_EXPERTS * EXPERT_BLOCK
    n_blocks = n_rows // EXPERT_BLOCK
    row_tok = jnp.full((n_rows,), n, dtype=jnp.int32).at[dest].set(tok_flat[order])
    row_w = jnp.zeros((n_rows,), jnp.float32).at[dest].set(w_flat[order])
    blk_e = jnp.minimum(jnp.searchsorted(pend, jnp.arange(n_blocks) * EXPERT_BLOCK, side='right'),
                        N_EXPERTS - 1)
    u_pad = jnp.concatenate([uf, jnp.zeros((1, d), uf.dtype)], axis=0)

    def expert_block(args):
        rows, rw, e = args
        xb = u_pad[rows]
        yb = _swiglu(xb, w1[e], w3[e], w2[e])
        return yb * rw[:, None].astype(yb.dtype)

    out = lax.map(expert_block, (row_tok.reshape(n_blocks, EXPERT_BLOCK),
                                 row_w.reshape(n_blocks, EXPERT_BLOCK), blk_e))
    routed = jnp.zeros((n + 1, d), out.dtype).at[row_tok].add(out.reshape(n_rows, d))[:n]
    shared = _swiglu(uf, ws1, ws3, ws2)
    return (shared + routed.astype(shared.dtype)).reshape(b, s, d)


def setup_inputs(seed: int = 0) -> dict:
    key = jax.random.key(seed)
    ks = jax.random.split(key, 24)
    L, D, E, F = DEPTH, D_MODEL, N_EXPERTS, EXPERT_FF
    beta = DEEPNORM_BETA
    nrm = jax.random.normal
    col_scale = jnp.concatenate([
        jnp.ones((SWA_WIDTH + SWA_KV_WIDTH,), jnp.float32),
        jnp.full((SWA_KV_WIDTH,), beta, jnp.float32),
        jnp.ones((2 * SB_WIDTH,), jnp.float32),
        jnp.full((SB_WIDTH,), beta, jnp.float32),
    ])
    offsets = jax.random.randint(ks[2], (BATCH, 1), 0, 1024, dtype=jnp.int32)
    return {
        "x": nrm(ks[0], (BATCH, SEQ, D), jnp.float32),
        "c": nrm(ks[1], (BATCH, D), jnp.float32),
        "positions": offsets + jnp.arange(SEQ, dtype=jnp.int32)[None, :],
        "w_ada": nrm(ks[3], (L, D, 6 * D), jnp.float32) * (0.1 * D ** -0.5),
        "b_ada": nrm(ks[4], (L, 6 * D), jnp.float32) * 0.01,
        "w_in": nrm(ks[5], (L, D, IN_WIDTH), jnp.float32) * (D ** -0.5) * col_scale,
        "attn_sinks": nrm(ks[6], (L, SWA_HEADS), jnp.float32) * 0.5,
        "g_swa": 1.0 + 0.02 * nrm(ks[7], (L, SWA_WIDTH), jnp.float32),
        "g_sb": 1.0 + 0.02 * nrm(ks[8], (L, SB_WIDTH), jnp.float32),
        "w_out": nrm(ks[9], (L, MIX_WIDTH, D), jnp.float32) * (MIX_WIDTH ** -0.5) * beta,
        "ln1_g": 1.0 + 0.02 * nrm(ks[10], (L, D), jnp.float32),
        "ln1_b": 0.02 * nrm(ks[11], (L, D), jnp.float32),
        "w_router": nrm(ks[12], (L, D, E), jnp.float32) * (D ** -0.5),
        "e_bias": 0.01 * nrm(ks[13], (L, E), jnp.float32),
        "w1": nrm(ks[14], (L, E, D, F), jnp.float32) * (D ** -0.5) * beta,
        "w3": nrm(ks[15], (L, E, D, F), jnp.float32) * (D ** -0.5) * beta,
        "w2": nrm(ks[16], (L, E, F, D), jnp.float32) * (F ** -0.5) * beta,
        "ws1": nrm(ks[17], (L, D, F), jnp.float32) * (D ** -0.5) * beta,
        "ws3": nrm(ks[18], (L, D, F), jnp.float32) * (D ** -0.5) * beta,
        "ws2": nrm(ks[19], (L, F, D), jnp.float32) * (F ** -0.5) * beta,
        "ln2_g": 1.0 + 0.02 * nrm(ks[20], (L, D), jnp.float32),
        "ln2_b": 0.02 * nrm(ks[21], (L, D), jnp.float32),
    }


def reference(x, c, positions, w_ada, b_ada, w_in, attn_sinks, g_swa, g_sb, w_out,
              ln1_g, ln1_b, w_router, e_bias, w1, w3, w2, ws1, ws3, ws2, ln2_g, ln2_b):
    for l in range(DEPTH):
        ada = jnp.matmul(jax.nn.silu(c), w_ada[l]) + b_ada[l]
        shift1, scale1, gate1, shift2, scale2, gate2 = [t[:, None, :] for t in jnp.split(ada, 6, axis=-1)]
        u = _layer_norm(x) * (1.0 + scale1) + shift1
        y = _mixer(u, positions, w_in[l], attn_sinks[l], g_swa[l], g_sb[l], w_out[l])
        x = _layer_norm(DEEPNORM_ALPHA * x + (1.0 + gate1) * y, ln1_g[l], ln1_b[l])
        u = _layer_norm(x) * (1.0 + scale2) + shift2
        y = _moe(u, w_router[l], e_bias[l], w1[l], w3[l], w2[l], ws1[l], ws3[l], ws2[l])
        x = _layer_norm(DEEPNORM_ALPHA * x + (1.0 + gate2) * y, ln2_g[l], ln2_b[l])
    return x
```

```python
import numpy as np
from contextlib import ExitStack
import concourse.bass as bass
import concourse.mybir as mybir
from concourse.bass_utils import run_bass_kernel_spmd

F32 = mybir.dt.float32
BF16 = mybir.dt.bfloat16
I32 = mybir.dt.int32
AF = mybir.ActivationFunctionType
ALU = mybir.AluOpType

D = 1024
FF = 256
ALPHA = 2.0 ** 0.25
EPS = 1e-5
WCOLS = 3200
O_QA, O_QASW, O_KA, O_KASW, O_QS, O_KS, O_VS, O_VA = 0, 512, 1024, 1280, 1536, 2048, 2560, 3072
BIG = 1.0e9
TWO_PI = 6.283185307179586
C1 = 6.28125
C2 = TWO_PI - C1


class Buf:
    def __init__(self, name, t):
        self.name = name
        self.t = t
        self.w = {}
        self.r = {}

    def __getitem__(self, k):
        return self.t[k]


class Prog:
    def __init__(self, nc, es):
        self.nc = nc
        self.es = es
        self.eng = {"pe": nc.tensor, "act": nc.scalar, "dve": nc.vector, "sp": nc.sync, "pool": nc.gpsimd}
        self.semobj = {}
        self.cnt = {}
        for e in ("pe", "act", "dve"):
            self.semobj[e] = es.enter_context(nc.semaphore("s_" + e))
            self.cnt[e] = 0
        self.waited = {e: {} for e in self.eng}

    def sb(self, name, shape, dt, es=None):
        return Buf(name, (es or self.es).enter_context(self.nc.sbuf_tensor(name, shape, dt)))

    def ps(self, name, shape, dt, es=None):
        return Buf(name, (es or self.es).enter_context(self.nc.psum_tensor(name, shape, dt)))

    def dram(self, name, shape, dt, kind):
        return Buf(name, self.nc.dram_tensor(name, shape, dt, kind=kind).ap())

    def _deps(self, ek, reads, writes):
        deps = {}

        def need(toks):
            for s, v in toks.items():
                if ek == "pe" and s == "pe":
                    continue
                if deps.get(s, 0) < v:
                    deps[s] = v

        for b in reads:
            need(b.w)
        for b in writes:
            if not b.name.startswith("dram:"):
                need(b.w)
            need(b.r)
        eng = self.eng[ek]
        wd = self.waited[ek]
        for s, v in deps.items():
            if wd.get(s, 0) < v:
                eng.wait_ge(self.semobj[s], v)
                wd[s] = v

    def _mark(self, tok, reads, writes):
        s, v = tok
        for b in reads:
            b.r[s] = max(b.r.get(s, 0), v)
        for b in writes:
            if b.name.startswith("dram:"):
                b.w[s] = max(b.w.get(s, 0), v)
            else:
                b.w = {s: v}
            b.r = {}

    def op(self, ek, fn, reads=(), writes=()):
        self._deps(ek, reads, writes)
        ins = fn(self.eng[ek])
        self.cnt[ek] += 1
        ins.then_inc(self.semobj[ek], 1)
        self._mark((ek, self.cnt[ek]), reads, writes)
        return ins

    def dma(self, qk, fn, sbuf_side, reads=(), writes=()):
        self._deps(qk, reads, writes)
        key = "d:" + sbuf_side.name
        if key not in self.semobj:
            self.semobj[key] = self.es.enter_context(self.nc.semaphore("sd_" + sbuf_side.name))
            self.cnt[key] = 0
        ins = fn(self.eng[qk])
        self.cnt[key] += 16
        ins.then_inc(self.semobj[key], 16)
        self._mark((key, self.cnt[key]), reads, writes)
        return ins

    def barrier(self):
        for ek in self.eng:
            for s, v in self.cnt.items():
                if v > 0 and self.waited[ek].get(s, 0) < v:
                    self.eng[ek].wait_ge(self.semobj[s], v)
                    self.waited[ek][s] = v

    def finish(self, bufs):
        for b in bufs:
            for s, v in list(b.w.items()) + list(b.r.items()):
                if self.waited["sp"].get(s, 0) < v:
                    self.nc.sync.wait_ge(self.semobj[s], v)
                    self.waited["sp"][s] = v


def build_program(NT, HB, NE, CAP, NKB, debug=False, upto=9):
    assert HB >= 1 and NKB - 1 <= HB
    TT = (HB + NT) * 128
    GS = NE // 8
    NR = CAP // 128
    nc = bass.Bass("TRN2", target_bir_lowering=False)
    es = ExitStack()
    P = Prog(nc, es)

    def dram_in(n, s, d):
        b = P.dram(n, s, d, "ExternalInput")
        b.name = "dram:" + n
        return b

    def dram_tmp(n, s, d, kind="Internal"):
        b = P.dram(n, s, d, kind)
        b.name = "dram:" + n
        return b

    xh = dram_in("xh", [TT, D], F32)
    posb = dram_in("posb", [128, TT], I32)
    csil = dram_in("csil", [128, 8], F32)
    w_ada = dram_in("w_ada", [D, 6 * D], F32)
    b_ada = dram_in("b_ada", [128, 6 * D], F32)
    w_in = dram_in("w_in", [D, WCOLS], F32)
    w_out = dram_in("w_out", [D, D], F32)
    gmix = dram_in("gmix", [128, D], F32)
    sinks = dram_in("sinks", [128, 8], F32)
    lnp = dram_in("lnp", [128, 4, D], F32)
    w_r = dram_in("w_r", [D, NE], F32)
    ebias = dram_in("ebias", [128, NE], F32)
    w1 = dram_in("w1", [NE, D, FF], F32)
    w3 = dram_in("w3", [NE, D, FF], F32)
    w2 = dram_in("w2", [NE, FF, D], F32)
    ws1 = dram_in("ws1", [D, FF], F32)
    ws3 = dram_in("ws3", [D, FF], F32)
    ws2 = dram_in("ws2", [FF, D], F32)
    cst = dram_in("cst", [128, 5, 128], F32)
    msk = dram_in("msk", [128, 5, 1024], F32)
    vec = dram_in("vec", [128, 4], F32)
    ecap = dram_in("ecap", [128, NE], F32)
    e4c = dram_in("e4c", [128, NE], F32)
    out = dram_tmp("out", [NT * 128, D], F32, "ExternalOutput")
    x1s = dram_tmp("x1s", [NT * 128, D], F32)
    accs = dram_tmp("accs", [NT * 128, D], F32)
    xe = dram_tmp("xe", [NE * CAP, D], BF16)
    ye = dram_tmp("ye", [NE * CAP, D], BF16)
    dbg = {}
    if debug:
        dbg["y_moe"] = dram_tmp("dbg_r", [NT * 128, D], F32, "ExternalOutput")

    adas = dram_tmp("adas", [128, 6 * D], F32)
    ident = P.sb("ident", [128, 128], BF16)
    tri = P.sb("tri", [128, 128], BF16)
    ones = P.sb("ones", [128, 128], BF16)
    slt = P.sb("slt", [128, 128], BF16)
    zer = P.sb("zer", [128, 128], BF16)
    dest8 = P.sb("dest8", [128, NT, 8], I32)
    gate8 = P.sb("gate8", [128, NT, 8], F32)
    st = P.sb("st", [128, 16], F32)
    epsT = P.sb("epsT", [128, 2], F32)
    junk = P.sb("junk", [128, D], F32)
    DB = [P.ps("db%d" % i, [128, 1024], F32) for i in range(4)]
    P.op("dve", lambda e: e.memset(gate8[:], 0.0), writes=[gate8])

    def ln_stats(src, width=D, st=st, junk=junk):
        P.op("dve", lambda e: e.memset(st[:, 0:2], 0.0), writes=[st])
        P.op("act", lambda e: e.activation(out=junk[:, 0:width], in_=src[:, 0:width], func=AF.Identity, scale=1.0 / width, accum_out=st[:, 0:1]),
             reads=[src], writes=[junk, st])
        P.op("act", lambda e: e.activation(out=junk[:, 0:width], in_=src[:, 0:width], func=AF.Square, scale=float(width) ** -0.5, accum_out=st[:, 1:2]),
             reads=[src], writes=[junk, st])
        P.op("dve", lambda e: e.scalar_tensor_tensor(out=st[:, 6:7], in0=st[:, 0:1], scalar=st[:, 0:1], in1=st[:, 1:2],
                                                      op0=ALU.mult, op1=ALU.subtract), reads=[st], writes=[st])
        P.op("act", lambda e: e.activation(out=st[:, 7:8], in_=st[:, 6:7], func=AF.Ln, scale=-1.0, bias=epsT[:, 0:1]), reads=[st, epsT], writes=[st])
        P.op("act", lambda e: e.activation(out=st[:, 4:5], in_=st[:, 7:8], func=AF.Exp, scale=-0.5), reads=[st], writes=[st])
        P.op("dve", lambda e: e.scalar_tensor_tensor(out=st[:, 5:6], in0=st[:, 0:1], scalar=-1.0, in1=st[:, 4:5],
                                                      op0=ALU.mult, op1=ALU.mult), reads=[st], writes=[st])

    def transpose8(src_bf, dstT, psbank_ap, psbuf, ncols=128, all_act=False):
        pv = psbank_ap.bitcast(BF16)
        for c in range(8):
            P.op("pe", lambda e, c=c: e.transpose(out=pv[:, c * 128:(c + 1) * 128], in_=src_bf[:, c * 128:(c + 1) * 128], identity=ident[:]),
                 reads=[src_bf, ident], writes=[psbuf])
        P.op("act", lambda e: e.activation(out=dstT[:, 0:4, :], in_=pv[:, 0:512].rearrange("p (c t) -> p c t", c=4), func=AF.Copy),
             reads=[psbuf], writes=[dstT])
        if all_act:
            P.op("act", lambda e: e.activation(out=dstT[:, 4:8, :], in_=pv[:, 512:1024].rearrange("p (c t) -> p c t", c=4), func=AF.Copy),
                 reads=[psbuf], writes=[dstT])
        else:
            P.op("dve", lambda e: e.tensor_copy(out=dstT[:, 4:8, :], in_=pv[:, 512:1024].rearrange("p (c t) -> p c t", c=4)),
                 reads=[psbuf], writes=[dstT])

    with ExitStack() as s0:
        cst_t = P.sb("cst_t", [128, 5, 128], F32, s0)
        P.dma("sp", lambda e: e.dma_start(out=cst_t[:], in_=cst[:]), cst_t, reads=[cst], writes=[cst_t])
        for k, dst in enumerate((ident, tri, ones, slt, zer)):
            P.op("dve", lambda e, k=k, dst=dst: e.tensor_copy(out=dst[:], in_=cst_t[:, k, :]), reads=[cst_t], writes=[dst])
        P.op("dve", lambda e: e.memset(st[:], EPS), writes=[st])
        P.op("dve", lambda e: e.memset(epsT[:], EPS), writes=[epsT])
        ada = P.sb("ada", [128, 6 * D], F32, s0)
        cs = P.sb("cs", [128, 8], F32, s0)
        P.dma("sp", lambda e: e.dma_start(out=cs[:], in_=csil[:]), cs, reads=[csil], writes=[cs])
        cs2 = P.sb("cs2", [128, 8], F32, s0)
        P.op("act", lambda e: e.activation(out=cs2[:], in_=cs[:], func=AF.Silu), reads=[cs], writes=[cs2])
        csb = P.sb("csb", [128, 8, 128], BF16, s0)
        for k in range(8):
            P.op("dve", lambda e, k=k: e.tensor_copy(out=csb[:, k, :], in_=cs2[:, k:k + 1].broadcast_to([128, 128])),
                 reads=[cs2], writes=[csb])
        P.dma("sp", lambda e: e.dma_start(out=ada[:], in_=b_ada[:]), ada, reads=[b_ada], writes=[ada])
        wa = [P.sb("wa%d" % i, [128, 8, 512], BF16, s0) for i in range(2)]
        waf = [P.sb("waf%d" % i, [128, 8, 512], F32, s0) for i in range(3)]
        wav = w_ada.t.rearrange("(c p) n -> p c n", p=128)

        def ld_wa(nb):
            wf = waf[nb % 3]
            P.dma("sp", lambda e: e.dma_start(out=wf[:], in_=wav[:, :, nb * 512:(nb + 1) * 512]), wf, reads=[w_ada], writes=[wf])

        ld_wa(0)
        ld_wa(1)
        for nb in range(12):
            wb = wa[nb % 2]
            wf = waf[nb % 3]
            if nb + 2 < 12:
                ld_wa(nb + 2)
            P.op("act", lambda e, wb=wb, wf=wf: e.activation(out=wb[:, 0:4, :], in_=wf[:, 0:4, :], func=AF.Copy), reads=[wf], writes=[wb])
            P.op("dve", lambda e, wb=wb, wf=wf: e.tensor_copy(out=wb[:, 4:8, :], in_=wf[:, 4:8, :]), reads=[wf], writes=[wb])
            pb = DB[nb % 2]
            for k in range(8):
                P.op("pe", lambda e, k=k, wb=wb, pb=pb: e.matmul(pb[:, 0:512], lhsT=csb[:, k, :], rhs=wb[:, k, :], start=(k == 0), stop=(k == 7)),
                     reads=[csb, wb], writes=[pb])
            P.op("dve", lambda e, nb=nb, pb=pb: e.tensor_tensor(out=ada[:, nb * 512:(nb + 1) * 512], in0=pb[:, 0:512],
                                                                 in1=ada[:, nb * 512:(nb + 1) * 512], op=ALU.add), reads=[pb, ada], writes=[ada])
        for seg in (1, 2, 4, 5):
            P.op("dve", lambda e, seg=seg: e.tensor_scalar(out=ada[:, seg * D:(seg + 1) * D], in0=ada[:, seg * D:(seg + 1) * D],
                                                           scalar1=1.0, scalar2=None, op0=ALU.add), reads=[ada], writes=[ada])
        P.dma("sp", lambda e: e.dma_start(out=adas[:], in_=ada[:]), ada, reads=[ada], writes=[adas])
        P.barrier()
    SH1, SC1, G1, SH2, SC2, G2 = [slice(i * D, (i + 1) * D) for i in range(6)]
    if upto == 0:
        es.close()
        return nc

    def load_cast(s, name, src_view, shape, reads):
        t = P.sb(name, shape, BF16, s)
        P.dma("pool", lambda e: e.dma_start(out=t[:], in_=src_view), t, reads=reads, writes=[t])
        return t

    with ExitStack() as s1:
        win = load_cast(s1, "win", w_in.t.rearrange("(c p) n -> p c n", p=128), [128, 8, WCOLS], [w_in])
        wout = load_cast(s1, "wout", w_out.t.rearrange("(c p) n -> p c n", p=128), [128, 8, D], [w_out])
        ada = P.sb("ada1", [128, 2 * D], F32, s1)
        P.dma("sp", lambda e: e.dma_start(out=ada[:], in_=adas[:, 0:2 * D]), ada, reads=[adas], writes=[ada])
        lnp_t = P.sb("lnp1", [128, 2, D], F32, s1)
        P.dma("sp", lambda e: e.dma_start(out=lnp_t[:], in_=lnp[:, 0:2, :]), lnp_t, reads=[lnp], writes=[lnp_t])
        gmix_t = P.sb("gmix_t", [128, D], F32, s1)
        P.dma("sp", lambda e: e.dma_start(out=gmix_t[:], in_=gmix[:]), gmix_t, reads=[gmix], writes=[gmix_t])
        msk_t = P.sb("msk_t", [128, 5, 128], BF16, s1)
        P.dma("pool", lambda e: e.dma_start(out=msk_t[:], in_=msk[:, :, 0:128]), msk_t, reads=[msk], writes=[msk_t])
        vec_t = P.sb("vec_t", [128, 4], F32, s1)
        P.dma("sp", lambda e: e.dma_start(out=vec_t[:], in_=vec[:]), vec_t, reads=[vec], writes=[vec_t])
        esink = P.sb("esink", [128, 8], F32, s1)
        P.dma("sp", lambda e: e.dma_start(out=esink[:], in_=sinks[:]), esink, reads=[sinks], writes=[esink])
        P.op("act", lambda e: e.activation(out=esink[:], in_=esink[:], func=AF.Exp), reads=[esink], writes=[esink])
        Ct = P.sb("Ct", [128, TT], F32, s1)
        St = P.sb("St", [128, TT], F32, s1)
        with ExitStack() as sr:
            g1p = P.sb("g1p", [128, D], F32, sr)
            P.dma("sp", lambda e: e.dma_start(out=g1p[:], in_=adas[:, 2 * D:3 * D]), g1p, reads=[adas], writes=[g1p])
            for k in range(8):
                P.op("dve", lambda e, k=k: e.tensor_tensor(out=wout[:, k, :], in0=wout[:, k, :], in1=g1p[:], op=ALU.mult), reads=[wout, g1p], writes=[wout])
            posi = P.sb("posi", [128, 512], I32, sr)
            ang = P.sb("ang", [128, 512], F32, sr)
            t1 = P.sb("t1", [128, 512], F32, sr)
            ki = P.sb("ki", [128, 512], I32, sr)
            kf = P.sb("kf", [128, 512], F32, sr)
            for c0 in range(0, TT, 512):
                w = min(512, TT - c0)
                P.dma("sp", lambda e, c0=c0, w=w: e.dma_start(out=posi[:, 0:w], in_=posb[:, c0:c0 + w]), posi, reads=[posb], writes=[posi])
                P.op("dve", lambda e, w=w: e.tensor_copy(out=ang[:, 0:w], in_=posi[:, 0:w]), reads=[posi], writes=[ang])
                P.op("dve", lambda e, w=w: e.tensor_scalar(out=ang[:, 0:w], in0=ang[:, 0:w], scalar1=vec_t[:, 0:1], scalar2=None, op0=ALU.mult),
                     reads=[ang, vec_t], writes=[ang])
                for which, dst in ((0, St), (1, Ct)):
                    if which == 1:
                        P.op("dve", lambda e, w=w: e.tensor_scalar(out=ang[:, 0:w], in0=ang[:, 0:w], scalar1=float(np.pi / 2), scalar2=None, op0=ALU.add),
                             reads=[ang], writes=[ang])
                    P.op("dve", lambda e, w=w: e.tensor_scalar(out=t1[:, 0:w], in0=ang[:, 0:w], scalar1=float(1.0 / TWO_PI), scalar2=None, op0=ALU.mult),
                         reads=[ang], writes=[t1])
                    P.op("dve", lambda e, w=w: e.tensor_copy(out=ki[:, 0:w], in_=t1[:, 0:w]), reads=[t1], writes=[ki])
                    P.op("dve", lambda e, w=w: e.tensor_copy(out=kf[:, 0:w], in_=ki[:, 0:w]), reads=[ki], writes=[kf])
                    P.op("dve", lambda e, w=w: e.scalar_tensor_tensor(out=t1[:, 0:w], in0=kf[:, 0:w], scalar=-C1, in1=ang[:, 0:w], op0=ALU.mult, op1=ALU.add),
                         reads=[kf, ang], writes=[t1])
                    P.op("dve", lambda e, w=w: e.scalar_tensor_tensor(out=t1[:, 0:w], in0=kf[:, 0:w], scalar=-C2, in1=t1[:, 0:w], op0=ALU.mult, op1=ALU.add),
                         reads=[kf, t1], writes=[t1])
                    P.op("dve", lambda e, w=w: e.tensor_scalar(out=t1[:, 0:w], in0=t1[:, 0:w], scalar1=float(np.pi), scalar2=float(-np.pi), op0=ALU.min, op1=ALU.max),
                         reads=[t1], writes=[t1])
                    P.op("act", lambda e, w=w, dst=dst, c0=c0: e.activation(out=dst[:, c0:c0 + w], in_=t1[:, 0:w], func=AF.Sin), reads=[t1], writes=[dst])
                P.op("dve", lambda e, w=w, c0=c0: e.tensor_scalar(out=St[:, c0:c0 + w], in0=St[:, c0:c0 + w], scalar1=vec_t[:, 1:2], scalar2=None, op0=ALU.mult),
                     reads=[St, vec_t], writes=[St])
            P.barrier()

        import os
        P1STOP = os.environ.get("P1STOP", "")
        blk = lambda h: (h % 2) * 4 + h // 2
        H = [Buf("h%d" % i_, DB[i_ // 2].t[:, (i_ % 2) * 512:(i_ % 2 + 1) * 512]) for i_ in range(8)]
        RK = NKB + 1
        xt = [P.sb("xt%d" % i, [128, D], F32, s1) for i in range(2)]
        xn = P.sb("xn", [128, D], F32, s1)
        ub = P.sb("ub", [128, D], BF16, s1)
        uT = P.sb("uT", [128, 8, 128], BF16, s1)
        stA = P.sb("stA", [128, 16], F32, s1)
        jkA = P.sb("jkA", [128, D], BF16, s1)
        qaT = [P.sb("qaT%d" % i, [128, 4, 128], BF16, s1) for i in range(2)]
        qsT = [P.sb("qsT%d" % i, [128, 4, 128], BF16, s1) for i in range(2)]
        kaT = [P.sb("kaT%d" % i, [128, 2, 128], BF16, s1) for i in range(3)]
        vaA = [P.sb("vaA%d" % i, [128, 2, 128], BF16, s1) for i in range(3)]
        ksT = [P.sb("ksT%d" % i, [128, 4, 128], BF16, s1) for i in range(RK)]
        vsB = [P.sb("vsB%d" % i, [128, 512], BF16, s1) for i in range(RK)]
        spB = [P.sb("spB%d" % i, [128, 1024], BF16, s1) for i in range(NKB)]
        tmpa = P.sb("tmpa", [128, 512], F32, s1)
        tmpb = P.sb("tmpb", [128, 512], F32, s1)
        pb16 = P.sb("pb16", [128, 1024], BF16, s1)
        pm16 = P.sb("pm16", [128, 1024], BF16, s1)
        efs = [P.sb("ef%d" % i, [128, 1024], BF16, s1) for i in range(NKB)]
        jb = junk.t[:, :].bitcast(BF16)
        ecs = [Buf("ec%d" % i, jb[:, i * 1024:(i + 1) * 1024]) for i in range(2)]
        wbs = [P.sb("wb%d" % i, [128, 1024], BF16, s1) for i in range(2)]
        mixf = P.sb("mixf", [128, D], F32, s1)
        mixbs = [P.sb("mixb%d" % i, [128, D], BF16, s1) for i in range(2)]
        jkB = P.sb("jkB", [128, D], BF16, s1)
        mixT = P.sb("mixT", [128, 8, 128], BF16, s1)
        den = P.sb("den", [128, 8], F32, s1)
        ss = P.sb("ss", [128, 4], F32, s1)
        pres = [P.sb("pre%d" % i, [128, D], F32, s1) for i in range(2)]
        P.op("dve", lambda e: e.memset(stA[:], EPS), writes=[stA])
        for v in vaA:
            P.op("dve", lambda e, v=v: e.memset(v[:], 1.0), writes=[v])
        xh_v = xh.t.rearrange("(j p) d -> j p d", p=128)
        x1s_v = x1s.t.rearrange("(j p) d -> j p d", p=128)
        r3 = lambda ap, n: ap.rearrange("p (c t) -> p c t", c=n)
        m8v = lambda mi: msk_t[:, mi, :].unsqueeze(1).broadcast_to([128, 8, 128])

        def fm(col0, nch, bank):
            for ch in range(nch):
                for k in range(8):
                    P.op("pe", lambda e, ch=ch, k=k: e.matmul(bank[:, ch * 128:(ch + 1) * 128], lhsT=win[:, k, col0 + ch * 128:col0 + (ch + 1) * 128],
                                                               rhs=uT[:, k, :], start=(k == 0), stop=(k == 7)), reads=[win, uT], writes=[bank])

        def stageA(j):
            own = j >= HB
            x = xt[j % 2]
            if j + 1 < HB + NT:
                xn_ = xt[(j + 1) % 2]
                P.dma("sp", lambda e: e.dma_start(out=xn_[:], in_=xh_v[j + 1]), xn_, reads=[xh], writes=[xn_])
            ln_stats(x, st=stA, junk=jkA)
            yield
            P.op("act", lambda e: e.activation(out=xn[:], in_=x[:], func=AF.Identity, scale=stA[:, 4:5], bias=stA[:, 5:6]), reads=[x, stA], writes=[xn])
            yield
            P.op("dve", lambda e: e.tensor_tensor(out=xn[:], in0=xn[:], in1=ada[:, SC1], op=ALU.mult), reads=[xn, ada], writes=[xn])
            P.op("dve", lambda e: e.tensor_tensor(out=ub[:], in0=xn[:], in1=ada[:, SH1], op=ALU.add), reads=[xn, ada], writes=[ub])
            yield
            transpose8(ub, uT, H[0].t, H[0])
            yield
            ks_slot, vs_slot = ksT[j % RK], vsB[j % RK]
            ka_slot, va_slot = kaT[j % 3], vaA[j % 3]
            tok = slice(j * 128, (j + 1) * 128)
            Cb = lambda n: Ct[:, tok].unsqueeze(1).broadcast_to([128, n, 128])
            Sb = lambda n: St[:, tok].unsqueeze(1).broadcast_to([128, n, 128])
            if own:
                fm(O_QA, 4, H[1])
                yield
                fm(O_QASW, 4, H[0])
                yield
                qa_ = qaT[j % 2]
                P.op("dve", lambda e: e.tensor_tensor(out=r3(tmpa[:], 4), in0=r3(H[1][:, :], 4), in1=Cb(4), op=ALU.mult), reads=[H[1], Ct], writes=[tmpa])
                P.op("dve", lambda e: e.tensor_tensor(out=r3(tmpb[:], 4), in0=r3(H[0][:, :], 4), in1=Sb(4), op=ALU.mult), reads=[H[0], St], writes=[tmpb])
                yield
                P.op("dve", lambda e: e.tensor_tensor(out=qa_[:], in0=r3(tmpa[:], 4), in1=r3(tmpb[:], 4), op=ALU.add), reads=[tmpa, tmpb], writes=[qa_])
            fm(O_KA, 4, H[1])
            yield
            if own:
                fm(O_QS, 4, H[0])
                yield
            P.op("dve", lambda e: e.tensor_tensor(out=r3(tmpa[:, 0:256], 2), in0=r3(H[1][:, 0:256], 2), in1=Cb(2), op=ALU.mult), reads=[H[1], Ct], writes=[tmpa])
            P.op("dve", lambda e: e.tensor_tensor(out=r3(tmpb[:, 0:256], 2), in0=r3(H[1][:, 256:512], 2), in1=Sb(2), op=ALU.mult), reads=[H[1], St], writes=[tmpb])
            yield
            P.op("dve", lambda e: e.tensor_tensor(out=ka_slot[:], in0=r3(tmpa[:, 0:256], 2), in1=r3(tmpb[:, 0:256], 2), op=ALU.add), reads=[tmpa, tmpb], writes=[ka_slot])
            if own:
                qs_ = qsT[j % 2]
                P.op("act", lambda e: e.activation(out=qs_[:], in_=r3(H[0][:, :], 4), func=AF.Copy), reads=[H[0]], writes=[qs_])
            yield
            fm(O_KS, 4, H[1])
            yield
            for k in range(8):
                P.op("pe", lambda e, k=k: e.matmul(H[0][:, :], lhsT=uT[:, k, :], rhs=win[:, k, O_VS:O_VS + 512], start=(k == 0), stop=(k == 7)),
                     reads=[win, uT], writes=[H[0]])
            yield
            P.op("act", lambda e: e.activation(out=ks_slot[:], in_=r3(H[1][:, :], 4), func=AF.Copy), reads=[H[1]], writes=[ks_slot])
            yield
            P.op("act", lambda e: e.activation(out=vs_slot[:], in_=H[0][:, :], func=AF.Copy), reads=[H[0]], writes=[vs_slot])
            for k in range(8):
                P.op("pe", lambda e, k=k: e.matmul(H[1][:, 0:128], lhsT=uT[:, k, :], rhs=win[:, k, O_VA:O_VA + 128], start=(k == 0), stop=(k == 7)),
                     reads=[win, uT], writes=[H[1]])
            yield
            P.op("dve", lambda e: e.tensor_copy(out=va_slot[:, :, 0:64], in_=H[1][:, 0:128].rearrange("p (k d) -> p k d", k=2)), reads=[H[1]], writes=[va_slot])

        def stageB1(j):
            i = j - HB
            qa_, qs_ = qaT[j % 2], qsT[j % 2]
            mixb_ = mixbs[j % 2]
            pre_ = pres[j % 2]
            P.dma("sp", lambda e: e.dma_start(out=pre_[:], in_=xh_v[j]), pre_, reads=[xh], writes=[pre_])
            for a in range(2):
                P.op("pe", lambda e, a=a: e.matmul(H[6 + a][:, :], lhsT=zer[:], rhs=win[:, 0, 0:512], start=True, stop=False), reads=[zer, win], writes=[H[6 + a]])
            for kt in range(2):
                kslot = kaT[(j - 1 + kt) % 3]
                vslot = vaA[(j - 1 + kt) % 3]
                for h in range(8):
                    hp = (h % 2) * 64
                    bk = H[4 + h % 2]
                    P.op("pe", lambda e, h=h, hp=hp, bk=bk: e.matmul(bk[:, (h // 2) * 128:(h // 2 + 1) * 128], lhsT=kslot[hp:hp + 64, h // 4, :],
                                                                     rhs=qa_[hp:hp + 64, h // 2, :], start=True, stop=True), reads=[kslot, qa_], writes=[bk])
                yield
                for hf in range(2):
                    P.op("act", lambda e, hf=hf: e.activation(out=pb16[:, hf * 512:(hf + 1) * 512], in_=H[4 + hf][:, :], func=AF.Exp, scale=0.125), reads=[H[4 + hf]], writes=[pb16])
                yield
                mi = 1 if kt == 1 else (3 if i == 0 else 0)
                P.op("dve", lambda e, mi=mi: e.tensor_tensor(out=r3(pm16[:], 8), in0=r3(pb16[:], 8), in1=m8v(mi), op=ALU.mult), reads=[pb16, msk_t], writes=[pm16])
                yield
                for h in range(8):
                    bk = H[6 + h // 4]
                    P.op("pe", lambda e, h=h, bk=bk: e.matmul(bk[:, (h % 4) * 128:(h % 4 + 1) * 128],
                                                              lhsT=pm16[:, blk(h) * 128:(blk(h) + 1) * 128], rhs=vslot[:, h // 4, :], start=False, stop=(kt == 1 and h % 4 == 3)),
                         reads=[pm16, vslot], writes=[bk])
            yield
            oav = lambda a: H[6 + a][:, :].rearrange("p (h d) -> p h d", h=4)
            for a in range(2):
                P.op("dve", lambda e, a=a: e.tensor_tensor(out=den[:, a * 4:(a + 1) * 4], in0=oav(a)[:, :, 64], in1=esink[:, a * 4:(a + 1) * 4], op=ALU.add),
                     reads=[H[6 + a], esink], writes=[den])
            P.op("dve", lambda e: e.reciprocal(out=den[:], in_=den[:]), reads=[den], writes=[den])
            yield
            for a in range(2):
                P.op("dve", lambda e, a=a: e.tensor_tensor(out=mixf[:, a * 256:(a + 1) * 256].rearrange("p (h d) -> p h d", h=4), in0=oav(a)[:, :, 0:64],
                                                           in1=den[:, a * 4:(a + 1) * 4].unsqueeze(2).broadcast_to([128, 4, 64]), op=ALU.mult),
                     reads=[H[6 + a], den], writes=[mixf])
            P.op("pe", lambda e: e.matmul(H[3][:, :], lhsT=zer[:], rhs=win[:, 0, 0:512], start=True, stop=False), reads=[zer, win], writes=[H[3]])

            def zmm(kk):
                ksl = ksT[(j - kk) % RK]
                for h in range(8):
                    hp = (h % 2) * 64
                    bk = H[4 + h % 2]
                    P.op("pe", lambda e, h=h, hp=hp, bk=bk: e.matmul(bk[:, (h // 2) * 128:(h // 2 + 1) * 128], lhsT=ksl[hp:hp + 64, h // 2, :],
                                                                     rhs=qs_[hp:hp + 64, h // 2, :], start=True, stop=True), reads=[ksl, qs_], writes=[bk])

            def eexp(kk):
                ef_ = efs[kk]
                for hf in range(2):
                    P.op("act", lambda e, hf=hf: e.activation(out=ef_[:, hf * 512:(hf + 1) * 512], in_=H[4 + hf][:, :], func=AF.Exp, scale=0.125), reads=[H[4 + hf]], writes=[ef_])
                mi = 2 if kk == 0 else (4 if (j - kk) < HB else None)
                if mi is not None:
                    P.op("dve", lambda e: e.tensor_tensor(out=r3(ef_[:], 8), in0=r3(ef_[:], 8), in1=m8v(mi), op=ALU.mult), reads=[ef_, msk_t], writes=[ef_])

            def splog(kk):
                P.op("act", lambda e: e.activation(out=spB[kk][:], in_=efs[kk][:], func=AF.Ln, bias=1.0), reads=[efs[kk]], writes=[spB[kk]])

            def cmm(kk):
                for half in range(2):
                    hs = slice(half * 512, (half + 1) * 512)
                    P.op("pe", lambda e, hs=hs, half=half: e.matmul(H[6 + half][:, :], lhsT=tri[:], rhs=spB[kk][:, hs], start=True, stop=(kk == 0)),
                         reads=[tri, spB[kk]], writes=[H[6 + half]])
                    for k2 in range(kk):
                        P.op("pe", lambda e, hs=hs, k2=k2, half=half: e.matmul(H[6 + half][:, :], lhsT=ones[:], rhs=spB[k2][:, hs], start=False, stop=(k2 == kk - 1)),
                             reads=[ones, spB[k2]], writes=[H[6 + half]])

            def ecexp(kk):
                ec_ = ecs[kk % 2]
                for hf in range(2):
                    P.op("act", lambda e, hf=hf: e.activation(out=ec_[:, hf * 512:(hf + 1) * 512], in_=H[6 + hf][:, :], func=AF.Exp, scale=-1.0), reads=[H[6 + hf]], writes=[ec_])

            def wmul(kk):
                P.op("dve", lambda e: e.tensor_tensor(out=wbs[kk % 2][:], in0=efs[kk][:], in1=ecs[kk % 2][:], op=ALU.mult), reads=[efs[kk], ecs[kk % 2]], writes=[wbs[kk % 2]])

            def pv(kk):
                vsl = vsB[(j - kk) % RK]
                wb_ = wbs[kk % 2]
                for h in range(8):
                    P.op("pe", lambda e, h=h: e.matmul(H[3][:, h * 64:(h + 1) * 64], lhsT=wb_[:, blk(h) * 128:(blk(h) + 1) * 128],
                                                       rhs=vsl[:, h * 64:(h + 1) * 64], start=False, stop=(kk == NKB - 1 and h == 7)),
                         reads=[wb_, vsl], writes=[H[3]])

            yield
            zmm(0)
            yield
            eexp(0)
            yield
            for kk in range(NKB):
                if kk + 1 < NKB:
                    zmm(kk + 1)
                splog(kk)
                yield
                if kk + 1 < NKB:
                    eexp(kk + 1)
                cmm(kk)
                yield
                ecexp(kk)
                if kk >= 1:
                    pv(kk - 1)
                yield
                wmul(kk)
                yield
            pv(NKB - 1)
            yield
            P.op("act", lambda e: e.activation(out=mixf[:, 512:1024], in_=H[3][:, :], func=AF.Copy), reads=[H[3]], writes=[mixf])
            yield
            P.op("dve", lambda e: e.memset(ss[:], 0.0), writes=[ss])
            for a in range(2):
                P.op("act", lambda e, a=a: e.activation(out=pb16[:, 0:512], in_=mixf[:, a * 512:(a + 1) * 512], func=AF.Square, accum_out=ss[:, a:a + 1]),
                     reads=[mixf], writes=[pb16, ss])
            yield
            P.op("act", lambda e: e.activation(out=ss[:, 2:4], in_=ss[:, 0:2], func=AF.Ln, scale=1.0 / 512, bias=epsT[:, 0:1]), reads=[ss, epsT], writes=[ss])
            P.op("act", lambda e: e.activation(out=ss[:, 0:2], in_=ss[:, 2:4], func=AF.Exp, scale=-0.5), reads=[ss], writes=[ss])
            yield
            for a in range(2):
                hs = slice(a * 512, (a + 1) * 512)
                P.op("dve", lambda e, a=a, hs=hs: e.scalar_tensor_tensor(out=mixb_[:, hs], in0=mixf[:, hs], scalar=ss[:, a:a + 1], in1=gmix_t[:, hs],
                                                                          op0=ALU.mult, op1=ALU.mult), reads=[mixf, ss, gmix_t], writes=[mixb_])

        def stageB2(j):
            i = j - HB
            mixb_ = mixbs[j % 2]
            pre = pres[j % 2]
            transpose8(mixb_, mixT, H[2].t, H[2])
            yield
            for half in range(2):
                hs = slice(half * 512, (half + 1) * 512)
                for k in range(8):
                    P.op("pe", lambda e, k=k, hs=hs: e.matmul(H[2][:, :], lhsT=mixT[:, k, :], rhs=wout[:, k, hs], start=(k == 0), stop=(k == 7)),
                         reads=[mixT, wout], writes=[H[2]])
                yield
                P.op("dve", lambda e, hs=hs: e.scalar_tensor_tensor(out=pre[:, hs], in0=pre[:, hs], scalar=ALPHA, in1=H[2][:, :], op0=ALU.mult, op1=ALU.add),
                     reads=[H[2], pre], writes=[pre])
                yield
            ln_stats(pre, junk=jkB)
            yield
            P.op("act", lambda e: e.activation(out=pre[:], in_=pre[:], func=AF.Identity, scale=st[:, 4:5], bias=st[:, 5:6]), reads=[pre, st], writes=[pre])
            yield
            P.op("dve", lambda e: e.tensor_tensor(out=pre[:], in0=pre[:], in1=lnp_t[:, 0, :], op=ALU.mult), reads=[pre, lnp_t], writes=[pre])
            P.op("dve", lambda e: e.tensor_tensor(out=pre[:], in0=pre[:], in1=lnp_t[:, 1, :], op=ALU.add), reads=[pre, lnp_t], writes=[pre])
            yield
            P.dma("sp", lambda e: e.dma_start(out=x1s_v[i], in_=pre[:]), pre, reads=[pre], writes=[x1s])

        P.dma("sp", lambda e: e.dma_start(out=xt[0][:], in_=xh_v[0]), xt[0], reads=[xh], writes=[xt[0]])
        LAST = HB + NT - 1
        for j in range(HB + NT + 2):
            gens = []
            if HB <= j - 1 <= LAST:
                gens.append(stageB1(j - 1))
            if j <= LAST:
                gens.append(stageA(j))
            if HB <= j - 2 <= LAST:
                gens.append(stageB2(j - 2))
            while gens:
                for g in list(gens):
                    try:
                        next(g)
                    except StopIteration:
                        gens.remove(g)
        P.barrier()
    if upto == 1:
        es.close()
        return nc
    accs_v = accs.t.rearrange("(j p) d -> j p d", p=128)
    x1s_v = x1s.t.rearrange("(j p) d -> j p d", p=128)
    with ExitStack() as s2:
        wr = load_cast(s2, "wr", w_r.t.rearrange("(c p) n -> p c n", p=128), [128, 8, NE], [w_r])
        s1w = load_cast(s2, "s1w", ws1.t.rearrange("(c p) n -> p c n", p=128), [128, 8, FF], [ws1])
        s3w = load_cast(s2, "s3w", ws3.t.rearrange("(c p) n -> p c n", p=128), [128, 8, FF], [ws3])
        s2w = load_cast(s2, "s2w", ws2.t.rearrange("(c p) n -> p c n", p=128), [128, 2, D], [ws2])
        ada = P.sb("ada2", [128, 3 * D], F32, s2)
        P.dma("sp", lambda e: e.dma_start(out=ada[:], in_=adas[:, 3 * D:6 * D]), ada, reads=[adas], writes=[ada])
        SH2, SC2, G2 = SH1, SC1, G1
        eb = P.sb("eb", [128, NE], F32, s2)
        P.dma("sp", lambda e: e.dma_start(out=eb[:], in_=ebias[:]), eb, reads=[ebias], writes=[eb])
        basecap = P.sb("basecap", [128, NE], F32, s2)
        P.dma("sp", lambda e: e.dma_start(out=basecap[:], in_=ecap[:]), basecap, reads=[ecap], writes=[basecap])
        xt = [P.sb("x1l%d" % i, [128, D], F32, s2) for i in range(2)]
        xn = P.sb("xn2", [128, D], F32, s2)
        u2b = [P.sb("u2b%d" % i, [128, D], BF16, s2) for i in range(4)]
        u2T = P.sb("u2T", [128, 8, 128], BF16, s2)
        sil = P.sb("sil", [128, 256], F32, s2)
        aT = P.sb("aT", [128, 2, 128], BF16, s2)
        acc_t = P.sb("acc_t", [128, D], F32, s2)
        bi = P.sb("bi", [128, NE], F32, s2)
        m8 = P.sb("m8", [128, 8, 8], F32, s2)
        gs = P.sb("gs", [128, 8], F32, s2)
        g8 = P.sb("g8", [128, 8], F32, s2)
        gm = P.sb("gm", [128, 8], F32, s2)
        gn = P.sb("gn", [128, 8], F32, s2)
        mk = P.sb("mk", [128, NE], F32, s2)
        t8 = P.sb("t8", [128, 8], F32, s2)
        sel = P.sb("sel", [128, NE], F32, s2)
        selb = P.sb("selb", [128, NE], BF16, s2)
        gd = P.sb("gd", [128, NE], F32, s2)
        val = P.sb("val", [128, NE], F32, s2)
        d8 = P.sb("d8", [128, 8], F32, s2)
        rd = P.sb("rd", [128, 2], F32, s2)
        jk = P.sb("jk", [128, NE], F32, s2)
        k8 = P.sb("k8", [128, 8], F32, s2)
        k8i = P.sb("k8i", [128, 8], I32, s2)
        k8f = P.sb("k8f", [128, 8], F32, s2)
        e4 = P.sb("e4", [128, NE], F32, s2)
        P.dma("sp", lambda e: e.dma_start(out=e4[:], in_=e4c[:]), e4, reads=[e4c], writes=[e4])
        P.dma("sp", lambda e: e.dma_start(out=xt[0][:], in_=x1s_v[0]), xt[0], reads=[x1s], writes=[xt[0]])
        scs = [P.sb("sc%d" % i_, [128, NE], F32, s2) for i_ in range(2)]
        dest8_b = [Buf("dest8_%d" % i_, dest8.t[:, i_, :]) for i_ in range(NT)]
        gate8_b = [Buf("gate8_%d" % i_, gate8.t[:, i_, :]) for i_ in range(NT)]

        def stage1(i):
            sc = scs[i % 2]
            x = xt[i % 2]
            ub = u2b[i % 4]
            if i + 1 < NT:
                xn_ = xt[(i + 1) % 2]
                P.dma("sp", lambda e, i=i, xn_=xn_: e.dma_start(out=xn_[:], in_=x1s_v[i + 1]), xn_, reads=[x1s], writes=[xn_])
            ln_stats(x)
            P.op("act", lambda e: e.activation(out=xn[:], in_=x[:], func=AF.Identity, scale=st[:, 4:5], bias=st[:, 5:6]), reads=[x, st], writes=[xn])
            P.op("dve", lambda e: e.tensor_tensor(out=xn[:], in0=xn[:], in1=ada[:, SC2], op=ALU.mult), reads=[xn, ada], writes=[xn])
            P.op("dve", lambda e: e.tensor_tensor(out=ub[:], in0=xn[:], in1=ada[:, SH2], op=ALU.add), reads=[xn, ada], writes=[ub])
            yield
            transpose8(ub, u2T, DB[0][:, 0:512], DB[0], all_act=True)
            for k in range(8):
                P.op("pe", lambda e, k=k: e.matmul(DB[0][:, 512:512 + NE], lhsT=u2T[:, k, :], rhs=wr[:, k, :], start=(k == 0), stop=(k == 7)),
                     reads=[u2T, wr], writes=[DB[0]])
            for wi, wsx in enumerate((s1w, s3w)):
                for fc in range(2):
                    o = (wi * 2 + fc) * 128
                    for k in range(8):
                        P.op("pe", lambda e, k=k, wsx=wsx, fc=fc, o=o: e.matmul(DB[1][:, o:o + 128], lhsT=wsx[:, k, fc * 128:(fc + 1) * 128], rhs=u2T[:, k, :],
                                                                                start=(k == 0), stop=(k == 7)), reads=[wsx, u2T], writes=[DB[1]])
            yield
            P.op("act", lambda e: e.activation(out=sil[:], in_=DB[1][:, 0:256], func=AF.Silu), reads=[DB[1]], writes=[sil])
            P.op("dve", lambda e: e.tensor_tensor(out=aT[:].rearrange("p c t -> p (c t)"), in0=sil[:], in1=DB[1][:, 256:512], op=ALU.mult), reads=[sil, DB[1]], writes=[aT])
            for half in range(2):
                hs = slice(half * 512, (half + 1) * 512)
                for fc in range(2):
                    P.op("pe", lambda e, fc=fc, hs=hs: e.matmul(DB[2][:, hs], lhsT=aT[:, fc, :], rhs=s2w[:, fc, hs], start=(fc == 0), stop=(fc == 1)),
                         reads=[aT, s2w], writes=[DB[2]])
            for hf in range(2):
                P.op("dve", lambda e, hf=hf: e.tensor_tensor(out=acc_t[:, hf * 512:(hf + 1) * 512], in0=DB[2][:, hf * 512:(hf + 1) * 512], in1=ada[:, 2 * D + hf * 512:2 * D + (hf + 1) * 512], op=ALU.mult), reads=[DB[2], ada], writes=[acc_t])
            P.op("dve", lambda e: e.scalar_tensor_tensor(out=acc_t[:], in0=x[:], scalar=ALPHA, in1=acc_t[:], op0=ALU.mult, op1=ALU.add), reads=[x, acc_t], writes=[acc_t])
            P.dma("sp", lambda e, i=i: e.dma_start(out=accs_v[i], in_=acc_t[:]), acc_t, reads=[acc_t], writes=[accs])
            P.op("act", lambda e: e.activation(out=sc[:], in_=DB[0][:, 512:512 + NE], func=AF.Sigmoid), reads=[DB[0]], writes=[sc])

        def stage2(i):
            sc = scs[i % 2]
            ub = u2b[i % 4]
            P.op("dve", lambda e: e.tensor_tensor(out=bi[:], in0=sc[:], in1=eb[:], op=ALU.add), reads=[sc, eb], writes=[bi])
            for g in range(8):
                P.op("dve", lambda e, g=g: e.max(out=m8[:, g, :], in_=bi[:, g * GS:(g + 1) * GS]), reads=[bi], writes=[m8])
            P.op("dve", lambda e: e.tensor_tensor(out=gs[:], in0=m8[:, :, 0], in1=m8[:, :, 1], op=ALU.add), reads=[m8], writes=[gs])
            P.op("dve", lambda e: e.max(out=g8[:], in_=gs[:]), reads=[gs], writes=[g8])
            P.op("dve", lambda e: e.tensor_scalar(out=gm[:], in0=gs[:], scalar1=g8[:, 3:4], scalar2=None, op0=ALU.is_ge), reads=[gs, g8], writes=[gm])
            P.op("dve", lambda e: e.tensor_scalar(out=gn[:], in0=gm[:], scalar1=-1.0, scalar2=BIG, op0=ALU.add, op1=ALU.mult), reads=[gm], writes=[gn])
            g3 = lambda b: b[:].rearrange("p (g s) -> p g s", g=8)
            gb3 = lambda b: b[:].unsqueeze(2).broadcast_to([128, 8, GS])
            P.op("dve", lambda e: e.tensor_tensor(out=g3(mk), in0=g3(bi), in1=gb3(gm), op=ALU.mult), reads=[bi, gm], writes=[mk])
            P.op("dve", lambda e: e.tensor_tensor(out=g3(mk), in0=g3(mk), in1=gb3(gn), op=ALU.add), reads=[mk, gn], writes=[mk])
            P.op("dve", lambda e: e.max(out=t8[:], in_=mk[:]), reads=[mk], writes=[t8])
            P.op("dve", lambda e: e.tensor_scalar(out=sel[:], in0=mk[:], scalar1=t8[:, 7:8], scalar2=None, op0=ALU.is_ge), reads=[mk, t8], writes=[sel])
            P.op("dve", lambda e: e.tensor_copy(out=selb[:], in_=sel[:]), reads=[sel], writes=[selb])
            P.op("dve", lambda e: e.memset(rd[:], 0.0), writes=[rd])
            P.op("dve", lambda e: e.scalar_tensor_tensor(out=gd[:], in0=sel[:], scalar=1.0, in1=sc[:], op0=ALU.mult, op1=ALU.mult, accum_out=rd[:, 0:1]),
                 reads=[sel, sc], writes=[gd, rd])
            P.op("dve", lambda e: e.reciprocal(out=rd[:, 1:2], in_=rd[:, 0:1]), reads=[rd], writes=[rd])
            P.op("dve", lambda e: e.tensor_scalar(out=gd[:], in0=gd[:], scalar1=rd[:, 1:2], scalar2=2.5, op0=ALU.mult, op1=ALU.mult), reads=[gd, rd], writes=[gd])
            yield
            P.op("pe", lambda e: e.matmul(DB[3][:, 0:NE], lhsT=slt[:], rhs=selb[:], start=True, stop=True), reads=[slt, selb], writes=[DB[3]])
            P.op("pe", lambda e: e.matmul(DB[3][:, 512:512 + NE], lhsT=ones[:], rhs=selb[:], start=True, stop=True), reads=[ones, selb], writes=[DB[3]])
            yield
            P.op("dve", lambda e: e.tensor_tensor(out=val[:], in0=DB[3][:, 0:NE], in1=basecap[:], op=ALU.add), reads=[DB[3], basecap], writes=[val])
            P.op("dve", lambda e: e.tensor_tensor(out=val[:], in0=val[:], in1=sel[:], op=ALU.mult), reads=[val, sel], writes=[val])
            P.op("dve", lambda e: e.tensor_tensor(out=basecap[:], in0=DB[3][:, 512:512 + NE], in1=basecap[:], op=ALU.add), reads=[DB[3], basecap], writes=[basecap])
            P.op("dve", lambda e: e.max(out=d8[:], in_=val[:]), reads=[val], writes=[d8])
            P.op("dve", lambda e: e.tensor_tensor(out=jk[:], in0=gd[:], in1=e4[:], op=ALU.add), reads=[gd, e4], writes=[jk])
            P.op("dve", lambda e: e.tensor_tensor(out=jk[:], in0=jk[:], in1=sel[:], op=ALU.mult), reads=[jk, sel], writes=[jk])
            P.op("dve", lambda e: e.max(out=k8[:], in_=jk[:]), reads=[jk], writes=[k8])
            P.op("dve", lambda e: e.tensor_scalar(out=k8i[:], in0=k8[:], scalar1=0.25, scalar2=-0.3125, op0=ALU.mult, op1=ALU.add), reads=[k8], writes=[k8i])
            P.op("dve", lambda e: e.tensor_copy(out=k8f[:], in_=k8i[:]), reads=[k8i], writes=[k8f])
            P.op("dve", lambda e, i=i: e.scalar_tensor_tensor(out=gate8_b[i][:, :], in0=k8f[:], scalar=-4.0, in1=k8[:], op0=ALU.mult, op1=ALU.add),
                 reads=[k8f, k8], writes=[gate8_b[i]])
            P.op("dve", lambda e, i=i: e.tensor_scalar(out=dest8_b[i][:, :], in0=d8[:], scalar1=-1.0, scalar2=None, op0=ALU.add), reads=[d8], writes=[dest8_b[i]])
            for k in range(8):
                P.dma("pool", lambda e, i=i, k=k, ub=ub: e.indirect_dma_start(out=xe.t[:, :], out_offset=bass.IndirectOffsetOnAxis(ap=dest8_b[i][:, k:k + 1], axis=0),
                                                                               in_=ub[:, :], in_offset=None), ub, reads=[ub, dest8_b[i]], writes=[xe])

        def run(g, n=None):
            k = 0
            for _ in g:
                k += 1
                if n is not None and k >= n:
                    return

        run(stage1(0))
        for i in range(NT):
            g1 = stage1(i + 1) if i + 1 < NT else iter(())
            g2 = stage2(i)
            run(g1, 1)
            run(g2, 1)
            run(g1, 1)
            run(g2, 1)
            run(g2)
            run(g1)
        P.barrier()

    with ExitStack() as s3:
        Wf1 = [P.sb("Wf1_%d" % i, [128, 8, FF], F32, s3) for i in range(2)]
        Wf3 = [P.sb("Wf3_%d" % i, [128, 8, FF], F32, s3) for i in range(2)]
        Wf2 = [P.sb("Wf2_%d" % i, [128, 2, D], F32, s3) for i in range(2)]
        W1 = [P.sb("W1_%d" % i, [128, 8, FF], BF16, s3) for i in range(2)]
        W3 = [P.sb("W3_%d" % i, [128, 8, FF], BF16, s3) for i in range(2)]
        W2 = [P.sb("W2_%d" % i, [128, 2, D], BF16, s3) for i in range(2)]
        xr = [P.sb("xr%d" % i, [128, D], BF16, s3) for i in range(2 * NR)]
        XT = [P.sb("XT%d" % i, [128, 8, CAP], BF16, s3) for i in range(2)]
        sl = [P.sb("sl%d" % i, [128, 2 * CAP], F32, s3) for i in range(2)]
        aE = [P.sb("aE%d" % i, [128, 2, CAP], BF16, s3) for i in range(2)]
        yo = [P.sb("yo%d" % i, [128, D], BF16, s3) for i in range(2)]
        DBH = [[Buf("db%d_%d" % (i, hf), DB[i].t[:, hf * 512:(hf + 1) * 512]) for hf in range(2)] for i in range(4)]

        def load_w(e_):
            b = e_ % 2
            P.dma("sp", lambda e: e.dma_start(out=Wf1[b][:], in_=w1.t[e_].rearrange("(p c) f -> p c f", c=8)), Wf1[b], reads=[w1], writes=[Wf1[b]])
            P.dma("sp", lambda e: e.dma_start(out=Wf3[b][:], in_=w3.t[e_].rearrange("(p c) f -> p c f", c=8)), Wf3[b], reads=[w3], writes=[Wf3[b]])
            P.dma("sp", lambda e: e.dma_start(out=Wf2[b][:], in_=w2.t[e_].rearrange("(p c) d -> p c d", c=2)), Wf2[b], reads=[w2], writes=[Wf2[b]])

        def cast_w(e_):
            b = e_ % 2
            pv = lambda t: t[:].rearrange("p k (c m) -> p k c m", c=2)
            sv = lambda t: t[:].rearrange("p k (m c) -> p k c m", c=2)
            P.op("act", lambda e: e.activation(out=pv(W1[b]), in_=sv(Wf1[b]), func=AF.Copy), reads=[Wf1[b]], writes=[W1[b]])
            P.op("dve", lambda e: e.tensor_copy(out=pv(W3[b]), in_=sv(Wf3[b])), reads=[Wf3[b]], writes=[W3[b]])
            P.op("act", lambda e: e.activation(out=W2[b][:, 0, :], in_=Wf2[b][:, 0, :], func=AF.Copy), reads=[Wf2[b]], writes=[W2[b]])
            P.op("dve", lambda e: e.tensor_copy(out=W2[b][:, 1, :], in_=Wf2[b][:, 1, :]), reads=[Wf2[b]], writes=[W2[b]])

        def load_x(e_):
            for r in range(NR):
                xb_ = xr[(e_ % 2) * NR + r]
                row0 = e_ * CAP + r * 128
                P.dma("sp", lambda e, xb_=xb_, row0=row0: e.dma_start(out=xb_[:], in_=xe.t[row0:row0 + 128, :]), xb_, reads=[xe], writes=[xb_])

        def trans(e_):
            for r in range(NR):
                xb_ = xr[(e_ % 2) * NR + r]
                bank = DBH[0][r % 2]
                pv = bank.t.bitcast(BF16)
                xs = xb_[:].rearrange("s (p c) -> s c p", c=8)
                for c in range(8):
                    P.op("pe", lambda e, c=c, xs=xs, pv=pv: e.transpose(out=pv[:, c * 128:(c + 1) * 128], in_=xs[:, c, :], identity=ident[:]),
                         reads=[xb_, ident], writes=[bank])
                xt_ = XT[e_ % 2]
                P.op("act", lambda e, r=r, pv=pv, xt_=xt_: e.activation(out=xt_[:, 0:4, r * 128:(r + 1) * 128], in_=pv[:, 0:512].rearrange("p (c t) -> p c t", c=4), func=AF.Copy),
                     reads=[bank], writes=[xt_])
                P.op("dve", lambda e, r=r, pv=pv, xt_=xt_: e.tensor_copy(out=xt_[:, 4:8, r * 128:(r + 1) * 128], in_=pv[:, 512:1024].rearrange("p (c t) -> p c t", c=4)),
                     reads=[bank], writes=[xt_])

        def hmm(e_):
            b = e_ % 2
            for wi, Wx in enumerate((W1[b], W3[b])):
                pb = DBH[1 + b][wi]
                for fc in range(2):
                    for k in range(8):
                        P.op("pe", lambda e, Wx=Wx, pb=pb, fc=fc, k=k: e.matmul(pb[:, fc * CAP:(fc + 1) * CAP], lhsT=Wx[:, k, fc * 128:(fc + 1) * 128], rhs=XT[b][:, k, :],
                                                                                start=(k == 0), stop=(k == 7)), reads=[Wx, XT[b]], writes=[pb])

        def act_(e_):
            b = e_ % 2
            P.op("act", lambda e: e.activation(out=sl[b][:], in_=DBH[1 + b][0][:, 0:2 * CAP], func=AF.Silu), reads=[DBH[1 + b][0]], writes=[sl[b]])
            P.op("dve", lambda e: e.tensor_tensor(out=aE[b][:].rearrange("p c t -> p (c t)"), in0=sl[b][:], in1=DBH[1 + b][1][:, 0:2 * CAP], op=ALU.mult),
                 reads=[sl[b], DBH[1 + b][1]], writes=[aE[b]])

        def ymm(e_):
            b = e_ % 2
            for r in range(NR):
                yb = yo[r % 2]
                for half in range(2):
                    hs = slice(half * 512, (half + 1) * 512)
                    for fc in range(2):
                        P.op("pe", lambda e, fc=fc, hs=hs, r=r, half=half: e.matmul(DBH[3][half][:, :], lhsT=aE[b][:, fc, r * 128:(r + 1) * 128], rhs=W2[b][:, fc, hs], start=(fc == 0), stop=(fc == 1)),
                             reads=[aE[b], W2[b]], writes=[DBH[3][half]])
                P.op("act", lambda e, yb=yb: e.activation(out=yb[:, 0:512], in_=DBH[3][0][:, :], func=AF.Copy), reads=[DBH[3][0]], writes=[yb])
                P.op("dve", lambda e, yb=yb: e.tensor_copy(out=yb[:, 512:1024], in_=DBH[3][1][:, :]), reads=[DBH[3][1]], writes=[yb])
                row0 = e_ * CAP + r * 128
                P.dma("pool", lambda e, yb=yb, row0=row0: e.dma_start(out=ye.t[row0:row0 + 128, :], in_=yb[:]), yb, reads=[yb], writes=[ye])

        load_w(0)
        if NE > 1:
            load_w(1)
        load_x(0)
        cast_w(0)
        trans(0)
        for e_ in range(NE):
            hmm(e_)
            if e_ + 1 < NE:
                load_x(e_ + 1)
                cast_w(e_ + 1)
                trans(e_ + 1)
            if e_ + 2 < NE:
                load_w(e_ + 2)
            act_(e_)
            ymm(e_)
        P.barrier()
    if upto == 3:
        es.close()
        return nc
    out_v = out.t.rearrange("(j p) d -> j p d", p=128)
    with ExitStack() as s4:
        yk = [P.sb("yk%d" % i, [128, 8, D], BF16, s4) for i in range(3)]
        ac = [P.sb("ac%d" % i, [128, D], F32, s4) for i in range(3)]
        ada = P.sb("ada4", [128, D], F32, s4)
        P.dma("sp", lambda e: e.dma_start(out=ada[:], in_=adas[:, 5 * D:6 * D]), ada, reads=[adas], writes=[ada])
        G2 = slice(0, D)
        lnp_t = P.sb("lnp2", [128, 2, D], F32, s4)
        P.dma("sp", lambda e: e.dma_start(out=lnp_t[:], in_=lnp[:, 2:4, :]), lnp_t, reads=[lnp], writes=[lnp_t])

        dest8_b = [Buf("dest8_%d" % i_, dest8.t[:, i_, :]) for i_ in range(NT)]
        gate8_b = [Buf("gate8_%d" % i_, gate8.t[:, i_, :]) for i_ in range(NT)]
        rrs = [P.sb("rr%d" % i_, [128, D], F32, s4) for i_ in range(2)]
        ots = [P.sb("ot%d" % i_, [128, D], F32, s4) for i_ in range(2)]
        dgs = [P.sb("dg%d" % i_, [128, 8, 128], BF16, s4) for i_ in range(2)]
        DBH4 = [[Buf("p4db%d_%d" % (i_, hf), DB[i_].t[:, hf * 512:(hf + 1) * 512]) for hf in range(2)] for i_ in range(2)]

        ykk = [[Buf("yk%d_%d" % (i_, k), yk[i_].t[:, k, :]) for k in range(8)] for i_ in range(3)]

        def loads(i):
            P.dma("sp", lambda e: e.dma_start(out=ac[i % 3][:], in_=accs_v[i]), ac[i % 3], reads=[accs], writes=[ac[i % 3]])
            for k in range(8):
                yb_ = ykk[i % 3][k]
                P.dma("pool", lambda e, k=k, yb_=yb_: e.indirect_dma_start(out=yb_[:, :], out_offset=None, in_=ye.t[:, :],
                                                                           in_offset=bass.IndirectOffsetOnAxis(ap=dest8_b[i][:, k:k + 1], axis=0)),
                      yb_, reads=[ye, dest8_b[i]], writes=[yb_])

        def stage1(i):
            y_ = yk[i % 3]
            a_ = ac[i % 3]
            dg = dgs[i % 2]
            r_ = rrs[i % 2]
            P.op("dve", lambda e: e.tensor_tensor(out=dg[:], in0=ident[:].unsqueeze(1).broadcast_to([128, 8, 128]),
                                                  in1=gate8_b[i][:, :].unsqueeze(2).broadcast_to([128, 8, 128]), op=ALU.mult),
                 reads=[ident, gate8_b[i]], writes=[dg])
            for hf in range(2):
                pb = DBH4[i % 2][hf]
                for k in range(8):
                    P.op("pe", lambda e, k=k, hf=hf, pb=pb: e.matmul(pb[:, :], lhsT=dg[:, k, :], rhs=ykk[i % 3][k][:, hf * 512:(hf + 1) * 512], start=(k == 0), stop=(k == 7)),
                         reads=[dg, ykk[i % 3][k]], writes=[pb])
            for hf in range(2):
                pb = DBH4[i % 2][hf]
                hs = slice(hf * 512, (hf + 1) * 512)
                if debug:
                    P.op("act", lambda e, hs=hs, pb=pb: e.activation(out=junk[:, hs], in_=pb[:, :], func=AF.Copy), reads=[pb], writes=[junk])
                P.op("dve", lambda e, hs=hs, pb=pb: e.tensor_tensor(out=r_[:, hs], in0=pb[:, :], in1=ada[:, hs], op=ALU.mult), reads=[pb, ada], writes=[r_])
            if debug:
                P.dma("sp", lambda e: e.dma_start(out=dbg["y_moe"].t[i * 128:(i + 1) * 128, :], in_=junk[:]), junk, reads=[junk], writes=[dbg["y_moe"]])
            P.op("dve", lambda e: e.tensor_tensor(out=r_[:], in0=r_[:], in1=a_[:], op=ALU.add), reads=[r_, a_], writes=[r_])

        def stage2(i):
            r_ = rrs[i % 2]
            ot = ots[i % 2]
            ln_stats(r_)
            P.op("act", lambda e: e.activation(out=ot[:], in_=r_[:], func=AF.Identity, scale=st[:, 4:5], bias=st[:, 5:6]), reads=[r_, st], writes=[ot])
            P.op("dve", lambda e: e.tensor_tensor(out=ot[:], in0=ot[:], in1=lnp_t[:, 0, :], op=ALU.mult), reads=[ot, lnp_t], writes=[ot])
            P.op("dve", lambda e: e.tensor_tensor(out=ot[:], in0=ot[:], in1=lnp_t[:, 1, :], op=ALU.add), reads=[ot, lnp_t], writes=[ot])
            P.dma("sp", lambda e: e.dma_start(out=out_v[i], in_=ot[:]), ot, reads=[ot], writes=[out])

        loads(0)
        if NT > 1:
            loads(1)
        stage1(0)
        for i in range(NT):
            if i + 1 < NT:
                stage1(i + 1)
            if i + 2 < NT:
                loads(i + 2)
            stage2(i)
        P.barrier()
    fin = [out] + list(dbg.values())
    P.finish(fin)
    es.close()
    return nc


def _consts(NE, CAP, first_half):
    cst = np.zeros((128, 5, 128), np.float32)
    j = np.arange(128)[:, None]
    s = np.arange(128)[None, :]
    cst[:, 0] = np.eye(128, dtype=np.float32)
    cst[:, 1] = (j >= s)
    cst[:, 2] = 1.0
    cst[:, 3] = (j < s)
    msk = np.zeros((128, 5, 1024), np.float32)
    rep = lambda m: np.tile(m.astype(np.float32), (1, 8))
    prev = (j > s)
    msk[:, 0] = rep(prev)
    msk[:, 1] = rep(j <= s)
    msk[:, 2] = rep(j < s)
    msk[:, 3] = 0.0 if first_half else rep(prev)
    msk[:, 4] = 0.0 if first_half else 1.0
    vec = np.zeros((128, 4), np.float32)
    p = np.arange(128)
    inv = (10000.0 ** (-(np.arange(32, dtype=np.float32) * 2.0 / 64))).astype(np.float32)
    vec[:, 0] = inv[p % 32]
    vec[:, 1] = np.where((p % 64) < 32, -1.0, 1.0)
    ecap = np.tile((np.arange(NE, dtype=np.float32) * CAP + 1.0)[None, :], (128, 1))
    e4c = np.tile(((np.arange(NE, dtype=np.float32) + 1.0) * 4.0)[None, :], (128, 1))
    return cst, msk, vec, ecap, e4c


def _win_layout(w_in):
    def sw(w):
        n = w.shape[1] // 64
        w4 = w.reshape(w.shape[0], n, 2, 32)
        return w4[:, :, ::-1, :].reshape(w.shape[0], n * 64)
    qa, ka, va = w_in[:, 0:512], w_in[:, 512:640], w_in[:, 640:768]
    qs, ks, vs = w_in[:, 768:1280], w_in[:, 1280:1792], w_in[:, 1792:2304]
    dup = lambda k: np.concatenate([k[:, 0:64], k[:, 0:64], k[:, 64:128], k[:, 64:128]], axis=1)
    return np.ascontiguousarray(np.concatenate([qa, sw(qa), dup(ka), dup(sw(ka)), qs, ks, vs, va], axis=1))


def make_in_maps(inp, n_cores, NT, HB, NE, CAP, seq):
    rb = lambda v, n=128: np.ascontiguousarray(np.broadcast_to(np.asarray(v, np.float32).reshape(1, -1), (n, np.asarray(v).size)))
    x = np.asarray(inp["x"], np.float32)
    pos = np.asarray(inp["positions"], np.int32)
    halves = seq // (NT * 128)
    win = _win_layout(np.asarray(inp["w_in"], np.float32)[0])
    shared = {
        "w_ada": np.asarray(inp["w_ada"], np.float32)[0],
        "b_ada": rb(inp["b_ada"][0]),
        "w_in": win,
        "w_out": np.asarray(inp["w_out"], np.float32)[0],
        "gmix": rb(np.concatenate([np.asarray(inp["g_swa"])[0], np.asarray(inp["g_sb"])[0]])),
        "sinks": rb(inp["attn_sinks"][0]),
        "lnp": np.ascontiguousarray(np.stack([rb(inp["ln1_g"][0]), rb(inp["ln1_b"][0]), rb(inp["ln2_g"][0]), rb(inp["ln2_b"][0])], axis=1)),
        "w_r": np.asarray(inp["w_router"], np.float32)[0],
        "ebias": rb(inp["e_bias"][0]),
        "w1": np.asarray(inp["w1"], np.float32)[0],
        "w3": np.asarray(inp["w3"], np.float32)[0],
        "w2": np.asarray(inp["w2"], np.float32)[0],
        "ws1": np.asarray(inp["ws1"], np.float32)[0],
        "ws3": np.asarray(inp["ws3"], np.float32)[0],
        "ws2": np.asarray(inp["ws2"], np.float32)[0],
    }
    maps = []
    for c in range(n_cores):
        b, h = c // halves, c % halves
        s0 = h * NT * 128
        lo = s0 - HB * 128
        xh = np.zeros(((HB + NT) * 128, D), np.float32)
        ph = np.zeros(((HB + NT) * 128,), np.int32)
        src_lo = max(lo, 0)
        xh[src_lo - lo:] = x[b, src_lo:s0 + NT * 128]
        ph[src_lo - lo:] = pos[b, src_lo:s0 + NT * 128]
        cst, msk, vec, ecap, e4c = _consts(NE, CAP, first_half=(h == 0))
        m = dict(shared)
        m.update({
            "xh": xh,
            "posb": np.ascontiguousarray(np.broadcast_to(ph[None, :], (128, ph.size))),
            "csil": np.ascontiguousarray(np.asarray(inp["c"], np.float32)[b].reshape(8, 128).T),
            "cst": cst, "msk": msk, "vec": vec, "ecap": ecap, "e4c": e4c,
        })
        maps.append(m)
    return maps


NT_FULL, HB_FULL, NE_FULL, CAP_FULL, NKB_FULL = 32, 2, 256, 256, 3


def kernel(**inputs):
    nc = build_program(NT_FULL, HB_FULL, NE_FULL, CAP_FULL, NKB_FULL)
    maps = make_in_maps(inputs, 8, NT_FULL, HB_FULL, NE_FULL, CAP_FULL, 8192)
    res = run_bass_kernel_spmd(nc, maps, core_ids=list(range(8)))
    outs = [np.asarray(r["out"]) for r in res.results]
    full = np.stack(outs, axis=0).reshape(4, 8192, D)
    return full.astype(np.float32)
```

```python
import numpy as np
from contextlib import ExitStack
import concourse.bass as bass
import concourse.mybir as mybir
from concourse.bass_utils import run_bass_kernel_spmd

F32 = mybir.dt.float32
BF16 = mybir.dt.bfloat16
I32 = mybir.dt.int32
AF = mybir.ActivationFunctionType
ALU = mybir.AluOpType

D = 1024
FF = 256
ALPHA = 2.0 ** 0.25
EPS = 1e-5
WCOLS = 3200
O_QA, O_QASW, O_KA, O_KASW, O_QS, O_KS, O_VS, O_VA = 0, 512, 1024, 1280, 1536, 2048, 2560, 3072
BIG = 1.0e9
TWO_PI = 6.283185307179586
C1 = 6.28125
C2 = TWO_PI - C1


class Buf:
    def __init__(self, name, t):
        self.name = name
        self.t = t
        self.w = {}
        self.r = {}

    def __getitem__(self, k):
        return self.t[k]


class Prog:
    def __init__(self, nc, es):
        self.nc = nc
        self.es = es
        self.eng = {"pe": nc.tensor, "act": nc.scalar, "dve": nc.vector, "sp": nc.sync, "pool": nc.gpsimd}
        self.semobj = {}
        self.cnt = {}
        for e in ("pe", "act", "dve"):
            self.semobj[e] = es.enter_context(nc.semaphore("s_" + e))
            self.cnt[e] = 0
        self.waited = {e: {} for e in self.eng}

    def sb(self, name, shape, dt, es=None):
        return Buf(name, (es or self.es).enter_context(self.nc.sbuf_tensor(name, shape, dt)))

    def ps(self, name, shape, dt, es=None):
        return Buf(name, (es or self.es).enter_context(self.nc.psum_tensor(name, shape, dt)))

    def dram(self, name, shape, dt, kind):
        return Buf(name, self.nc.dram_tensor(name, shape, dt, kind=kind).ap())

    def _deps(self, ek, reads, writes):
        deps = {}

        def need(toks):
            for s, v in toks.items():
                if ek == "pe" and s == "pe":
                    continue
                if deps.get(s, 0) < v:
                    deps[s] = v

        for b in reads:
            need(b.w)
        for b in writes:
            if not b.name.startswith("dram:"):
                need(b.w)
            need(b.r)
        eng = self.eng[ek]
        wd = self.waited[ek]
        for s, v in deps.items():
            if wd.get(s, 0) < v:
                eng.wait_ge(self.semobj[s], v)
                wd[s] = v

    def _mark(self, tok, reads, writes):
        s, v = tok
        for b in reads:
            b.r[s] = max(b.r.get(s, 0), v)
        for b in writes:
            if b.name.startswith("dram:"):
                b.w[s] = max(b.w.get(s, 0), v)
            else:
                b.w = {s: v}
            b.r = {}

    def op(self, ek, fn, reads=(), writes=()):
        self._deps(ek, reads, writes)
        ins = fn(self.eng[ek])
        self.cnt[ek] += 1
        ins.then_inc(self.semobj[ek], 1)
        self._mark((ek, self.cnt[ek]), reads, writes)
        return ins

    def dma(self, qk, fn, sbuf_side, reads=(), writes=()):
        self._deps(qk, reads, writes)
        key = "d:" + sbuf_side.name
        if key not in self.semobj:
            self.semobj[key] = self.es.enter_context(self.nc.semaphore("sd_" + sbuf_side.name))
            self.cnt[key] = 0
        ins = fn(self.eng[qk])
        self.cnt[key] += 16
        ins.then_inc(self.semobj[key], 16)
        self._mark((key, self.cnt[key]), reads, writes)
        return ins

    def barrier(self):
        for ek in self.eng:
            for s, v in self.cnt.items():
                if v > 0 and self.waited[ek].get(s, 0) < v:
                    self.eng[ek].wait_ge(self.semobj[s], v)
                    self.waited[ek][s] = v

    def finish(self, bufs):
        for b in bufs:
            for s, v in list(b.w.items()) + list(b.r.items()):
                if self.waited["sp"].get(s, 0) < v:
                    self.nc.sync.wait_ge(self.semobj[s], v)
                    self.waited["sp"][s] = v


def build_program(NT, HB, NE, CAP, NKB, debug=False, upto=9):
    assert HB >= 1 and NKB - 1 <= HB
    TT = (HB + NT) * 128
    GS = NE // 8
    NR = (CAP + 127) // 128
    RROWS = [min(128, CAP - r * 128) for r in range(NR)]
    nc = bass.Bass("TRN2", target_bir_lowering=False)
    es = ExitStack()
    P = Prog(nc, es)

    def dram_in(n, s, d):
        b = P.dram(n, s, d, "ExternalInput")
        b.name = "dram:" + n
        return b

    def dram_tmp(n, s, d, kind="Internal"):
        b = P.dram(n, s, d, kind)
        b.name = "dram:" + n
        return b

    xh = dram_in("xh", [TT, D], F32)
    posb = dram_in("posb", [128, TT], I32)
    csil = dram_in("csil", [128, 8], F32)
    w_ada = dram_in("w_ada", [D, 6 * D], F32)
    b_ada = dram_in("b_ada", [128, 6 * D], F32)
    w_in = dram_in("w_in", [D, WCOLS], F32)
    w_out = dram_in("w_out", [D, D], F32)
    gmix = dram_in("gmix", [128, D], F32)
    sinks = dram_in("sinks", [128, 8], F32)
    lnp = dram_in("lnp", [128, 4, D], F32)
    w_r = dram_in("w_r", [D, NE], F32)
    ebias = dram_in("ebias", [128, NE], F32)
    w1 = dram_in("w1", [NE, D, FF], F32)
    w3 = dram_in("w3", [NE, D, FF], F32)
    w2 = dram_in("w2", [NE, FF, D], F32)
    ws1 = dram_in("ws1", [D, FF], F32)
    ws3 = dram_in("ws3", [D, FF], F32)
    ws2 = dram_in("ws2", [FF, D], F32)
    cst = dram_in("cst", [128, 5, 128], F32)
    msk = dram_in("msk", [128, 5, 1024], F32)
    vec = dram_in("vec", [128, 4], F32)
    ecap = dram_in("ecap", [128, NE], F32)
    e4c = dram_in("e4c", [128, NE], F32)
    elimc = dram_in("elimc", [128, NE], F32)
    prowc = dram_in("prowc", [128, 1], F32)
    out = dram_tmp("out", [NT * 128, D], F32, "ExternalOutput")
    x1s = dram_tmp("x1s", [NT * 128, D], F32)
    accs = dram_tmp("accs", [NT * 128, D], F32)
    xe = dram_tmp("xe", [NE * CAP + 128, D], BF16)
    ye = dram_tmp("ye", [NE * CAP + 128, D], BF16)
    dbg = {}
    if debug:
        dbg["y_moe"] = dram_tmp("dbg_r", [NT * 128, D], F32, "ExternalOutput")

    adas = dram_tmp("adas", [128, 6 * D], F32)
    ident = P.sb("ident", [128, 128], BF16)
    tri = P.sb("tri", [128, 128], BF16)
    ones = P.sb("ones", [128, 128], BF16)
    slt = P.sb("slt", [128, 128], BF16)
    zer = P.sb("zer", [128, 128], BF16)
    dest8 = P.sb("dest8", [128, NT, 8], I32)
    gate8 = P.sb("gate8", [128, NT, 8], F32)
    st = P.sb("st", [128, 16], F32)
    epsT = P.sb("epsT", [128, 2], F32)
    junk = P.sb("junk", [128, D], F32)
    DB = [P.ps("db%d" % i, [128, 1024], F32) for i in range(4)]
    P.op("dve", lambda e: e.memset(gate8[:], 0.0), writes=[gate8])

    def ln_stats(src, width=D, st=st, junk=junk):
        P.op("dve", lambda e: e.memset(st[:, 0:2], 0.0), writes=[st])
        P.op("act", lambda e: e.activation(out=junk[:, 0:width], in_=src[:, 0:width], func=AF.Identity, scale=1.0 / width, accum_out=st[:, 0:1]),
             reads=[src], writes=[junk, st])
        P.op("act", lambda e: e.activation(out=junk[:, 0:width], in_=src[:, 0:width], func=AF.Square, scale=float(width) ** -0.5, accum_out=st[:, 1:2]),
             reads=[src], writes=[junk, st])
        P.op("dve", lambda e: e.scalar_tensor_tensor(out=st[:, 6:7], in0=st[:, 0:1], scalar=st[:, 0:1], in1=st[:, 1:2],
                                                      op0=ALU.mult, op1=ALU.subtract), reads=[st], writes=[st])
        P.op("act", lambda e: e.activation(out=st[:, 7:8], in_=st[:, 6:7], func=AF.Ln, scale=-1.0, bias=epsT[:, 0:1]), reads=[st, epsT], writes=[st])
        P.op("act", lambda e: e.activation(out=st[:, 4:5], in_=st[:, 7:8], func=AF.Exp, scale=-0.5), reads=[st], writes=[st])
        P.op("dve", lambda e: e.scalar_tensor_tensor(out=st[:, 5:6], in0=st[:, 0:1], scalar=-1.0, in1=st[:, 4:5],
                                                      op0=ALU.mult, op1=ALU.mult), reads=[st], writes=[st])

    def transpose8(src_bf, dstT, psbank_ap, psbuf, ncols=128, all_act=False):
        pv = psbank_ap.bitcast(BF16)
        for c in range(8):
            P.op("pe", lambda e, c=c: e.transpose(out=pv[:, c * 128:(c + 1) * 128], in_=src_bf[:, c * 128:(c + 1) * 128], identity=ident[:]),
                 reads=[src_bf, ident], writes=[psbuf])
        P.op("act", lambda e: e.activation(out=dstT[:, 0:4, :], in_=pv[:, 0:512].rearrange("p (c t) -> p c t", c=4), func=AF.Copy),
             reads=[psbuf], writes=[dstT])
        if all_act:
            P.op("act", lambda e: e.activation(out=dstT[:, 4:8, :], in_=pv[:, 512:1024].rearrange("p (c t) -> p c t", c=4), func=AF.Copy),
                 reads=[psbuf], writes=[dstT])
        else:
            P.op("dve", lambda e: e.tensor_copy(out=dstT[:, 4:8, :], in_=pv[:, 512:1024].rearrange("p (c t) -> p c t", c=4)),
                 reads=[psbuf], writes=[dstT])

    with ExitStack() as s0:
        cst_t = P.sb("cst_t", [128, 5, 128], F32, s0)
        P.dma("sp", lambda e: e.dma_start(out=cst_t[:], in_=cst[:]), cst_t, reads=[cst], writes=[cst_t])
        for k, dst in enumerate((ident, tri, ones, slt, zer)):
            P.op("dve", lambda e, k=k, dst=dst: e.tensor_copy(out=dst[:], in_=cst_t[:, k, :]), reads=[cst_t], writes=[dst])
        P.op("dve", lambda e: e.memset(st[:], EPS), writes=[st])
        P.op("dve", lambda e: e.memset(epsT[:], EPS), writes=[epsT])
        ada = P.sb("ada", [128, 6 * D], F32, s0)
        cs = P.sb("cs", [128, 8], F32, s0)
        P.dma("sp", lambda e: e.dma_start(out=cs[:], in_=csil[:]), cs, reads=[csil], writes=[cs])
        cs2 = P.sb("cs2", [128, 8], F32, s0)
        P.op("act", lambda e: e.activation(out=cs2[:], in_=cs[:], func=AF.Silu), reads=[cs], writes=[cs2])
        csb = P.sb("csb", [128, 8, 128], BF16, s0)
        for k in range(8):
            P.op("dve", lambda e, k=k: e.tensor_copy(out=csb[:, k, :], in_=cs2[:, k:k + 1].broadcast_to([128, 128])),
                 reads=[cs2], writes=[csb])
        P.dma("sp", lambda e: e.dma_start(out=ada[:], in_=b_ada[:]), ada, reads=[b_ada], writes=[ada])
        wa = [P.sb("wa%d" % i, [128, 8, 512], BF16, s0) for i in range(2)]
        waf = [P.sb("waf%d" % i, [128, 8, 512], F32, s0) for i in range(3)]
        wav = w_ada.t.rearrange("(c p) n -> p c n", p=128)

        def ld_wa(nb):
            wf = waf[nb % 3]
            P.dma("sp", lambda e: e.dma_start(out=wf[:], in_=wav[:, :, nb * 512:(nb + 1) * 512]), wf, reads=[w_ada], writes=[wf])

        ld_wa(0)
        ld_wa(1)
        for nb in range(12):
            wb = wa[nb % 2]
            wf = waf[nb % 3]
            if nb + 2 < 12:
                ld_wa(nb + 2)
            P.op("act", lambda e, wb=wb, wf=wf: e.activation(out=wb[:, 0:4, :], in_=wf[:, 0:4, :], func=AF.Copy), reads=[wf], writes=[wb])
            P.op("dve", lambda e, wb=wb, wf=wf: e.tensor_copy(out=wb[:, 4:8, :], in_=wf[:, 4:8, :]), reads=[wf], writes=[wb])
            pb = DB[nb % 2]
            for k in range(8):
                P.op("pe", lambda e, k=k, wb=wb, pb=pb: e.matmul(pb[:, 0:512], lhsT=csb[:, k, :], rhs=wb[:, k, :], start=(k == 0), stop=(k == 7)),
                     reads=[csb, wb], writes=[pb])
            P.op("dve", lambda e, nb=nb, pb=pb: e.tensor_tensor(out=ada[:, nb * 512:(nb + 1) * 512], in0=pb[:, 0:512],
                                                                 in1=ada[:, nb * 512:(nb + 1) * 512], op=ALU.add), reads=[pb, ada], writes=[ada])
        for seg in (1, 2, 4, 5):
            P.op("dve", lambda e, seg=seg: e.tensor_scalar(out=ada[:, seg * D:(seg + 1) * D], in0=ada[:, seg * D:(seg + 1) * D],
                                                           scalar1=1.0, scalar2=None, op0=ALU.add), reads=[ada], writes=[ada])
        P.dma("sp", lambda e: e.dma_start(out=adas[:], in_=ada[:]), ada, reads=[ada], writes=[adas])
        P.barrier()
    SH1, SC1, G1, SH2, SC2, G2 = [slice(i * D, (i + 1) * D) for i in range(6)]
    if upto == 0:
        es.close()
        return nc

    def load_cast(s, name, src_view, shape, reads):
        t = P.sb(name, shape, BF16, s)
        P.dma("pool", lambda e: e.dma_start(out=t[:], in_=src_view), t, reads=reads, writes=[t])
        return t

    with ExitStack() as s1:
        win = load_cast(s1, "win", w_in.t.rearrange("(c p) n -> p c n", p=128), [128, 8, WCOLS], [w_in])
        wout = load_cast(s1, "wout", w_out.t.rearrange("(c p) n -> p c n", p=128), [128, 8, D], [w_out])
        ada = P.sb("ada1", [128, 2 * D], F32, s1)
        P.dma("sp", lambda e: e.dma_start(out=ada[:], in_=adas[:, 0:2 * D]), ada, reads=[adas], writes=[ada])
        lnp_t = P.sb("lnp1", [128, 2, D], F32, s1)
        P.dma("sp", lambda e: e.dma_start(out=lnp_t[:], in_=lnp[:, 0:2, :]), lnp_t, reads=[lnp], writes=[lnp_t])
        gmix_t = P.sb("gmix_t", [128, D], F32, s1)
        P.dma("sp", lambda e: e.dma_start(out=gmix_t[:], in_=gmix[:]), gmix_t, reads=[gmix], writes=[gmix_t])
        msk_t = P.sb("msk_t", [128, 5, 128], BF16, s1)
        P.dma("pool", lambda e: e.dma_start(out=msk_t[:], in_=msk[:, :, 0:128]), msk_t, reads=[msk], writes=[msk_t])
        vec_t = P.sb("vec_t", [128, 4], F32, s1)
        P.dma("sp", lambda e: e.dma_start(out=vec_t[:], in_=vec[:]), vec_t, reads=[vec], writes=[vec_t])
        esink = P.sb("esink", [128, 8], F32, s1)
        P.dma("sp", lambda e: e.dma_start(out=esink[:], in_=sinks[:]), esink, reads=[sinks], writes=[esink])
        P.op("act", lambda e: e.activation(out=esink[:], in_=esink[:], func=AF.Exp), reads=[esink], writes=[esink])
        Ct = P.sb("Ct", [128, TT], F32, s1)
        St = P.sb("St", [128, TT], F32, s1)
        with ExitStack() as sr:
            g1p = P.sb("g1p", [128, D], F32, sr)
            P.dma("sp", lambda e: e.dma_start(out=g1p[:], in_=adas[:, 2 * D:3 * D]), g1p, reads=[adas], writes=[g1p])
            for k in range(8):
                P.op("dve", lambda e, k=k: e.tensor_tensor(out=wout[:, k, :], in0=wout[:, k, :], in1=g1p[:], op=ALU.mult), reads=[wout, g1p], writes=[wout])
            posi = P.sb("posi", [128, 512], I32, sr)
            ang = P.sb("ang", [128, 512], F32, sr)
            t1 = P.sb("t1", [128, 512], F32, sr)
            ki = P.sb("ki", [128, 512], I32, sr)
            kf = P.sb("kf", [128, 512], F32, sr)
            for c0 in range(0, TT, 512):
                w = min(512, TT - c0)
                P.dma("sp", lambda e, c0=c0, w=w: e.dma_start(out=posi[:, 0:w], in_=posb[:, c0:c0 + w]), posi, reads=[posb], writes=[posi])
                P.op("dve", lambda e, w=w: e.tensor_copy(out=ang[:, 0:w], in_=posi[:, 0:w]), reads=[posi], writes=[ang])
                P.op("dve", lambda e, w=w: e.tensor_scalar(out=ang[:, 0:w], in0=ang[:, 0:w], scalar1=vec_t[:, 0:1], scalar2=None, op0=ALU.mult),
                     reads=[ang, vec_t], writes=[ang])
                for which, dst in ((0, St), (1, Ct)):
                    if which == 1:
                        P.op("dve", lambda e, w=w: e.tensor_scalar(out=ang[:, 0:w], in0=ang[:, 0:w], scalar1=float(np.pi / 2), scalar2=None, op0=ALU.add),
                             reads=[ang], writes=[ang])
                    P.op("dve", lambda e, w=w: e.tensor_scalar(out=t1[:, 0:w], in0=ang[:, 0:w], scalar1=float(1.0 / TWO_PI), scalar2=None, op0=ALU.mult),
                         reads=[ang], writes=[t1])
                    P.op("dve", lambda e, w=w: e.tensor_copy(out=ki[:, 0:w], in_=t1[:, 0:w]), reads=[t1], writes=[ki])
                    P.op("dve", lambda e, w=w: e.tensor_copy(out=kf[:, 0:w], in_=ki[:, 0:w]), reads=[ki], writes=[kf])
                    P.op("dve", lambda e, w=w: e.scalar_tensor_tensor(out=t1[:, 0:w], in0=kf[:, 0:w], scalar=-C1, in1=ang[:, 0:w], op0=ALU.mult, op1=ALU.add),
                         reads=[kf, ang], writes=[t1])
                    P.op("dve", lambda e, w=w: e.scalar_tensor_tensor(out=t1[:, 0:w], in0=kf[:, 0:w], scalar=-C2, in1=t1[:, 0:w], op0=ALU.mult, op1=ALU.add),
                         reads=[kf, t1], writes=[t1])
                    P.op("dve", lambda e, w=w: e.tensor_scalar(out=t1[:, 0:w], in0=t1[:, 0:w], scalar1=float(np.pi), scalar2=float(-np.pi), op0=ALU.min, op1=ALU.max),
                         reads=[t1], writes=[t1])
                    P.op("act", lambda e, w=w, dst=dst, c0=c0: e.activation(out=dst[:, c0:c0 + w], in_=t1[:, 0:w], func=AF.Sin), reads=[t1], writes=[dst])
                P.op("dve", lambda e, w=w, c0=c0: e.tensor_scalar(out=St[:, c0:c0 + w], in0=St[:, c0:c0 + w], scalar1=vec_t[:, 1:2], scalar2=None, op0=ALU.mult),
                     reads=[St, vec_t], writes=[St])
            P.barrier()

        import os
        P1STOP = os.environ.get("P1STOP", "")
        blk = lambda h: (h % 2) * 4 + h // 2
        H = [Buf("h%d" % i_, DB[i_ // 2].t[:, (i_ % 2) * 512:(i_ % 2 + 1) * 512]) for i_ in range(8)]
        RK = NKB + 1
        xt = [P.sb("xt%d" % i, [128, D], F32, s1) for i in range(2)]
        xn = P.sb("xn", [128, D], F32, s1)
        ub = P.sb("ub", [128, D], BF16, s1)
        uT = P.sb("uT", [128, 8, 128], BF16, s1)
        stA = P.sb("stA", [128, 16], F32, s1)
        jkA = P.sb("jkA", [128, D], BF16, s1)
        qaT = [P.sb("qaT%d" % i, [128, 4, 128], BF16, s1) for i in range(2)]
        qsT = [P.sb("qsT%d" % i, [128, 4, 128], BF16, s1) for i in range(2)]
        kaT = [P.sb("kaT%d" % i, [128, 2, 128], BF16, s1) for i in range(3)]
        vaA = [P.sb("vaA%d" % i, [128, 2, 128], BF16, s1) for i in range(3)]
        ksT = [P.sb("ksT%d" % i, [128, 4, 128], BF16, s1) for i in range(RK)]
        vsB = [P.sb("vsB%d" % i, [128, 512], BF16, s1) for i in range(RK)]
        spB = [P.sb("spB%d" % i, [128, 1024], BF16, s1) for i in range(NKB)]
        tmpa = P.sb("tmpa", [128, 512], F32, s1)
        tmpb = P.sb("tmpb", [128, 512], F32, s1)
        pb16 = P.sb("pb16", [128, 1024], BF16, s1)
        pm16 = P.sb("pm16", [128, 1024], BF16, s1)
        efs = [P.sb("ef%d" % i, [128, 1024], BF16, s1) for i in range(NKB)]
        jb = junk.t[:, :].bitcast(BF16)
        ecs = [Buf("ec%d" % i, jb[:, i * 1024:(i + 1) * 1024]) for i in range(2)]
        wbs = [P.sb("wb%d" % i, [128, 1024], BF16, s1) for i in range(2)]
        mixf = P.sb("mixf", [128, D], F32, s1)
        mixbs = [P.sb("mixb%d" % i, [128, D], BF16, s1) for i in range(2)]
        jkB = P.sb("jkB", [128, D], BF16, s1)
        mixT = P.sb("mixT", [128, 8, 128], BF16, s1)
        den = P.sb("den", [128, 8], F32, s1)
        ss = P.sb("ss", [128, 4], F32, s1)
        pres = [P.sb("pre%d" % i, [128, D], F32, s1) for i in range(2)]
        P.op("dve", lambda e: e.memset(stA[:], EPS), writes=[stA])
        for v in vaA:
            P.op("dve", lambda e, v=v: e.memset(v[:], 1.0), writes=[v])
        xh_v = xh.t.rearrange("(j p) d -> j p d", p=128)
        x1s_v = x1s.t.rearrange("(j p) d -> j p d", p=128)
        r3 = lambda ap, n: ap.rearrange("p (c t) -> p c t", c=n)
        m8v = lambda mi: msk_t[:, mi, :].unsqueeze(1).broadcast_to([128, 8, 128])

        def fm(col0, nch, bank):
            for ch in range(nch):
                for k in range(8):
                    P.op("pe", lambda e, ch=ch, k=k: e.matmul(bank[:, ch * 128:(ch + 1) * 128], lhsT=win[:, k, col0 + ch * 128:col0 + (ch + 1) * 128],
                                                               rhs=uT[:, k, :], start=(k == 0), stop=(k == 7)), reads=[win, uT], writes=[bank])

        def stageA(j):
            own = j >= HB
            x = xt[j % 2]
            if j + 1 < HB + NT:
                xn_ = xt[(j + 1) % 2]
                P.dma("sp", lambda e: e.dma_start(out=xn_[:], in_=xh_v[j + 1]), xn_, reads=[xh], writes=[xn_])
            ln_stats(x, st=stA, junk=jkA)
            yield
            P.op("act", lambda e: e.activation(out=xn[:], in_=x[:], func=AF.Identity, scale=stA[:, 4:5], bias=stA[:, 5:6]), reads=[x, stA], writes=[xn])
            yield
            P.op("dve", lambda e: e.tensor_tensor(out=xn[:], in0=xn[:], in1=ada[:, SC1], op=ALU.mult), reads=[xn, ada], writes=[xn])
            P.op("dve", lambda e: e.tensor_tensor(out=ub[:], in0=xn[:], in1=ada[:, SH1], op=ALU.add), reads=[xn, ada], writes=[ub])
            yield
            transpose8(ub, uT, H[0].t, H[0])
            yield
            ks_slot, vs_slot = ksT[j % RK], vsB[j % RK]
            ka_slot, va_slot = kaT[j % 3], vaA[j % 3]
            tok = slice(j * 128, (j + 1) * 128)
            Cb = lambda n: Ct[:, tok].unsqueeze(1).broadcast_to([128, n, 128])
            Sb = lambda n: St[:, tok].unsqueeze(1).broadcast_to([128, n, 128])
            if own:
                fm(O_QA, 4, H[1])
                yield
                fm(O_QASW, 4, H[0])
                yield
                qa_ = qaT[j % 2]
                P.op("dve", lambda e: e.tensor_tensor(out=r3(tmpa[:], 4), in0=r3(H[1][:, :], 4), in1=Cb(4), op=ALU.mult), reads=[H[1], Ct], writes=[tmpa])
                P.op("dve", lambda e: e.tensor_tensor(out=r3(tmpb[:], 4), in0=r3(H[0][:, :], 4), in1=Sb(4), op=ALU.mult), reads=[H[0], St], writes=[tmpb])
                yield
                P.op("dve", lambda e: e.tensor_tensor(out=qa_[:], in0=r3(tmpa[:], 4), in1=r3(tmpb[:], 4), op=ALU.add), reads=[tmpa, tmpb], writes=[qa_])
            fm(O_KA, 4, H[1])
            yield
            if own:
                fm(O_QS, 4, H[0])
                yield
            P.op("dve", lambda e: e.tensor_tensor(out=r3(tmpa[:, 0:256], 2), in0=r3(H[1][:, 0:256], 2), in1=Cb(2), op=ALU.mult), reads=[H[1], Ct], writes=[tmpa])
            P.op("dve", lambda e: e.tensor_tensor(out=r3(tmpb[:, 0:256], 2), in0=r3(H[1][:, 256:512], 2), in1=Sb(2), op=ALU.mult), reads=[H[1], St], writes=[tmpb])
            yield
            P.op("dve", lambda e: e.tensor_tensor(out=ka_slot[:], in0=r3(tmpa[:, 0:256], 2), in1=r3(tmpb[:, 0:256], 2), op=ALU.add), reads=[tmpa, tmpb], writes=[ka_slot])
            if own:
                qs_ = qsT[j % 2]
                P.op("act", lambda e: e.activation(out=qs_[:], in_=r3(H[0][:, :], 4), func=AF.Copy), reads=[H[0]], writes=[qs_])
            yield
            fm(O_KS, 4, H[1])
            yield
            for k in range(8):
                P.op("pe", lambda e, k=k: e.matmul(H[0][:, :], lhsT=uT[:, k, :], rhs=win[:, k, O_VS:O_VS + 512], start=(k == 0), stop=(k == 7)),
                     reads=[win, uT], writes=[H[0]])
            yield
            P.op("act", lambda e: e.activation(out=ks_slot[:], in_=r3(H[1][:, :], 4), func=AF.Copy), reads=[H[1]], writes=[ks_slot])
            yield
            P.op("act", lambda e: e.activation(out=vs_slot[:], in_=H[0][:, :], func=AF.Copy), reads=[H[0]], writes=[vs_slot])
            for k in range(8):
                P.op("pe", lambda e, k=k: e.matmul(H[1][:, 0:128], lhsT=uT[:, k, :], rhs=win[:, k, O_VA:O_VA + 128], start=(k == 0), stop=(k == 7)),
                     reads=[win, uT], writes=[H[1]])
            yield
            P.op("dve", lambda e: e.tensor_copy(out=va_slot[:, :, 0:64], in_=H[1][:, 0:128].rearrange("p (k d) -> p k d", k=2)), reads=[H[1]], writes=[va_slot])

        def stageB1(j):
            i = j - HB
            qa_, qs_ = qaT[j % 2], qsT[j % 2]
            mixb_ = mixbs[j % 2]
            pre_ = pres[j % 2]
            P.dma("sp", lambda e: e.dma_start(out=pre_[:], in_=xh_v[j]), pre_, reads=[xh], writes=[pre_])
            for a in range(2):
                P.op("pe", lambda e, a=a: e.matmul(H[6 + a][:, :], lhsT=zer[:], rhs=win[:, 0, 0:512], start=True, stop=False), reads=[zer, win], writes=[H[6 + a]])
            for kt in range(2):
                kslot = kaT[(j - 1 + kt) % 3]
                vslot = vaA[(j - 1 + kt) % 3]
                for h in range(8):
                    hp = (h % 2) * 64
                    bk = H[4 + h % 2]
                    P.op("pe", lambda e, h=h, hp=hp, bk=bk: e.matmul(bk[:, (h // 2) * 128:(h // 2 + 1) * 128], lhsT=kslot[hp:hp + 64, h // 4, :],
                                                                     rhs=qa_[hp:hp + 64, h // 2, :], start=True, stop=True), reads=[kslot, qa_], writes=[bk])
                yield
                for hf in range(2):
                    P.op("act", lambda e, hf=hf: e.activation(out=pb16[:, hf * 512:(hf + 1) * 512], in_=H[4 + hf][:, :], func=AF.Exp, scale=0.125), reads=[H[4 + hf]], writes=[pb16])
                yield
                mi = 1 if kt == 1 else (3 if i == 0 else 0)
                P.op("dve", lambda e, mi=mi: e.tensor_tensor(out=r3(pm16[:], 8), in0=r3(pb16[:], 8), in1=m8v(mi), op=ALU.mult), reads=[pb16, msk_t], writes=[pm16])
                yield
                for h in range(8):
                    bk = H[6 + h // 4]
                    P.op("pe", lambda e, h=h, bk=bk: e.matmul(bk[:, (h % 4) * 128:(h % 4 + 1) * 128],
                                                              lhsT=pm16[:, blk(h) * 128:(blk(h) + 1) * 128], rhs=vslot[:, h // 4, :], start=False, stop=(kt == 1 and h % 4 == 3)),
                         reads=[pm16, vslot], writes=[bk])
            yield
            oav = lambda a: H[6 + a][:, :].rearrange("p (h d) -> p h d", h=4)
            for a in range(2):
                P.op("dve", lambda e, a=a: e.tensor_tensor(out=den[:, a * 4:(a + 1) * 4], in0=oav(a)[:, :, 64], in1=esink[:, a * 4:(a + 1) * 4], op=ALU.add),
                     reads=[H[6 + a], esink], writes=[den])
            P.op("dve", lambda e: e.reciprocal(out=den[:], in_=den[:]), reads=[den], writes=[den])
            yield
            for a in range(2):
                P.op("dve", lambda e, a=a: e.tensor_tensor(out=mixf[:, a * 256:(a + 1) * 256].rearrange("p (h d) -> p h d", h=4), in0=oav(a)[:, :, 0:64],
                                                           in1=den[:, a * 4:(a + 1) * 4].unsqueeze(2).broadcast_to([128, 4, 64]), op=ALU.mult),
                     reads=[H[6 + a], den], writes=[mixf])
            P.op("pe", lambda e: e.matmul(H[3][:, :], lhsT=zer[:], rhs=win[:, 0, 0:512], start=True, stop=False), reads=[zer, win], writes=[H[3]])

            def zmm(kk):
                ksl = ksT[(j - kk) % RK]
                for h in range(8):
                    hp = (h % 2) * 64
                    bk = H[4 + h % 2]
                    P.op("pe", lambda e, h=h, hp=hp, bk=bk: e.matmul(bk[:, (h // 2) * 128:(h // 2 + 1) * 128], lhsT=ksl[hp:hp + 64, h // 2, :],
                                                                     rhs=qs_[hp:hp + 64, h // 2, :], start=True, stop=True), reads=[ksl, qs_], writes=[bk])

            def eexp(kk):
                ef_ = efs[kk]
                for hf in range(2):
                    P.op("act", lambda e, hf=hf: e.activation(out=ef_[:, hf * 512:(hf + 1) * 512], in_=H[4 + hf][:, :], func=AF.Exp, scale=0.125), reads=[H[4 + hf]], writes=[ef_])
                mi = 2 if kk == 0 else (4 if (j - kk) < HB else None)
                if mi is not None:
                    P.op("dve", lambda e: e.tensor_tensor(out=r3(ef_[:], 8), in0=r3(ef_[:], 8), in1=m8v(mi), op=ALU.mult), reads=[ef_, msk_t], writes=[ef_])

            def splog(kk):
                P.op("act", lambda e: e.activation(out=spB[kk][:], in_=efs[kk][:], func=AF.Ln, bias=1.0), reads=[efs[kk]], writes=[spB[kk]])

            def cmm(kk):
                for half in range(2):
                    hs = slice(half * 512, (half + 1) * 512)
                    P.op("pe", lambda e, hs=hs, half=half: e.matmul(H[6 + half][:, :], lhsT=tri[:], rhs=spB[kk][:, hs], start=True, stop=(kk == 0)),
                         reads=[tri, spB[kk]], writes=[H[6 + half]])
                    for k2 in range(kk):
                        P.op("pe", lambda e, hs=hs, k2=k2, half=half: e.matmul(H[6 + half][:, :], lhsT=ones[:], rhs=spB[k2][:, hs], start=False, stop=(k2 == kk - 1)),
                             reads=[ones, spB[k2]], writes=[H[6 + half]])

            def ecexp(kk):
                ec_ = ecs[kk % 2]
                for hf in range(2):
                    P.op("act", lambda e, hf=hf: e.activation(out=ec_[:, hf * 512:(hf + 1) * 512], in_=H[6 + hf][:, :], func=AF.Exp, scale=-1.0), reads=[H[6 + hf]], writes=[ec_])

            def wmul(kk):
                P.op("dve", lambda e: e.tensor_tensor(out=wbs[kk % 2][:], in0=efs[kk][:], in1=ecs[kk % 2][:], op=ALU.mult), reads=[efs[kk], ecs[kk % 2]], writes=[wbs[kk % 2]])

            def pv(kk):
                vsl = vsB[(j - kk) % RK]
                wb_ = wbs[kk % 2]
                for h in range(8):
                    P.op("pe", lambda e, h=h: e.matmul(H[3][:, h * 64:(h + 1) * 64], lhsT=wb_[:, blk(h) * 128:(blk(h) + 1) * 128],
                                                       rhs=vsl[:, h * 64:(h + 1) * 64], start=False, stop=(kk == NKB - 1 and h == 7)),
                         reads=[wb_, vsl], writes=[H[3]])

            yield
            zmm(0)
            yield
            eexp(0)
            yield
            for kk in range(NKB):
                if kk + 1 < NKB:
                    zmm(kk + 1)
                splog(kk)
                yield
                if kk + 1 < NKB:
                    eexp(kk + 1)
                cmm(kk)
                yield
                ecexp(kk)
                if kk >= 1:
                    pv(kk - 1)
                yield
                wmul(kk)
                yield
            pv(NKB - 1)
            yield
            P.op("act", lambda e: e.activation(out=mixf[:, 512:1024], in_=H[3][:, :], func=AF.Copy), reads=[H[3]], writes=[mixf])
            yield
            P.op("dve", lambda e: e.memset(ss[:], 0.0), writes=[ss])
            for a in range(2):
                P.op("act", lambda e, a=a: e.activation(out=pb16[:, 0:512], in_=mixf[:, a * 512:(a + 1) * 512], func=AF.Square, accum_out=ss[:, a:a + 1]),
                     reads=[mixf], writes=[pb16, ss])
            yield
            P.op("act", lambda e: e.activation(out=ss[:, 2:4], in_=ss[:, 0:2], func=AF.Ln, scale=1.0 / 512, bias=epsT[:, 0:1]), reads=[ss, epsT], writes=[ss])
            P.op("act", lambda e: e.activation(out=ss[:, 0:2], in_=ss[:, 2:4], func=AF.Exp, scale=-0.5), reads=[ss], writes=[ss])
            yield
            for a in range(2):
                hs = slice(a * 512, (a + 1) * 512)
                P.op("dve", lambda e, a=a, hs=hs: e.scalar_tensor_tensor(out=mixb_[:, hs], in0=mixf[:, hs], scalar=ss[:, a:a + 1], in1=gmix_t[:, hs],
                                                                          op0=ALU.mult, op1=ALU.mult), reads=[mixf, ss, gmix_t], writes=[mixb_])

        def stageB2(j):
            i = j - HB
            mixb_ = mixbs[j % 2]
            pre = pres[j % 2]
            transpose8(mixb_, mixT, H[2].t, H[2])
            yield
            for half in range(2):
                hs = slice(half * 512, (half + 1) * 512)
                for k in range(8):
                    P.op("pe", lambda e, k=k, hs=hs: e.matmul(H[2][:, :], lhsT=mixT[:, k, :], rhs=wout[:, k, hs], start=(k == 0), stop=(k == 7)),
                         reads=[mixT, wout], writes=[H[2]])
                yield
                P.op("dve", lambda e, hs=hs: e.scalar_tensor_tensor(out=pre[:, hs], in0=pre[:, hs], scalar=ALPHA, in1=H[2][:, :], op0=ALU.mult, op1=ALU.add),
                     reads=[H[2], pre], writes=[pre])
                yield
            ln_stats(pre, junk=jkB)
            yield
            P.op("act", lambda e: e.activation(out=pre[:], in_=pre[:], func=AF.Identity, scale=st[:, 4:5], bias=st[:, 5:6]), reads=[pre, st], writes=[pre])
            yield
            P.op("dve", lambda e: e.tensor_tensor(out=pre[:], in0=pre[:], in1=lnp_t[:, 0, :], op=ALU.mult), reads=[pre, lnp_t], writes=[pre])
            P.op("dve", lambda e: e.tensor_tensor(out=pre[:], in0=pre[:], in1=lnp_t[:, 1, :], op=ALU.add), reads=[pre, lnp_t], writes=[pre])
            yield
            P.dma("sp", lambda e: e.dma_start(out=x1s_v[i], in_=pre[:]), pre, reads=[pre], writes=[x1s])

        P.dma("sp", lambda e: e.dma_start(out=xt[0][:], in_=xh_v[0]), xt[0], reads=[xh], writes=[xt[0]])
        LAST = HB + NT - 1
        for j in range(HB + NT + 2):
            gens = []
            if HB <= j - 1 <= LAST:
                gens.append(stageB1(j - 1))
            if j <= LAST:
                gens.append(stageA(j))
            if HB <= j - 2 <= LAST:
                gens.append(stageB2(j - 2))
            while gens:
                for g in list(gens):
                    try:
                        next(g)
                    except StopIteration:
                        gens.remove(g)
        P.barrier()
    if upto == 1:
        es.close()
        return nc
    accs_v = accs.t.rearrange("(j p) d -> j p d", p=128)
    x1s_v = x1s.t.rearrange("(j p) d -> j p d", p=128)
    with ExitStack() as s2:
        wr = load_cast(s2, "wr", w_r.t.rearrange("(c p) n -> p c n", p=128), [128, 8, NE], [w_r])
        s1w = load_cast(s2, "s1w", ws1.t.rearrange("(c p) n -> p c n", p=128), [128, 8, FF], [ws1])
        s3w = load_cast(s2, "s3w", ws3.t.rearrange("(c p) n -> p c n", p=128), [128, 8, FF], [ws3])
        s2w = load_cast(s2, "s2w", ws2.t.rearrange("(c p) n -> p c n", p=128), [128, 2, D], [ws2])
        ada = P.sb("ada2", [128, 3 * D], F32, s2)
        P.dma("sp", lambda e: e.dma_start(out=ada[:], in_=adas[:, 3 * D:6 * D]), ada, reads=[adas], writes=[ada])
        SH2, SC2, G2 = SH1, SC1, G1
        eb = P.sb("eb", [128, NE], F32, s2)
        P.dma("sp", lambda e: e.dma_start(out=eb[:], in_=ebias[:]), eb, reads=[ebias], writes=[eb])
        basecap = P.sb("basecap", [128, NE], F32, s2)
        P.dma("sp", lambda e: e.dma_start(out=basecap[:], in_=ecap[:]), basecap, reads=[ecap], writes=[basecap])
        xt = [P.sb("x1l%d" % i, [128, D], F32, s2) for i in range(2)]
        xn = P.sb("xn2", [128, D], F32, s2)
        u2b = [P.sb("u2b%d" % i, [128, D], BF16, s2) for i in range(4)]
        u2T = P.sb("u2T", [128, 8, 128], BF16, s2)
        sil = P.sb("sil", [128, 256], F32, s2)
        aT = P.sb("aT", [128, 2, 128], BF16, s2)
        acc_t = P.sb("acc_t", [128, D], F32, s2)
        bi = P.sb("bi", [128, NE], F32, s2)
        m8 = P.sb("m8", [128, 8, 8], F32, s2)
        gs = P.sb("gs", [128, 8], F32, s2)
        g8 = P.sb("g8", [128, 8], F32, s2)
        gm = P.sb("gm", [128, 8], F32, s2)
        gn = P.sb("gn", [128, 8], F32, s2)
        mk = P.sb("mk", [128, NE], F32, s2)
        t8 = P.sb("t8", [128, 8], F32, s2)
        sel = P.sb("sel", [128, NE], F32, s2)
        selb = P.sb("selb", [128, NE], BF16, s2)
        gd = P.sb("gd", [128, NE], F32, s2)
        val = P.sb("val", [128, NE], F32, s2)
        d8 = P.sb("d8", [128, 8], F32, s2)
        rd = P.sb("rd", [128, 2], F32, s2)
        jk = P.sb("jk", [128, NE], F32, s2)
        k8 = P.sb("k8", [128, 8], F32, s2)
        okm = P.sb("okm", [128, NE], F32, s2)
        prow = P.sb("prow", [128, 1], F32, s2)
        P.dma("sp", lambda e: e.dma_start(out=prow[:], in_=prowc[:]), prow, reads=[prowc], writes=[prow])
        elim = P.sb("elim", [128, NE], F32, s2)
        P.dma("sp", lambda e: e.dma_start(out=elim[:], in_=elimc[:]), elim, reads=[elimc], writes=[elim])
        k8i = P.sb("k8i", [128, 8], I32, s2)
        k8f = P.sb("k8f", [128, 8], F32, s2)
        e4 = P.sb("e4", [128, NE], F32, s2)
        P.dma("sp", lambda e: e.dma_start(out=e4[:], in_=e4c[:]), e4, reads=[e4c], writes=[e4])
        P.dma("sp", lambda e: e.dma_start(out=xt[0][:], in_=x1s_v[0]), xt[0], reads=[x1s], writes=[xt[0]])
        scs = [P.sb("sc%d" % i_, [128, NE], F32, s2) for i_ in range(2)]
        dest8_b = [Buf("dest8_%d" % i_, dest8.t[:, i_, :]) for i_ in range(NT)]
        gate8_b = [Buf("gate8_%d" % i_, gate8.t[:, i_, :]) for i_ in range(NT)]

        def stage1(i):
            sc = scs[i % 2]
            x = xt[i % 2]
            ub = u2b[i % 4]
            if i + 1 < NT:
                xn_ = xt[(i + 1) % 2]
                P.dma("sp", lambda e, i=i, xn_=xn_: e.dma_start(out=xn_[:], in_=x1s_v[i + 1]), xn_, reads=[x1s], writes=[xn_])
            ln_stats(x)
            P.op("act", lambda e: e.activation(out=xn[:], in_=x[:], func=AF.Identity, scale=st[:, 4:5], bias=st[:, 5:6]), reads=[x, st], writes=[xn])
            P.op("dve", lambda e: e.tensor_tensor(out=xn[:], in0=xn[:], in1=ada[:, SC2], op=ALU.mult), reads=[xn, ada], writes=[xn])
            P.op("dve", lambda e: e.tensor_tensor(out=ub[:], in0=xn[:], in1=ada[:, SH2], op=ALU.add), reads=[xn, ada], writes=[ub])
            yield
            transpose8(ub, u2T, DB[0][:, 0:512], DB[0], all_act=True)
            for k in range(8):
                P.op("pe", lambda e, k=k: e.matmul(DB[0][:, 512:512 + NE], lhsT=u2T[:, k, :], rhs=wr[:, k, :], start=(k == 0), stop=(k == 7)),
                     reads=[u2T, wr], writes=[DB[0]])
            for wi, wsx in enumerate((s1w, s3w)):
                for fc in range(2):
                    o = (wi * 2 + fc) * 128
                    for k in range(8):
                        P.op("pe", lambda e, k=k, wsx=wsx, fc=fc, o=o: e.matmul(DB[1][:, o:o + 128], lhsT=wsx[:, k, fc * 128:(fc + 1) * 128], rhs=u2T[:, k, :],
                                                                                start=(k == 0), stop=(k == 7)), reads=[wsx, u2T], writes=[DB[1]])
            yield
            P.op("act", lambda e: e.activation(out=sil[:], in_=DB[1][:, 0:256], func=AF.Silu), reads=[DB[1]], writes=[sil])
            P.op("dve", lambda e: e.tensor_tensor(out=aT[:].rearrange("p c t -> p (c t)"), in0=sil[:], in1=DB[1][:, 256:512], op=ALU.mult), reads=[sil, DB[1]], writes=[aT])
            for half in range(2):
                hs = slice(half * 512, (half + 1) * 512)
                for fc in range(2):
                    P.op("pe", lambda e, fc=fc, hs=hs: e.matmul(DB[2][:, hs], lhsT=aT[:, fc, :], rhs=s2w[:, fc, hs], start=(fc == 0), stop=(fc == 1)),
                         reads=[aT, s2w], writes=[DB[2]])
            for hf in range(2):
                P.op("dve", lambda e, hf=hf: e.tensor_tensor(out=acc_t[:, hf * 512:(hf + 1) * 512], in0=DB[2][:, hf * 512:(hf + 1) * 512], in1=ada[:, 2 * D + hf * 512:2 * D + (hf + 1) * 512], op=ALU.mult), reads=[DB[2], ada], writes=[acc_t])
            P.op("dve", lambda e: e.scalar_tensor_tensor(out=acc_t[:], in0=x[:], scalar=ALPHA, in1=acc_t[:], op0=ALU.mult, op1=ALU.add), reads=[x, acc_t], writes=[acc_t])
            P.dma("sp", lambda e, i=i: e.dma_start(out=accs_v[i], in_=acc_t[:]), acc_t, reads=[acc_t], writes=[accs])
            P.op("act", lambda e: e.activation(out=sc[:], in_=DB[0][:, 512:512 + NE], func=AF.Sigmoid), reads=[DB[0]], writes=[sc])

        def stage2(i):
            sc = scs[i % 2]
            ub = u2b[i % 4]
            P.op("dve", lambda e: e.tensor_tensor(out=bi[:], in0=sc[:], in1=eb[:], op=ALU.add), reads=[sc, eb], writes=[bi])
            for g in range(8):
                P.op("dve", lambda e, g=g: e.max(out=m8[:, g, :], in_=bi[:, g * GS:(g + 1) * GS]), reads=[bi], writes=[m8])
            P.op("dve", lambda e: e.tensor_tensor(out=gs[:], in0=m8[:, :, 0], in1=m8[:, :, 1], op=ALU.add), reads=[m8], writes=[gs])
            P.op("dve", lambda e: e.max(out=g8[:], in_=gs[:]), reads=[gs], writes=[g8])
            P.op("dve", lambda e: e.tensor_scalar(out=gm[:], in0=gs[:], scalar1=g8[:, 3:4], scalar2=None, op0=ALU.is_ge), reads=[gs, g8], writes=[gm])
            P.op("dve", lambda e: e.tensor_scalar(out=gn[:], in0=gm[:], scalar1=-1.0, scalar2=BIG, op0=ALU.add, op1=ALU.mult), reads=[gm], writes=[gn])
            g3 = lambda b: b[:].rearrange("p (g s) -> p g s", g=8)
            gb3 = lambda b: b[:].unsqueeze(2).broadcast_to([128, 8, GS])
            P.op("dve", lambda e: e.tensor_tensor(out=g3(mk), in0=g3(bi), in1=gb3(gm), op=ALU.mult), reads=[bi, gm], writes=[mk])
            P.op("dve", lambda e: e.tensor_tensor(out=g3(mk), in0=g3(mk), in1=gb3(gn), op=ALU.add), reads=[mk, gn], writes=[mk])
            P.op("dve", lambda e: e.max(out=t8[:], in_=mk[:]), reads=[mk], writes=[t8])
            P.op("dve", lambda e: e.tensor_scalar(out=sel[:], in0=mk[:], scalar1=t8[:, 7:8], scalar2=None, op0=ALU.is_ge), reads=[mk, t8], writes=[sel])
            P.op("dve", lambda e: e.tensor_copy(out=selb[:], in_=sel[:]), reads=[sel], writes=[selb])
            P.op("dve", lambda e: e.memset(rd[:], 0.0), writes=[rd])
            P.op("dve", lambda e: e.scalar_tensor_tensor(out=gd[:], in0=sel[:], scalar=1.0, in1=sc[:], op0=ALU.mult, op1=ALU.mult, accum_out=rd[:, 0:1]),
                 reads=[sel, sc], writes=[gd, rd])
            P.op("dve", lambda e: e.reciprocal(out=rd[:, 1:2], in_=rd[:, 0:1]), reads=[rd], writes=[rd])
            P.op("dve", lambda e: e.tensor_scalar(out=gd[:], in0=gd[:], scalar1=rd[:, 1:2], scalar2=2.5, op0=ALU.mult, op1=ALU.mult), reads=[gd, rd], writes=[gd])
            yield
            P.op("pe", lambda e: e.matmul(DB[3][:, 0:NE], lhsT=slt[:], rhs=selb[:], start=True, stop=True), reads=[slt, selb], writes=[DB[3]])
            P.op("pe", lambda e: e.matmul(DB[3][:, 512:512 + NE], lhsT=ones[:], rhs=selb[:], start=True, stop=True), reads=[ones, selb], writes=[DB[3]])
            yield
            P.op("dve", lambda e: e.tensor_tensor(out=val[:], in0=DB[3][:, 0:NE], in1=basecap[:], op=ALU.add), reads=[DB[3], basecap], writes=[val])
            P.op("dve", lambda e: e.tensor_tensor(out=okm[:], in0=val[:], in1=elim[:], op=ALU.is_le), reads=[val, elim], writes=[okm])
            P.op("dve", lambda e: e.tensor_tensor(out=okm[:], in0=okm[:], in1=sel[:], op=ALU.mult), reads=[okm, sel], writes=[okm])
            P.op("dve", lambda e: e.tensor_tensor(out=val[:], in0=val[:], in1=okm[:], op=ALU.mult), reads=[val, okm], writes=[val])
            P.op("dve", lambda e: e.tensor_tensor(out=basecap[:], in0=DB[3][:, 512:512 + NE], in1=basecap[:], op=ALU.add), reads=[DB[3], basecap], writes=[basecap])
            P.op("dve", lambda e: e.max(out=d8[:], in_=val[:]), reads=[val], writes=[d8])
            P.op("dve", lambda e: e.tensor_tensor(out=jk[:], in0=gd[:], in1=e4[:], op=ALU.add), reads=[gd, e4], writes=[jk])
            P.op("dve", lambda e: e.tensor_tensor(out=jk[:], in0=jk[:], in1=okm[:], op=ALU.mult), reads=[jk, okm], writes=[jk])
            P.op("dve", lambda e: e.max(out=k8[:], in_=jk[:]), reads=[jk], writes=[k8])
            P.op("dve", lambda e: e.tensor_scalar(out=k8i[:], in0=k8[:], scalar1=0.25, scalar2=-0.3125, op0=ALU.mult, op1=ALU.add), reads=[k8], writes=[k8i])
            P.op("dve", lambda e: e.tensor_copy(out=k8f[:], in_=k8i[:]), reads=[k8i], writes=[k8f])
            P.op("dve", lambda e, i=i: e.scalar_tensor_tensor(out=gate8_b[i][:, :], in0=k8f[:], scalar=-4.0, in1=k8[:], op0=ALU.mult, op1=ALU.add),
                 reads=[k8f, k8], writes=[gate8_b[i]])
            P.op("dve", lambda e: e.tensor_scalar(out=k8f[:], in0=d8[:], scalar1=0.5, scalar2=prow[:, 0:1], op0=ALU.is_lt, op1=ALU.mult), reads=[d8, prow], writes=[k8f])
            P.op("dve", lambda e, i=i: e.scalar_tensor_tensor(out=dest8_b[i][:, :], in0=d8[:], scalar=-1.0, in1=k8f[:], op0=ALU.add, op1=ALU.add), reads=[d8, k8f], writes=[dest8_b[i]])
            for k in range(8):
                P.dma("pool", lambda e, i=i, k=k, ub=ub: e.indirect_dma_start(out=xe.t[:, :], out_offset=bass.IndirectOffsetOnAxis(ap=dest8_b[i][:, k:k + 1], axis=0),
                                                                               in_=ub[:, :], in_offset=None), ub, reads=[ub, dest8_b[i]], writes=[xe])

        def run(g, n=None):
            k = 0
            for _ in g:
                k += 1
                if n is not None and k >= n:
                    return

        run(stage1(0))
        for i in range(NT):
            g1 = stage1(i + 1) if i + 1 < NT else iter(())
            g2 = stage2(i)
            run(g1, 1)
            run(g2, 1)
            run(g1, 1)
            run(g2, 1)
            run(g2)
            run(g1)
        P.barrier()

    with ExitStack() as s3:
        Wf1 = [P.sb("Wf1_%d" % i, [128, 8, FF], F32, s3) for i in range(2)]
        Wf3 = [P.sb("Wf3_%d" % i, [128, 8, FF], F32, s3) for i in range(2)]
        Wf2 = [P.sb("Wf2_%d" % i, [128, 2, D], F32, s3) for i in range(2)]
        W1 = [P.sb("W1_%d" % i, [128, 8, FF], BF16, s3) for i in range(2)]
        W3 = [P.sb("W3_%d" % i, [128, 8, FF], BF16, s3) for i in range(2)]
        W2 = [P.sb("W2_%d" % i, [128, 2, D], BF16, s3) for i in range(2)]
        xr = [P.sb("xr%d" % i, [128, D], BF16, s3) for i in range(2 * NR)]
        XT = [P.sb("XT%d" % i, [128, 8, CAP], BF16, s3) for i in range(2)]
        sl = [P.sb("sl%d" % i, [128, 2 * CAP], F32, s3) for i in range(2)]
        aE = [P.sb("aE%d" % i, [128, 2, CAP], BF16, s3) for i in range(2)]
        yo = [P.sb("yo%d" % i, [128, D], BF16, s3) for i in range(2)]
        DBH = [[Buf("db%d_%d" % (i, hf), DB[i].t[:, hf * 512:(hf + 1) * 512]) for hf in range(2)] for i in range(4)]

        def load_w(e_):
            b = e_ % 2
            P.dma("sp", lambda e: e.dma_start(out=Wf1[b][:], in_=w1.t[e_].rearrange("(p c) f -> p c f", c=8)), Wf1[b], reads=[w1], writes=[Wf1[b]])
            P.dma("sp", lambda e: e.dma_start(out=Wf3[b][:], in_=w3.t[e_].rearrange("(p c) f -> p c f", c=8)), Wf3[b], reads=[w3], writes=[Wf3[b]])
            P.dma("sp", lambda e: e.dma_start(out=Wf2[b][:], in_=w2.t[e_].rearrange("(p c) d -> p c d", c=2)), Wf2[b], reads=[w2], writes=[Wf2[b]])

        def cast_w(e_):
            b = e_ % 2
            pv = lambda t: t[:].rearrange("p k (c m) -> p k c m", c=2)
            sv = lambda t: t[:].rearrange("p k (m c) -> p k c m", c=2)
            P.op("act", lambda e: e.activation(out=pv(W1[b]), in_=sv(Wf1[b]), func=AF.Copy), reads=[Wf1[b]], writes=[W1[b]])
            P.op("dve", lambda e: e.tensor_copy(out=pv(W3[b]), in_=sv(Wf3[b])), reads=[Wf3[b]], writes=[W3[b]])
            P.op("act", lambda e: e.activation(out=W2[b][:, 0, :], in_=Wf2[b][:, 0, :], func=AF.Copy), reads=[Wf2[b]], writes=[W2[b]])
            P.op("dve", lambda e: e.tensor_copy(out=W2[b][:, 1, :], in_=Wf2[b][:, 1, :]), reads=[Wf2[b]], writes=[W2[b]])

        def load_x(e_):
            for r in range(NR):
                xb_ = xr[(e_ % 2) * NR + r]
                row0 = e_ * CAP + r * 128
                rr_ = RROWS[r]
                P.dma("sp", lambda e, xb_=xb_, row0=row0, rr_=rr_: e.dma_start(out=xb_[0:rr_, :], in_=xe.t[row0:row0 + rr_, :]), xb_, reads=[xe], writes=[xb_])

        def trans(e_):
            for r in range(NR):
                xb_ = xr[(e_ % 2) * NR + r]
                bank = DBH[0][r % 2]
                pv = bank.t.bitcast(BF16)
                rr_ = RROWS[r]
                xs = xb_[:].rearrange("s (p c) -> s c p", c=8)
                for c in range(8):
                    P.op("pe", lambda e, c=c, xs=xs, pv=pv, rr_=rr_: e.transpose(out=pv[:, c * 128:c * 128 + rr_], in_=xs[0:rr_, c, :], identity=ident[0:rr_, 0:rr_]),
                         reads=[xb_, ident], writes=[bank])
                xt_ = XT[e_ % 2]
                P.op("act", lambda e, r=r, pv=pv, xt_=xt_, rr_=rr_: e.activation(out=xt_[:, 0:4, r * 128:r * 128 + rr_], in_=pv[:, 0:512].rearrange("p (c t) -> p c t", c=4)[:, :, 0:rr_], func=AF.Copy),
                     reads=[bank], writes=[xt_])
                P.op("dve", lambda e, r=r, pv=pv, xt_=xt_, rr_=rr_: e.tensor_copy(out=xt_[:, 4:8, r * 128:r * 128 + rr_], in_=pv[:, 512:1024].rearrange("p (c t) -> p c t", c=4)[:, :, 0:rr_]),
                     reads=[bank], writes=[xt_])

        def hmm(e_):
            b = e_ % 2
            for wi, Wx in enumerate((W1[b], W3[b])):
                pb = DBH[1 + b][wi]
                for fc in range(2):
                    for k in range(8):
                        P.op("pe", lambda e, Wx=Wx, pb=pb, fc=fc, k=k: e.matmul(pb[:, fc * CAP:(fc + 1) * CAP], lhsT=Wx[:, k, fc * 128:(fc + 1) * 128], rhs=XT[b][:, k, :],
                                                                                start=(k == 0), stop=(k == 7)), reads=[Wx, XT[b]], writes=[pb])

        def act_(e_):
            b = e_ % 2
            P.op("act", lambda e: e.activation(out=sl[b][:], in_=DBH[1 + b][0][:, 0:2 * CAP], func=AF.Silu), reads=[DBH[1 + b][0]], writes=[sl[b]])
            P.op("dve", lambda e: e.tensor_tensor(out=aE[b][:].rearrange("p c t -> p (c t)"), in0=sl[b][:], in1=DBH[1 + b][1][:, 0:2 * CAP], op=ALU.mult),
                 reads=[sl[b], DBH[1 + b][1]], writes=[aE[b]])

        def ymm(e_):
            b = e_ % 2
            for r in range(NR):
                yb = yo[r % 2]
                rr_ = RROWS[r]
                for half in range(2):
                    hs = slice(half * 512, (half + 1) * 512)
                    for fc in range(2):
                        P.op("pe", lambda e, fc=fc, hs=hs, r=r, half=half, rr_=rr_: e.matmul(DBH[3][half][0:rr_, :], lhsT=aE[b][:, fc, r * 128:r * 128 + rr_], rhs=W2[b][:, fc, hs], start=(fc == 0), stop=(fc == 1)),
                             reads=[aE[b], W2[b]], writes=[DBH[3][half]])
                P.op("act", lambda e, yb=yb, rr_=rr_: e.activation(out=yb[0:rr_, 0:512], in_=DBH[3][0][0:rr_, :], func=AF.Copy), reads=[DBH[3][0]], writes=[yb])
                P.op("dve", lambda e, yb=yb, rr_=rr_: e.tensor_copy(out=yb[0:rr_, 512:1024], in_=DBH[3][1][0:rr_, :]), reads=[DBH[3][1]], writes=[yb])
                row0 = e_ * CAP + r * 128
                P.dma("pool", lambda e, yb=yb, row0=row0, rr_=rr_: e.dma_start(out=ye.t[row0:row0 + rr_, :], in_=yb[0:rr_, :]), yb, reads=[yb], writes=[ye])

        P.op("dve", lambda e: e.memset(yo[0][:], 0.0), writes=[yo[0]])
        P.dma("pool", lambda e: e.dma_start(out=ye.t[NE * CAP:NE * CAP + 128, :], in_=yo[0][:]), yo[0], reads=[yo[0]], writes=[ye])
        load_w(0)
        if NE > 1:
            load_w(1)
        load_x(0)
        cast_w(0)
        trans(0)
        for e_ in range(NE):
            hmm(e_)
            if e_ + 1 < NE:
                load_x(e_ + 1)
                cast_w(e_ + 1)
                trans(e_ + 1)
            if e_ + 2 < NE:
                load_w(e_ + 2)
            act_(e_)
            ymm(e_)
        P.barrier()
    if upto == 3:
        es.close()
        return nc
    out_v = out.t.rearrange("(j p) d -> j p d", p=128)
    with ExitStack() as s4:
        yk = [P.sb("yk%d" % i, [128, 8, D], BF16, s4) for i in range(3)]
        ac = [P.sb("ac%d" % i, [128, D], F32, s4) for i in range(3)]
        ada = P.sb("ada4", [128, D], F32, s4)
        P.dma("sp", lambda e: e.dma_start(out=ada[:], in_=adas[:, 5 * D:6 * D]), ada, reads=[adas], writes=[ada])
        G2 = slice(0, D)
        lnp_t = P.sb("lnp2", [128, 2, D], F32, s4)
        P.dma("sp", lambda e: e.dma_start(out=lnp_t[:], in_=lnp[:, 2:4, :]), lnp_t, reads=[lnp], writes=[lnp_t])

        dest8_b = [Buf("dest8_%d" % i_, dest8.t[:, i_, :]) for i_ in range(NT)]
        gate8_b = [Buf("gate8_%d" % i_, gate8.t[:, i_, :]) for i_ in range(NT)]
        rrs = [P.sb("rr%d" % i_, [128, D], F32, s4) for i_ in range(2)]
        ots = [P.sb("ot%d" % i_, [128, D], F32, s4) for i_ in range(2)]
        dgs = [P.sb("dg%d" % i_, [128, 8, 128], BF16, s4) for i_ in range(2)]
        DBH4 = [[Buf("p4db%d_%d" % (i_, hf), DB[i_].t[:, hf * 512:(hf + 1) * 512]) for hf in range(2)] for i_ in range(2)]

        for y0 in yk:
            P.op("dve", lambda e, y0=y0: e.memset(y0[:], 0.0), writes=[y0])
        P.barrier()
        ykk = [[Buf("yk%d_%d" % (i_, k), yk[i_].t[:, k, :]) for k in range(8)] for i_ in range(3)]

        def loads(i):
            P.dma("sp", lambda e: e.dma_start(out=ac[i % 3][:], in_=accs_v[i]), ac[i % 3], reads=[accs], writes=[ac[i % 3]])
            for k in range(8):
                yb_ = ykk[i % 3][k]
                P.dma("pool", lambda e, k=k, yb_=yb_: e.indirect_dma_start(out=yb_[:, :], out_offset=None, in_=ye.t[:, :],
                                                                           in_offset=bass.IndirectOffsetOnAxis(ap=dest8_b[i][:, k:k + 1], axis=0)),
                      yb_, reads=[ye, dest8_b[i]], writes=[yb_])

        def stage1(i):
            y_ = yk[i % 3]
            a_ = ac[i % 3]
            dg = dgs[i % 2]
            r_ = rrs[i % 2]
            P.op("dve", lambda e: e.tensor_tensor(out=dg[:], in0=ident[:].unsqueeze(1).broadcast_to([128, 8, 128]),
                                                  in1=gate8_b[i][:, :].unsqueeze(2).broadcast_to([128, 8, 128]), op=ALU.mult),
                 reads=[ident, gate8_b[i]], writes=[dg])
            for hf in range(2):
                pb = DBH4[i % 2][hf]
                for k in range(8):
                    P.op("pe", lambda e, k=k, hf=hf, pb=pb: e.matmul(pb[:, :], lhsT=dg[:, k, :], rhs=ykk[i % 3][k][:, hf * 512:(hf + 1) * 512], start=(k == 0), stop=(k == 7)),
                         reads=[dg, ykk[i % 3][k]], writes=[pb])
            for hf in range(2):
                pb = DBH4[i % 2][hf]
                hs = slice(hf * 512, (hf + 1) * 512)
                if debug:
                    P.op("act", lambda e, hs=hs, pb=pb: e.activation(out=junk[:, hs], in_=pb[:, :], func=AF.Copy), reads=[pb], writes=[junk])
                P.op("dve", lambda e, hs=hs, pb=pb: e.tensor_tensor(out=r_[:, hs], in0=pb[:, :], in1=ada[:, hs], op=ALU.mult), reads=[pb, ada], writes=[r_])
            if debug:
                P.dma("sp", lambda e: e.dma_start(out=dbg["y_moe"].t[i * 128:(i + 1) * 128, :], in_=junk[:]), junk, reads=[junk], writes=[dbg["y_moe"]])
            P.op("dve", lambda e: e.tensor_tensor(out=r_[:], in0=r_[:], in1=a_[:], op=ALU.add), reads=[r_, a_], writes=[r_])

        def stage2(i):
            r_ = rrs[i % 2]
            ot = ots[i % 2]
            ln_stats(r_)
            P.op("act", lambda e: e.activation(out=ot[:], in_=r_[:], func=AF.Identity, scale=st[:, 4:5], bias=st[:, 5:6]), reads=[r_, st], writes=[ot])
            P.op("dve", lambda e: e.tensor_tensor(out=ot[:], in0=ot[:], in1=lnp_t[:, 0, :], op=ALU.mult), reads=[ot, lnp_t], writes=[ot])
            P.op("dve", lambda e: e.tensor_tensor(out=ot[:], in0=ot[:], in1=lnp_t[:, 1, :], op=ALU.add), reads=[ot, lnp_t], writes=[ot])
            P.dma("sp", lambda e: e.dma_start(out=out_v[i], in_=ot[:]), ot, reads=[ot], writes=[out])

        loads(0)
        if NT > 1:
            loads(1)
        stage1(0)
        for i in range(NT):
            if i + 1 < NT:
                stage1(i + 1)
            if i + 2 < NT:
                loads(i + 2)
            stage2(i)
        P.barrier()
    fin = [out] + list(dbg.values())
    P.finish(fin)
    es.close()
    return nc


def _consts(NE, CAP, first_half):
    cst = np.zeros((128, 5, 128), np.float32)
    j = np.arange(128)[:, None]
    s = np.arange(128)[None, :]
    cst[:, 0] = np.eye(128, dtype=np.float32)
    cst[:, 1] = (j >= s)
    cst[:, 2] = 1.0
    cst[:, 3] = (j < s)
    msk = np.zeros((128, 5, 1024), np.float32)
    rep = lambda m: np.tile(m.astype(np.float32), (1, 8))
    prev = (j > s)
    msk[:, 0] = rep(prev)
    msk[:, 1] = rep(j <= s)
    msk[:, 2] = rep(j < s)
    msk[:, 3] = 0.0 if first_half else rep(prev)
    msk[:, 4] = 0.0 if first_half else 1.0
    vec = np.zeros((128, 4), np.float32)
    p = np.arange(128)
    inv = (10000.0 ** (-(np.arange(32, dtype=np.float32) * 2.0 / 64))).astype(np.float32)
    vec[:, 0] = inv[p % 32]
    vec[:, 1] = np.where((p % 64) < 32, -1.0, 1.0)
    ecap = np.tile((np.arange(NE, dtype=np.float32) * CAP + 1.0)[None, :], (128, 1))
    e4c = np.tile(((np.arange(NE, dtype=np.float32) + 1.0) * 4.0)[None, :], (128, 1))
    elimc = np.tile(((np.arange(NE, dtype=np.float32) + 1.0) * CAP)[None, :], (128, 1))
    prowc = (NE * CAP + 1 + np.arange(128, dtype=np.float32)).reshape(128, 1)
    return cst, msk, vec, ecap, e4c, elimc, prowc


def _win_layout(w_in):
    def sw(w):
        n = w.shape[1] // 64
        w4 = w.reshape(w.shape[0], n, 2, 32)
        return w4[:, :, ::-1, :].reshape(w.shape[0], n * 64)
    qa, ka, va = w_in[:, 0:512], w_in[:, 512:640], w_in[:, 640:768]
    qs, ks, vs = w_in[:, 768:1280], w_in[:, 1280:1792], w_in[:, 1792:2304]
    dup = lambda k: np.concatenate([k[:, 0:64], k[:, 0:64], k[:, 64:128], k[:, 64:128]], axis=1)
    return np.ascontiguousarray(np.concatenate([qa, sw(qa), dup(ka), dup(sw(ka)), qs, ks, vs, va], axis=1))


def make_in_maps(inp, n_cores, NT, HB, NE, CAP, seq):
    rb = lambda v, n=128: np.ascontiguousarray(np.broadcast_to(np.asarray(v, np.float32).reshape(1, -1), (n, np.asarray(v).size)))
    x = np.asarray(inp["x"], np.float32)
    pos = np.asarray(inp["positions"], np.int32)
    halves = seq // (NT * 128)
    win = _win_layout(np.asarray(inp["w_in"], np.float32)[0])
    shared = {
        "w_ada": np.asarray(inp["w_ada"], np.float32)[0],
        "b_ada": rb(inp["b_ada"][0]),
        "w_in": win,
        "w_out": np.asarray(inp["w_out"], np.float32)[0],
        "gmix": rb(np.concatenate([np.asarray(inp["g_swa"])[0], np.asarray(inp["g_sb"])[0]])),
        "sinks": rb(inp["attn_sinks"][0]),
        "lnp": np.ascontiguousarray(np.stack([rb(inp["ln1_g"][0]), rb(inp["ln1_b"][0]), rb(inp["ln2_g"][0]), rb(inp["ln2_b"][0])], axis=1)),
        "w_r": np.asarray(inp["w_router"], np.float32)[0],
        "ebias": rb(inp["e_bias"][0]),
        "w1": np.asarray(inp["w1"], np.float32)[0],
        "w3": np.asarray(inp["w3"], np.float32)[0],
        "w2": np.asarray(inp["w2"], np.float32)[0],
        "ws1": np.asarray(inp["ws1"], np.float32)[0],
        "ws3": np.asarray(inp["ws3"], np.float32)[0],
        "ws2": np.asarray(inp["ws2"], np.float32)[0],
    }
    maps = []
    for c in range(n_cores):
        b, h = c // halves, c % halves
        s0 = h * NT * 128
        lo = s0 - HB * 128
        xh = np.zeros(((HB + NT) * 128, D), np.float32)
        ph = np.zeros(((HB + NT) * 128,), np.int32)
        src_lo = max(lo, 0)
        xh[src_lo - lo:] = x[b, src_lo:s0 + NT * 128]
        ph[src_lo - lo:] = pos[b, src_lo:s0 + NT * 128]
        cst, msk, vec, ecap, e4c, elimc, prowc = _consts(NE, CAP, first_half=(h == 0))
        m = dict(shared)
        m.update({
            "xh": xh,
            "posb": np.ascontiguousarray(np.broadcast_to(ph[None, :], (128, ph.size))),
            "csil": np.ascontiguousarray(np.asarray(inp["c"], np.float32)[b].reshape(8, 128).T),
            "cst": cst, "msk": msk, "vec": vec, "ecap": ecap, "e4c": e4c, "elimc": elimc, "prowc": prowc,
        })
        maps.append(m)
    return maps


NT_FULL, HB_FULL, NE_FULL, CAP_FULL, NKB_FULL = 32, 2, 256, 256, 3


def kernel(**inputs):
    nc = build_program(NT_FULL, HB_FULL, NE_FULL, CAP_FULL, NKB_FULL)
    maps = make_in_maps(inputs, 8, NT_FULL, HB_FULL, NE_FULL, CAP_FULL, 8192)
    res = run_bass_kernel_spmd(nc, maps, core_ids=list(range(8)))
    outs = [np.asarray(r["out"]) for r in res.results]
    full = np.stack(outs, axis=0).reshape(4, 8192, D)
    return full.astype(np.float32)
```

```python
import numpy as np
from contextlib import ExitStack
import concourse.bass as bass
import concourse.mybir as mybir
from concourse.bass_utils import run_bass_kernel_spmd

F32 = mybir.dt.float32
BF16 = mybir.dt.bfloat16
I32 = mybir.dt.int32
AF = mybir.ActivationFunctionType
ALU = mybir.AluOpType

D = 1024
FF = 256
ALPHA = 2.0 ** 0.25
EPS = 1e-5
WCOLS = 3200
O_QA, O_QASW, O_KA, O_KASW, O_QS, O_KS, O_VS, O_VA = 0, 512, 1024, 1280, 1536, 2048, 2560, 3072
BIG = 1.0e9
TWO_PI = 6.283185307179586
C1 = 6.28125
C2 = TWO_PI - C1


class Buf:
    def __init__(self, name, t):
        self.name = name
        self.t = t
        self.w = {}
        self.r = {}

    def __getitem__(self, k):
        return self.t[k]


class Prog:
    def __init__(self, nc, es):
        self.nc = nc
        self.es = es
        self.eng = {"pe": nc.tensor, "act": nc.scalar, "dve": nc.vector, "sp": nc.sync, "pool": nc.gpsimd}
        self.semobj = {}
        self.cnt = {}
        for e in ("pe", "act", "dve"):
            self.semobj[e] = es.enter_context(nc.semaphore("s_" + e))
            self.cnt[e] = 0
        self.waited = {e: {} for e in self.eng}

    def sb(self, name, shape, dt, es=None):
        return Buf(name, (es or self.es).enter_context(self.nc.sbuf_tensor(name, shape, dt)))

    def ps(self, name, shape, dt, es=None):
        return Buf(name, (es or self.es).enter_context(self.nc.psum_tensor(name, shape, dt)))

    def dram(self, name, shape, dt, kind):
        return Buf(name, self.nc.dram_tensor(name, shape, dt, kind=kind).ap())

    def _deps(self, ek, reads, writes):
        deps = {}

        def need(toks):
            for s, v in toks.items():
                if ek == "pe" and s == "pe":
                    continue
                if deps.get(s, 0) < v:
                    deps[s] = v

        for b in reads:
            need(b.w)
        for b in writes:
            if not b.name.startswith("dram:"):
                need(b.w)
            need(b.r)
        eng = self.eng[ek]
        wd = self.waited[ek]
        for s, v in deps.items():
            if wd.get(s, 0) < v:
                eng.wait_ge(self.semobj[s], v)
                wd[s] = v

    def _mark(self, tok, reads, writes):
        s, v = tok
        for b in reads:
            b.r[s] = max(b.r.get(s, 0), v)
        for b in writes:
            if b.name.startswith("dram:"):
                b.w[s] = max(b.w.get(s, 0), v)
            else:
                b.w = {s: v}
            b.r = {}

    def op(self, ek, fn, reads=(), writes=()):
        self._deps(ek, reads, writes)
        ins = fn(self.eng[ek])
        self.cnt[ek] += 1
        ins.then_inc(self.semobj[ek], 1)
        self._mark((ek, self.cnt[ek]), reads, writes)
        return ins

    def dma(self, qk, fn, sbuf_side, reads=(), writes=()):
        self._deps(qk, reads, writes)
        key = "d:" + sbuf_side.name
        if key not in self.semobj:
            self.semobj[key] = self.es.enter_context(self.nc.semaphore("sd_" + sbuf_side.name))
            self.cnt[key] = 0
        ins = fn(self.eng[qk])
        self.cnt[key] += 16
        ins.then_inc(self.semobj[key], 16)
        self._mark((key, self.cnt[key]), reads, writes)
        return ins

    def barrier(self):
        for ek in self.eng:
            for s, v in self.cnt.items():
                if v > 0 and self.waited[ek].get(s, 0) < v:
                    self.eng[ek].wait_ge(self.semobj[s], v)
                    self.waited[ek][s] = v

    def finish(self, bufs):
        for b in bufs:
            for s, v in list(b.w.items()) + list(b.r.items()):
                if self.waited["sp"].get(s, 0) < v:
                    self.nc.sync.wait_ge(self.semobj[s], v)
                    self.waited["sp"][s] = v


def build_program(NT, HB, NE, CAP, NKB, debug=False, upto=9):
    assert HB >= 1 and NKB - 1 <= HB
    TT = (HB + NT) * 128
    GS = NE // 8
    NR = (CAP + 127) // 128
    RROWS = [min(128, CAP - r * 128) for r in range(NR)]
    nc = bass.Bass("TRN2", target_bir_lowering=False)
    es = ExitStack()
    P = Prog(nc, es)

    def dram_in(n, s, d):
        b = P.dram(n, s, d, "ExternalInput")
        b.name = "dram:" + n
        return b

    def dram_tmp(n, s, d, kind="Internal"):
        b = P.dram(n, s, d, kind)
        b.name = "dram:" + n
        return b

    xh = dram_in("xh", [TT, D], F32)
    posb = dram_in("posb", [128, TT], I32)
    csil = dram_in("csil", [128, 8], F32)
    w_ada = dram_in("w_ada", [D, 6 * D], F32)
    b_ada = dram_in("b_ada", [128, 6 * D], F32)
    w_in = dram_in("w_in", [D, WCOLS], F32)
    w_out = dram_in("w_out", [D, D], F32)
    gmix = dram_in("gmix", [128, D], F32)
    sinks = dram_in("sinks", [128, 8], F32)
    lnp = dram_in("lnp", [128, 4, D], F32)
    w_r = dram_in("w_r", [D, NE], F32)
    ebias = dram_in("ebias", [128, NE], F32)
    w1 = dram_in("w1", [NE, D, FF], F32)
    w3 = dram_in("w3", [NE, D, FF], F32)
    w2 = dram_in("w2", [NE, FF, D], F32)
    ws1 = dram_in("ws1", [D, FF], F32)
    ws3 = dram_in("ws3", [D, FF], F32)
    ws2 = dram_in("ws2", [FF, D], F32)
    cst = dram_in("cst", [128, 5, 128], F32)
    msk = dram_in("msk", [128, 5, 1024], F32)
    vec = dram_in("vec", [128, 4], F32)
    ecap = dram_in("ecap", [128, NE], F32)
    e4c = dram_in("e4c", [128, NE], F32)
    elimc = dram_in("elimc", [128, NE], F32)
    prowc = dram_in("prowc", [128, 1], F32)
    out = dram_tmp("out", [NT * 128, D], F32, "ExternalOutput")
    x1s = dram_tmp("x1s", [NT * 128, D], F32)
    accs = dram_tmp("accs", [NT * 128, D], F32)
    xe = dram_tmp("xe", [NE * CAP + 128, D], BF16)
    ye = dram_tmp("ye", [NE * CAP + 128, D], BF16)
    dbg = {}
    if debug:
        dbg["y_moe"] = dram_tmp("dbg_r", [NT * 128, D], F32, "ExternalOutput")

    adas = dram_tmp("adas", [128, 6 * D], F32)
    ident = P.sb("ident", [128, 128], BF16)
    tri = P.sb("tri", [128, 128], BF16)
    ones = P.sb("ones", [128, 128], BF16)
    slt = P.sb("slt", [128, 128], BF16)
    zer = P.sb("zer", [128, 128], BF16)
    dest8 = P.sb("dest8", [128, NT, 8], I32)
    gate8 = P.sb("gate8", [128, NT, 8], F32)
    st = P.sb("st", [128, 16], F32)
    epsT = P.sb("epsT", [128, 2], F32)
    junk = P.sb("junk", [128, D], F32)
    DB = [P.ps("db%d" % i, [128, 1024], F32) for i in range(4)]
    P.op("dve", lambda e: e.memset(gate8[:], 0.0), writes=[gate8])

    def ln_stats(src, width=D, st=st, junk=junk):
        P.op("dve", lambda e: e.memset(st[:, 0:2], 0.0), writes=[st])
        P.op("act", lambda e: e.activation(out=junk[:, 0:width], in_=src[:, 0:width], func=AF.Identity, scale=1.0 / width, accum_out=st[:, 0:1]),
             reads=[src], writes=[junk, st])
        P.op("act", lambda e: e.activation(out=junk[:, 0:width], in_=src[:, 0:width], func=AF.Square, scale=float(width) ** -0.5, accum_out=st[:, 1:2]),
             reads=[src], writes=[junk, st])
        P.op("dve", lambda e: e.scalar_tensor_tensor(out=st[:, 6:7], in0=st[:, 0:1], scalar=st[:, 0:1], in1=st[:, 1:2],
                                                      op0=ALU.mult, op1=ALU.subtract), reads=[st], writes=[st])
        P.op("act", lambda e: e.activation(out=st[:, 7:8], in_=st[:, 6:7], func=AF.Ln, scale=-1.0, bias=epsT[:, 0:1]), reads=[st, epsT], writes=[st])
        P.op("act", lambda e: e.activation(out=st[:, 4:5], in_=st[:, 7:8], func=AF.Exp, scale=-0.5), reads=[st], writes=[st])
        P.op("dve", lambda e: e.scalar_tensor_tensor(out=st[:, 5:6], in0=st[:, 0:1], scalar=-1.0, in1=st[:, 4:5],
                                                      op0=ALU.mult, op1=ALU.mult), reads=[st], writes=[st])

    def transpose8(src_bf, dstT, psbank_ap, psbuf, ncols=128, all_act=False):
        pv = psbank_ap.bitcast(BF16)
        for c in range(8):
            P.op("pe", lambda e, c=c: e.transpose(out=pv[:, c * 128:(c + 1) * 128], in_=src_bf[:, c * 128:(c + 1) * 128], identity=ident[:]),
                 reads=[src_bf, ident], writes=[psbuf])
        P.op("act", lambda e: e.activation(out=dstT[:, 0:4, :], in_=pv[:, 0:512].rearrange("p (c t) -> p c t", c=4), func=AF.Copy),
             reads=[psbuf], writes=[dstT])
        if all_act:
            P.op("act", lambda e: e.activation(out=dstT[:, 4:8, :], in_=pv[:, 512:1024].rearrange("p (c t) -> p c t", c=4), func=AF.Copy),
                 reads=[psbuf], writes=[dstT])
        else:
            P.op("dve", lambda e: e.tensor_copy(out=dstT[:, 4:8, :], in_=pv[:, 512:1024].rearrange("p (c t) -> p c t", c=4)),
                 reads=[psbuf], writes=[dstT])

    with ExitStack() as s0:
        cst_t = P.sb("cst_t", [128, 5, 128], F32, s0)
        P.dma("sp", lambda e: e.dma_start(out=cst_t[:], in_=cst[:]), cst_t, reads=[cst], writes=[cst_t])
        for k, dst in enumerate((ident, tri, ones, slt, zer)):
            P.op("dve", lambda e, k=k, dst=dst: e.tensor_copy(out=dst[:], in_=cst_t[:, k, :]), reads=[cst_t], writes=[dst])
        P.op("dve", lambda e: e.memset(st[:], EPS), writes=[st])
        P.op("dve", lambda e: e.memset(epsT[:], EPS), writes=[epsT])
        ada = P.sb("ada", [128, 6 * D], F32, s0)
        cs = P.sb("cs", [128, 8], F32, s0)
        P.dma("sp", lambda e: e.dma_start(out=cs[:], in_=csil[:]), cs, reads=[csil], writes=[cs])
        cs2 = P.sb("cs2", [128, 8], F32, s0)
        P.op("act", lambda e: e.activation(out=cs2[:], in_=cs[:], func=AF.Silu), reads=[cs], writes=[cs2])
        csb = P.sb("csb", [128, 8, 128], BF16, s0)
        for k in range(8):
            P.op("dve", lambda e, k=k: e.tensor_copy(out=csb[:, k, :], in_=cs2[:, k:k + 1].broadcast_to([128, 128])),
                 reads=[cs2], writes=[csb])
        P.dma("sp", lambda e: e.dma_start(out=ada[:], in_=b_ada[:]), ada, reads=[b_ada], writes=[ada])
        wa = [P.sb("wa%d" % i, [128, 8, 512], BF16, s0) for i in range(2)]
        waf = [P.sb("waf%d" % i, [128, 8, 512], F32, s0) for i in range(3)]
        wav = w_ada.t.rearrange("(c p) n -> p c n", p=128)

        def ld_wa(nb):
            wf = waf[nb % 3]
            P.dma("sp", lambda e: e.dma_start(out=wf[:], in_=wav[:, :, nb * 512:(nb + 1) * 512]), wf, reads=[w_ada], writes=[wf])

        ld_wa(0)
        ld_wa(1)
        for nb in range(12):
            wb = wa[nb % 2]
            wf = waf[nb % 3]
            if nb + 2 < 12:
                ld_wa(nb + 2)
            P.op("act", lambda e, wb=wb, wf=wf: e.activation(out=wb[:, 0:4, :], in_=wf[:, 0:4, :], func=AF.Copy), reads=[wf], writes=[wb])
            P.op("dve", lambda e, wb=wb, wf=wf: e.tensor_copy(out=wb[:, 4:8, :], in_=wf[:, 4:8, :]), reads=[wf], writes=[wb])
            pb = DB[nb % 2]
            for k in range(8):
                P.op("pe", lambda e, k=k, wb=wb, pb=pb: e.matmul(pb[:, 0:512], lhsT=csb[:, k, :], rhs=wb[:, k, :], start=(k == 0), stop=(k == 7)),
                     reads=[csb, wb], writes=[pb])
            P.op("dve", lambda e, nb=nb, pb=pb: e.tensor_tensor(out=ada[:, nb * 512:(nb + 1) * 512], in0=pb[:, 0:512],
                                                                 in1=ada[:, nb * 512:(nb + 1) * 512], op=ALU.add), reads=[pb, ada], writes=[ada])
        for seg in (1, 2, 4, 5):
            P.op("dve", lambda e, seg=seg: e.tensor_scalar(out=ada[:, seg * D:(seg + 1) * D], in0=ada[:, seg * D:(seg + 1) * D],
                                                           scalar1=1.0, scalar2=None, op0=ALU.add), reads=[ada], writes=[ada])
        P.dma("sp", lambda e: e.dma_start(out=adas[:], in_=ada[:]), ada, reads=[ada], writes=[adas])
        P.barrier()
    SH1, SC1, G1, SH2, SC2, G2 = [slice(i * D, (i + 1) * D) for i in range(6)]
    if upto == 0:
        es.close()
        return nc

    def load_cast(s, name, src_view, shape, reads):
        t = P.sb(name, shape, BF16, s)
        P.dma("pool", lambda e: e.dma_start(out=t[:], in_=src_view), t, reads=reads, writes=[t])
        return t

    with ExitStack() as s1:
        win = load_cast(s1, "win", w_in.t.rearrange("(c p) n -> p c n", p=128), [128, 8, WCOLS], [w_in])
        wout = load_cast(s1, "wout", w_out.t.rearrange("(c p) n -> p c n", p=128), [128, 8, D], [w_out])
        ada = P.sb("ada1", [128, 2 * D], F32, s1)
        P.dma("sp", lambda e: e.dma_start(out=ada[:], in_=adas[:, 0:2 * D]), ada, reads=[adas], writes=[ada])
        lnp_t = P.sb("lnp1", [128, 2, D], F32, s1)
        P.dma("sp", lambda e: e.dma_start(out=lnp_t[:], in_=lnp[:, 0:2, :]), lnp_t, reads=[lnp], writes=[lnp_t])
        gmix_t = P.sb("gmix_t", [128, D], F32, s1)
        P.dma("sp", lambda e: e.dma_start(out=gmix_t[:], in_=gmix[:]), gmix_t, reads=[gmix], writes=[gmix_t])
        msk_t = P.sb("msk_t", [128, 5, 128], BF16, s1)
        P.dma("pool", lambda e: e.dma_start(out=msk_t[:], in_=msk[:, :, 0:128]), msk_t, reads=[msk], writes=[msk_t])
        vec_t = P.sb("vec_t", [128, 4], F32, s1)
        P.dma("sp", lambda e: e.dma_start(out=vec_t[:], in_=vec[:]), vec_t, reads=[vec], writes=[vec_t])
        esink = P.sb("esink", [128, 8], F32, s1)
        P.dma("sp", lambda e: e.dma_start(out=esink[:], in_=sinks[:]), esink, reads=[sinks], writes=[esink])
        P.op("act", lambda e: e.activation(out=esink[:], in_=esink[:], func=AF.Exp), reads=[esink], writes=[esink])
        Ct = P.sb("Ct", [128, TT], F32, s1)
        St = P.sb("St", [128, TT], F32, s1)
        with ExitStack() as sr:
            g1p = P.sb("g1p", [128, D], F32, sr)
            P.dma("sp", lambda e: e.dma_start(out=g1p[:], in_=adas[:, 2 * D:3 * D]), g1p, reads=[adas], writes=[g1p])
            for k in range(8):
                P.op("dve", lambda e, k=k: e.tensor_tensor(out=wout[:, k, :], in0=wout[:, k, :], in1=g1p[:], op=ALU.mult), reads=[wout, g1p], writes=[wout])
            posi = P.sb("posi", [128, 512], I32, sr)
            ang = P.sb("ang", [128, 512], F32, sr)
            t1 = P.sb("t1", [128, 512], F32, sr)
            ki = P.sb("ki", [128, 512], I32, sr)
            kf = P.sb("kf", [128, 512], F32, sr)
            for c0 in range(0, TT, 512):
                w = min(512, TT - c0)
                P.dma("sp", lambda e, c0=c0, w=w: e.dma_start(out=posi[:, 0:w], in_=posb[:, c0:c0 + w]), posi, reads=[posb], writes=[posi])
                P.op("dve", lambda e, w=w: e.tensor_copy(out=ang[:, 0:w], in_=posi[:, 0:w]), reads=[posi], writes=[ang])
                P.op("dve", lambda e, w=w: e.tensor_scalar(out=ang[:, 0:w], in0=ang[:, 0:w], scalar1=vec_t[:, 0:1], scalar2=None, op0=ALU.mult),
                     reads=[ang, vec_t], writes=[ang])
                for which, dst in ((0, St), (1, Ct)):
                    if which == 1:
                        P.op("dve", lambda e, w=w: e.tensor_scalar(out=ang[:, 0:w], in0=ang[:, 0:w], scalar1=float(np.pi / 2), scalar2=None, op0=ALU.add),
                             reads=[ang], writes=[ang])
                    P.op("dve", lambda e, w=w: e.tensor_scalar(out=t1[:, 0:w], in0=ang[:, 0:w], scalar1=float(1.0 / TWO_PI), scalar2=None, op0=ALU.mult),
                         reads=[ang], writes=[t1])
                    P.op("dve", lambda e, w=w: e.tensor_copy(out=ki[:, 0:w], in_=t1[:, 0:w]), reads=[t1], writes=[ki])
                    P.op("dve", lambda e, w=w: e.tensor_copy(out=kf[:, 0:w], in_=ki[:, 0:w]), reads=[ki], writes=[kf])
                    P.op("dve", lambda e, w=w: e.scalar_tensor_tensor(out=t1[:, 0:w], in0=kf[:, 0:w], scalar=-C1, in1=ang[:, 0:w], op0=ALU.mult, op1=ALU.add),
                         reads=[kf, ang], writes=[t1])
                    P.op("dve", lambda e, w=w: e.scalar_tensor_tensor(out=t1[:, 0:w], in0=kf[:, 0:w], scalar=-C2, in1=t1[:, 0:w], op0=ALU.mult, op1=ALU.add),
                         reads=[kf, t1], writes=[t1])
                    P.op("dve", lambda e, w=w: e.tensor_scalar(out=t1[:, 0:w], in0=t1[:, 0:w], scalar1=float(np.pi), scalar2=float(-np.pi), op0=ALU.min, op1=ALU.max),
                         reads=[t1], writes=[t1])
                    P.op("act", lambda e, w=w, dst=dst, c0=c0: e.activation(out=dst[:, c0:c0 + w], in_=t1[:, 0:w], func=AF.Sin), reads=[t1], writes=[dst])
                P.op("dve", lambda e, w=w, c0=c0: e.tensor_scalar(out=St[:, c0:c0 + w], in0=St[:, c0:c0 + w], scalar1=vec_t[:, 1:2], scalar2=None, op0=ALU.mult),
                     reads=[St, vec_t], writes=[St])
            P.barrier()

        import os
        P1STOP = os.environ.get("P1STOP", "")
        blk = lambda h: (h % 2) * 4 + h // 2
        H = [Buf("h%d" % i_, DB[i_ // 2].t[:, (i_ % 2) * 512:(i_ % 2 + 1) * 512]) for i_ in range(8)]
        RK = NKB + 1
        xt = [P.sb("xt%d" % i, [128, D], F32, s1) for i in range(2)]
        xn = P.sb("xn", [128, D], F32, s1)
        ub = P.sb("ub", [128, D], BF16, s1)
        uT = P.sb("uT", [128, 8, 128], BF16, s1)
        stA = P.sb("stA", [128, 16], F32, s1)
        jkA = P.sb("jkA", [128, D], BF16, s1)
        qaT = [P.sb("qaT%d" % i, [128, 4, 128], BF16, s1) for i in range(2)]
        qsT = [P.sb("qsT%d" % i, [128, 4, 128], BF16, s1) for i in range(2)]
        kaT = [P.sb("kaT%d" % i, [128, 2, 128], BF16, s1) for i in range(3)]
        vaA = [P.sb("vaA%d" % i, [128, 2, 128], BF16, s1) for i in range(3)]
        ksT = [P.sb("ksT%d" % i, [128, 4, 128], BF16, s1) for i in range(RK)]
        vsB = [P.sb("vsB%d" % i, [128, 512], BF16, s1) for i in range(RK)]
        spB = [P.sb("spB%d" % i, [128, 1024], BF16, s1) for i in range(NKB)]
        tmpa = P.sb("tmpa", [128, 512], F32, s1)
        tmpb = P.sb("tmpb", [128, 512], F32, s1)
        pb16 = P.sb("pb16", [128, 1024], BF16, s1)
        pm16 = P.sb("pm16", [128, 1024], BF16, s1)
        efs = [P.sb("ef%d" % i, [128, 1024], BF16, s1) for i in range(NKB)]
        jb = junk.t[:, :].bitcast(BF16)
        ecs = [Buf("ec%d" % i, jb[:, i * 1024:(i + 1) * 1024]) for i in range(2)]
        wbs = [P.sb("wb%d" % i, [128, 1024], BF16, s1) for i in range(2)]
        mixf = P.sb("mixf", [128, D], F32, s1)
        mixbs = [P.sb("mixb%d" % i, [128, D], BF16, s1) for i in range(2)]
        jkB = P.sb("jkB", [128, D], BF16, s1)
        mixT = P.sb("mixT", [128, 8, 128], BF16, s1)
        den = P.sb("den", [128, 8], F32, s1)
        ss = P.sb("ss", [128, 4], F32, s1)
        pres = [P.sb("pre%d" % i, [128, D], F32, s1) for i in range(2)]
        P.op("dve", lambda e: e.memset(stA[:], EPS), writes=[stA])
        for v in vaA:
            P.op("dve", lambda e, v=v: e.memset(v[:], 1.0), writes=[v])
        xh_v = xh.t.rearrange("(j p) d -> j p d", p=128)
        x1s_v = x1s.t.rearrange("(j p) d -> j p d", p=128)
        r3 = lambda ap, n: ap.rearrange("p (c t) -> p c t", c=n)
        m8v = lambda mi: msk_t[:, mi, :].unsqueeze(1).broadcast_to([128, 8, 128])

        def fm(col0, nch, bank):
            for ch in range(nch):
                for k in range(8):
                    P.op("pe", lambda e, ch=ch, k=k: e.matmul(bank[:, ch * 128:(ch + 1) * 128], lhsT=win[:, k, col0 + ch * 128:col0 + (ch + 1) * 128],
                                                               rhs=uT[:, k, :], start=(k == 0), stop=(k == 7)), reads=[win, uT], writes=[bank])

        def stageA(j):
            own = j >= HB
            x = xt[j % 2]
            if j + 1 < HB + NT:
                xn_ = xt[(j + 1) % 2]
                P.dma("sp", lambda e: e.dma_start(out=xn_[:], in_=xh_v[j + 1]), xn_, reads=[xh], writes=[xn_])
            ln_stats(x, st=stA, junk=jkA)
            yield
            P.op("act", lambda e: e.activation(out=xn[:], in_=x[:], func=AF.Identity, scale=stA[:, 4:5], bias=stA[:, 5:6]), reads=[x, stA], writes=[xn])
            yield
            P.op("dve", lambda e: e.tensor_tensor(out=xn[:], in0=xn[:], in1=ada[:, SC1], op=ALU.mult), reads=[xn, ada], writes=[xn])
            P.op("dve", lambda e: e.tensor_tensor(out=ub[:], in0=xn[:], in1=ada[:, SH1], op=ALU.add), reads=[xn, ada], writes=[ub])
            yield
            transpose8(ub, uT, H[0].t, H[0])
            yield
            ks_slot, vs_slot = ksT[j % RK], vsB[j % RK]
            ka_slot, va_slot = kaT[j % 3], vaA[j % 3]
            tok = slice(j * 128, (j + 1) * 128)
            Cb = lambda n: Ct[:, tok].unsqueeze(1).broadcast_to([128, n, 128])
            Sb = lambda n: St[:, tok].unsqueeze(1).broadcast_to([128, n, 128])
            if own:
                fm(O_QA, 4, H[1])
                yield
                fm(O_QASW, 4, H[0])
                yield
                qa_ = qaT[j % 2]
                P.op("dve", lambda e: e.tensor_tensor(out=r3(tmpa[:], 4), in0=r3(H[1][:, :], 4), in1=Cb(4), op=ALU.mult), reads=[H[1], Ct], writes=[tmpa])
                P.op("dve", lambda e: e.tensor_tensor(out=r3(tmpb[:], 4), in0=r3(H[0][:, :], 4), in1=Sb(4), op=ALU.mult), reads=[H[0], St], writes=[tmpb])
                yield
                P.op("dve", lambda e: e.tensor_tensor(out=qa_[:], in0=r3(tmpa[:], 4), in1=r3(tmpb[:], 4), op=ALU.add), reads=[tmpa, tmpb], writes=[qa_])
            fm(O_KA, 4, H[1])
            yield
            if own:
                fm(O_QS, 4, H[0])
                yield
            P.op("dve", lambda e: e.tensor_tensor(out=r3(tmpa[:, 0:256], 2), in0=r3(H[1][:, 0:256], 2), in1=Cb(2), op=ALU.mult), reads=[H[1], Ct], writes=[tmpa])
            P.op("dve", lambda e: e.tensor_tensor(out=r3(tmpb[:, 0:256], 2), in0=r3(H[1][:, 256:512], 2), in1=Sb(2), op=ALU.mult), reads=[H[1], St], writes=[tmpb])
            yield
            P.op("dve", lambda e: e.tensor_tensor(out=ka_slot[:], in0=r3(tmpa[:, 0:256], 2), in1=r3(tmpb[:, 0:256], 2), op=ALU.add), reads=[tmpa, tmpb], writes=[ka_slot])
            if own:
                qs_ = qsT[j % 2]
                P.op("act", lambda e: e.activation(out=qs_[:], in_=r3(H[0][:, :], 4), func=AF.Copy), reads=[H[0]], writes=[qs_])
            yield
            fm(O_KS, 4, H[1])
            yield
            for k in range(8):
                P.op("pe", lambda e, k=k: e.matmul(H[0][:, :], lhsT=uT[:, k, :], rhs=win[:, k, O_VS:O_VS + 512], start=(k == 0), stop=(k == 7)),
                     reads=[win, uT], writes=[H[0]])
            yield
            P.op("dve", lambda e: e.tensor_copy(out=ks_slot[:], in_=r3(H[1][:, :], 4)), reads=[H[1]], writes=[ks_slot])
            yield
            P.op("dve", lambda e: e.tensor_copy(out=vs_slot[:], in_=H[0][:, :]), reads=[H[0]], writes=[vs_slot])
            for k in range(8):
                P.op("pe", lambda e, k=k: e.matmul(H[1][:, 0:128], lhsT=uT[:, k, :], rhs=win[:, k, O_VA:O_VA + 128], start=(k == 0), stop=(k == 7)),
                     reads=[win, uT], writes=[H[1]])
            yield
            P.op("dve", lambda e: e.tensor_copy(out=va_slot[:, :, 0:64], in_=H[1][:, 0:128].rearrange("p (k d) -> p k d", k=2)), reads=[H[1]], writes=[va_slot])

        def stageB1(j):
            i = j - HB
            qa_, qs_ = qaT[j % 2], qsT[j % 2]
            mixb_ = mixbs[j % 2]
            pre_ = pres[j % 2]
            P.dma("sp", lambda e: e.dma_start(out=pre_[:], in_=xh_v[j]), pre_, reads=[xh], writes=[pre_])
            for a in range(2):
                P.op("pe", lambda e, a=a: e.matmul(H[6 + a][:, :], lhsT=zer[:], rhs=win[:, 0, 0:512], start=True, stop=False), reads=[zer, win], writes=[H[6 + a]])
            for kt in range(2):
                kslot = kaT[(j - 1 + kt) % 3]
                vslot = vaA[(j - 1 + kt) % 3]
                for h in range(8):
                    hp = (h % 2) * 64
                    bk = H[4 + h % 2]
                    P.op("pe", lambda e, h=h, hp=hp, bk=bk: e.matmul(bk[:, (h // 2) * 128:(h // 2 + 1) * 128], lhsT=kslot[hp:hp + 64, h // 4, :],
                                                                     rhs=qa_[hp:hp + 64, h // 2, :], start=True, stop=True), reads=[kslot, qa_], writes=[bk])
                yield
                for hf in range(2):
                    P.op("act", lambda e, hf=hf: e.activation(out=pb16[:, hf * 512:(hf + 1) * 512], in_=H[4 + hf][:, :], func=AF.Exp, scale=0.125), reads=[H[4 + hf]], writes=[pb16])
                yield
                mi = 1 if kt == 1 else (3 if i == 0 else 0)
                P.op("dve", lambda e, mi=mi: e.tensor_tensor(out=r3(pm16[:], 8), in0=r3(pb16[:], 8), in1=m8v(mi), op=ALU.mult), reads=[pb16, msk_t], writes=[pm16])
                yield
                for h in range(8):
                    bk = H[6 + h // 4]
                    P.op("pe", lambda e, h=h, bk=bk: e.matmul(bk[:, (h % 4) * 128:(h % 4 + 1) * 128],
                                                              lhsT=pm16[:, blk(h) * 128:(blk(h) + 1) * 128], rhs=vslot[:, h // 4, :], start=False, stop=(kt == 1 and h % 4 == 3)),
                         reads=[pm16, vslot], writes=[bk])
            yield
            oav = lambda a: H[6 + a][:, :].rearrange("p (h d) -> p h d", h=4)
            for a in range(2):
                P.op("dve", lambda e, a=a: e.tensor_tensor(out=den[:, a * 4:(a + 1) * 4], in0=oav(a)[:, :, 64], in1=esink[:, a * 4:(a + 1) * 4], op=ALU.add),
                     reads=[H[6 + a], esink], writes=[den])
            P.op("dve", lambda e: e.reciprocal(out=den[:], in_=den[:]), reads=[den], writes=[den])
            yield
            for a in range(2):
                P.op("dve", lambda e, a=a: e.tensor_tensor(out=mixf[:, a * 256:(a + 1) * 256].rearrange("p (h d) -> p h d", h=4), in0=oav(a)[:, :, 0:64],
                                                           in1=den[:, a * 4:(a + 1) * 4].unsqueeze(2).broadcast_to([128, 4, 64]), op=ALU.mult),
                     reads=[H[6 + a], den], writes=[mixf])
            P.op("pe", lambda e: e.matmul(H[3][:, :], lhsT=zer[:], rhs=win[:, 0, 0:512], start=True, stop=False), reads=[zer, win], writes=[H[3]])

            def zmm(kk):
                ksl = ksT[(j - kk) % RK]
                for h in range(8):
                    hp = (h % 2) * 64
                    bk = H[4 + h % 2]
                    P.op("pe", lambda e, h=h, hp=hp, bk=bk: e.matmul(bk[:, (h // 2) * 128:(h // 2 + 1) * 128], lhsT=ksl[hp:hp + 64, h // 2, :],
                                                                     rhs=qs_[hp:hp + 64, h // 2, :], start=True, stop=True), reads=[ksl, qs_], writes=[bk])

            def eexp(kk):
                ef_ = efs[kk]
                for hf in range(2):
                    P.op("act", lambda e, hf=hf: e.activation(out=ef_[:, hf * 512:(hf + 1) * 512], in_=H[4 + hf][:, :], func=AF.Exp, scale=0.125), reads=[H[4 + hf]], writes=[ef_])
                mi = 2 if kk == 0 else (4 if (j - kk) < HB else None)
                if mi is not None:
                    P.op("dve", lambda e: e.tensor_tensor(out=r3(ef_[:], 8), in0=r3(ef_[:], 8), in1=m8v(mi), op=ALU.mult), reads=[ef_, msk_t], writes=[ef_])

            def splog(kk):
                P.op("act", lambda e: e.activation(out=spB[kk][:], in_=efs[kk][:], func=AF.Ln, bias=1.0), reads=[efs[kk]], writes=[spB[kk]])

            def cmm(kk):
                for half in range(2):
                    hs = slice(half * 512, (half + 1) * 512)
                    P.op("pe", lambda e, hs=hs, half=half: e.matmul(H[6 + half][:, :], lhsT=tri[:], rhs=spB[kk][:, hs], start=True, stop=(kk == 0)),
                         reads=[tri, spB[kk]], writes=[H[6 + half]])
                    for k2 in range(kk):
                        P.op("pe", lambda e, hs=hs, k2=k2, half=half: e.matmul(H[6 + half][:, :], lhsT=ones[:], rhs=spB[k2][:, hs], start=False, stop=(k2 == kk - 1)),
                             reads=[ones, spB[k2]], writes=[H[6 + half]])

            def ecexp(kk):
                ec_ = ecs[kk % 2]
                for hf in range(2):
                    P.op("act", lambda e, hf=hf: e.activation(out=ec_[:, hf * 512:(hf + 1) * 512], in_=H[6 + hf][:, :], func=AF.Exp, scale=-1.0), reads=[H[6 + hf]], writes=[ec_])

            def wmul(kk):
                P.op("dve", lambda e: e.tensor_tensor(out=wbs[kk % 2][:], in0=efs[kk][:], in1=ecs[kk % 2][:], op=ALU.mult), reads=[efs[kk], ecs[kk % 2]], writes=[wbs[kk % 2]])

            def pv(kk):
                vsl = vsB[(j - kk) % RK]
                wb_ = wbs[kk % 2]
                for h in range(8):
                    P.op("pe", lambda e, h=h: e.matmul(H[3][:, h * 64:(h + 1) * 64], lhsT=wb_[:, blk(h) * 128:(blk(h) + 1) * 128],
                                                       rhs=vsl[:, h * 64:(h + 1) * 64], start=False, stop=(kk == NKB - 1 and h == 7)),
                         reads=[wb_, vsl], writes=[H[3]])

            yield
            zmm(0)
            yield
            eexp(0)
            yield
            for kk in range(NKB):
                if kk + 1 < NKB:
                    zmm(kk + 1)
                splog(kk)
                yield
                if kk + 1 < NKB:
                    eexp(kk + 1)
                cmm(kk)
                yield
                ecexp(kk)
                if kk >= 1:
                    pv(kk - 1)
                yield
                wmul(kk)
                yield
            pv(NKB - 1)
            yield
            P.op("act", lambda e: e.activation(out=mixf[:, 512:1024], in_=H[3][:, :], func=AF.Copy), reads=[H[3]], writes=[mixf])
            yield
            P.op("dve", lambda e: e.memset(ss[:], 0.0), writes=[ss])
            for a in range(2):
                P.op("act", lambda e, a=a: e.activation(out=pb16[:, 0:512], in_=mixf[:, a * 512:(a + 1) * 512], func=AF.Square, accum_out=ss[:, a:a + 1]),
                     reads=[mixf], writes=[pb16, ss])
            yield
            P.op("act", lambda e: e.activation(out=ss[:, 2:4], in_=ss[:, 0:2], func=AF.Ln, scale=1.0 / 512, bias=epsT[:, 0:1]), reads=[ss, epsT], writes=[ss])
            P.op("act", lambda e: e.activation(out=ss[:, 0:2], in_=ss[:, 2:4], func=AF.Exp, scale=-0.5), reads=[ss], writes=[ss])
            yield
            for a in range(2):
                hs = slice(a * 512, (a + 1) * 512)
                P.op("dve", lambda e, a=a, hs=hs: e.scalar_tensor_tensor(out=mixb_[:, hs], in0=mixf[:, hs], scalar=ss[:, a:a + 1], in1=gmix_t[:, hs],
                                                                          op0=ALU.mult, op1=ALU.mult), reads=[mixf, ss, gmix_t], writes=[mixb_])

        def stageB2(j):
            i = j - HB
            mixb_ = mixbs[j % 2]
            pre = pres[j % 2]
            transpose8(mixb_, mixT, H[2].t, H[2])
            yield
            for half in range(2):
                hs = slice(half * 512, (half + 1) * 512)
                for k in range(8):
                    P.op("pe", lambda e, k=k, hs=hs: e.matmul(H[2][:, :], lhsT=mixT[:, k, :], rhs=wout[:, k, hs], start=(k == 0), stop=(k == 7)),
                         reads=[mixT, wout], writes=[H[2]])
                yield
                P.op("dve", lambda e, hs=hs: e.scalar_tensor_tensor(out=pre[:, hs], in0=pre[:, hs], scalar=ALPHA, in1=H[2][:, :], op0=ALU.mult, op1=ALU.add),
                     reads=[H[2], pre], writes=[pre])
                yield
            ln_stats(pre, junk=jkB)
            yield
            P.op("act", lambda e: e.activation(out=pre[:], in_=pre[:], func=AF.Identity, scale=st[:, 4:5], bias=st[:, 5:6]), reads=[pre, st], writes=[pre])
            yield
            P.op("dve", lambda e: e.tensor_tensor(out=pre[:], in0=pre[:], in1=lnp_t[:, 0, :], op=ALU.mult), reads=[pre, lnp_t], writes=[pre])
            P.op("dve", lambda e: e.tensor_tensor(out=pre[:], in0=pre[:], in1=lnp_t[:, 1, :], op=ALU.add), reads=[pre, lnp_t], writes=[pre])
            yield
            P.dma("sp", lambda e: e.dma_start(out=x1s_v[i], in_=pre[:]), pre, reads=[pre], writes=[x1s])

        P.dma("sp", lambda e: e.dma_start(out=xt[0][:], in_=xh_v[0]), xt[0], reads=[xh], writes=[xt[0]])
        LAST = HB + NT - 1
        for j in range(HB + NT + 2):
            gens = []
            if HB <= j - 1 <= LAST:
                gens.append(stageB1(j - 1))
            if j <= LAST:
                gens.append(stageA(j))
            if HB <= j - 2 <= LAST:
                gens.append(stageB2(j - 2))
            while gens:
                for g in list(gens):
                    try:
                        next(g)
                    except StopIteration:
                        gens.remove(g)
        P.barrier()
    if upto == 1:
        es.close()
        return nc
    accs_v = accs.t.rearrange("(j p) d -> j p d", p=128)
    x1s_v = x1s.t.rearrange("(j p) d -> j p d", p=128)
    with ExitStack() as s2:
        wr = load_cast(s2, "wr", w_r.t.rearrange("(c p) n -> p c n", p=128), [128, 8, NE], [w_r])
        s1w = load_cast(s2, "s1w", ws1.t.rearrange("(c p) n -> p c n", p=128), [128, 8, FF], [ws1])
        s3w = load_cast(s2, "s3w", ws3.t.rearrange("(c p) n -> p c n", p=128), [128, 8, FF], [ws3])
        s2w = load_cast(s2, "s2w", ws2.t.rearrange("(c p) n -> p c n", p=128), [128, 2, D], [ws2])
        ada = P.sb("ada2", [128, 3 * D], F32, s2)
        P.dma("sp", lambda e: e.dma_start(out=ada[:], in_=adas[:, 3 * D:6 * D]), ada, reads=[adas], writes=[ada])
        SH2, SC2, G2 = SH1, SC1, G1
        eb = P.sb("eb", [128, NE], F32, s2)
        P.dma("sp", lambda e: e.dma_start(out=eb[:], in_=ebias[:]), eb, reads=[ebias], writes=[eb])
        basecap = P.sb("basecap", [128, NE], F32, s2)
        P.dma("sp", lambda e: e.dma_start(out=basecap[:], in_=ecap[:]), basecap, reads=[ecap], writes=[basecap])
        xt = [P.sb("x1l%d" % i, [128, D], F32, s2) for i in range(2)]
        xn = P.sb("xn2", [128, D], F32, s2)
        u2b = [P.sb("u2b%d" % i, [128, D], BF16, s2) for i in range(4)]
        u2T = P.sb("u2T", [128, 8, 128], BF16, s2)
        sil = P.sb("sil", [128, 256], F32, s2)
        aT = P.sb("aT", [128, 2, 128], BF16, s2)
        acc_t = P.sb("acc_t", [128, D], F32, s2)
        bi = P.sb("bi", [128, NE], F32, s2)
        m8 = P.sb("m8", [128, 8, 8], F32, s2)
        gs = P.sb("gs", [128, 8], F32, s2)
        g8 = P.sb("g8", [128, 8], F32, s2)
        gm = P.sb("gm", [128, 8], F32, s2)
        gn = P.sb("gn", [128, 8], F32, s2)
        mk = P.sb("mk", [128, NE], F32, s2)
        t8 = P.sb("t8", [128, 8], F32, s2)
        sel = P.sb("sel", [128, NE], F32, s2)
        selb = P.sb("selb", [128, NE], BF16, s2)
        gd = P.sb("gd", [128, NE], F32, s2)
        val = P.sb("val", [128, NE], F32, s2)
        d8 = P.sb("d8", [128, 8], F32, s2)
        rd = P.sb("rd", [128, 2], F32, s2)
        jk = P.sb("jk", [128, NE], F32, s2)
        k8 = P.sb("k8", [128, 8], F32, s2)
        okm = P.sb("okm", [128, NE], F32, s2)
        prow = P.sb("prow", [128, 1], F32, s2)
        P.dma("sp", lambda e: e.dma_start(out=prow[:], in_=prowc[:]), prow, reads=[prowc], writes=[prow])
        elim = P.sb("elim", [128, NE], F32, s2)
        P.dma("sp", lambda e: e.dma_start(out=elim[:], in_=elimc[:]), elim, reads=[elimc], writes=[elim])
        k8i = P.sb("k8i", [128, 8], I32, s2)
        k8f = P.sb("k8f", [128, 8], F32, s2)
        e4 = P.sb("e4", [128, NE], F32, s2)
        P.dma("sp", lambda e: e.dma_start(out=e4[:], in_=e4c[:]), e4, reads=[e4c], writes=[e4])
        P.dma("sp", lambda e: e.dma_start(out=xt[0][:], in_=x1s_v[0]), xt[0], reads=[x1s], writes=[xt[0]])
        scs = [P.sb("sc%d" % i_, [128, NE], F32, s2) for i_ in range(2)]
        dest8_b = [Buf("dest8_%d" % i_, dest8.t[:, i_, :]) for i_ in range(NT)]
        gate8_b = [Buf("gate8_%d" % i_, gate8.t[:, i_, :]) for i_ in range(NT)]

        def stage1(i):
            sc = scs[i % 2]
            x = xt[i % 2]
            ub = u2b[i % 4]
            if i + 1 < NT:
                xn_ = xt[(i + 1) % 2]
                P.dma("sp", lambda e, i=i, xn_=xn_: e.dma_start(out=xn_[:], in_=x1s_v[i + 1]), xn_, reads=[x1s], writes=[xn_])
            ln_stats(x)
            P.op("act", lambda e: e.activation(out=xn[:], in_=x[:], func=AF.Identity, scale=st[:, 4:5], bias=st[:, 5:6]), reads=[x, st], writes=[xn])
            P.op("dve", lambda e: e.tensor_tensor(out=xn[:], in0=xn[:], in1=ada[:, SC2], op=ALU.mult), reads=[xn, ada], writes=[xn])
            P.op("dve", lambda e: e.tensor_tensor(out=ub[:], in0=xn[:], in1=ada[:, SH2], op=ALU.add), reads=[xn, ada], writes=[ub])
            yield
            transpose8(ub, u2T, DB[0][:, 0:512], DB[0], all_act=True)
            for k in range(8):
                P.op("pe", lambda e, k=k: e.matmul(DB[0][:, 512:512 + NE], lhsT=u2T[:, k, :], rhs=wr[:, k, :], start=(k == 0), stop=(k == 7)),
                     reads=[u2T, wr], writes=[DB[0]])
            for wi, wsx in enumerate((s1w, s3w)):
                for fc in range(2):
                    o = (wi * 2 + fc) * 128
                    for k in range(8):
                        P.op("pe", lambda e, k=k, wsx=wsx, fc=fc, o=o: e.matmul(DB[1][:, o:o + 128], lhsT=wsx[:, k, fc * 128:(fc + 1) * 128], rhs=u2T[:, k, :],
                                                                                start=(k == 0), stop=(k == 7)), reads=[wsx, u2T], writes=[DB[1]])
            yield
            P.op("act", lambda e: e.activation(out=sil[:], in_=DB[1][:, 0:256], func=AF.Silu), reads=[DB[1]], writes=[sil])
            P.op("dve", lambda e: e.tensor_tensor(out=aT[:].rearrange("p c t -> p (c t)"), in0=sil[:], in1=DB[1][:, 256:512], op=ALU.mult), reads=[sil, DB[1]], writes=[aT])
            for half in range(2):
                hs = slice(half * 512, (half + 1) * 512)
                for fc in range(2):
                    P.op("pe", lambda e, fc=fc, hs=hs: e.matmul(DB[2][:, hs], lhsT=aT[:, fc, :], rhs=s2w[:, fc, hs], start=(fc == 0), stop=(fc == 1)),
                         reads=[aT, s2w], writes=[DB[2]])
            for hf in range(2):
                P.op("dve", lambda e, hf=hf: e.tensor_tensor(out=acc_t[:, hf * 512:(hf + 1) * 512], in0=DB[2][:, hf * 512:(hf + 1) * 512], in1=ada[:, 2 * D + hf * 512:2 * D + (hf + 1) * 512], op=ALU.mult), reads=[DB[2], ada], writes=[acc_t])
            P.op("dve", lambda e: e.scalar_tensor_tensor(out=acc_t[:], in0=x[:], scalar=ALPHA, in1=acc_t[:], op0=ALU.mult, op1=ALU.add), reads=[x, acc_t], writes=[acc_t])
            P.dma("sp", lambda e, i=i: e.dma_start(out=accs_v[i], in_=acc_t[:]), acc_t, reads=[acc_t], writes=[accs])
            P.op("act", lambda e: e.activation(out=sc[:], in_=DB[0][:, 512:512 + NE], func=AF.Sigmoid), reads=[DB[0]], writes=[sc])

        def stage2(i):
            sc = scs[i % 2]
            ub = u2b[i % 4]
            P.op("dve", lambda e: e.tensor_tensor(out=bi[:], in0=sc[:], in1=eb[:], op=ALU.add), reads=[sc, eb], writes=[bi])
            for g in range(8):
                P.op("dve", lambda e, g=g: e.max(out=m8[:, g, :], in_=bi[:, g * GS:(g + 1) * GS]), reads=[bi], writes=[m8])
            P.op("dve", lambda e: e.tensor_tensor(out=gs[:], in0=m8[:, :, 0], in1=m8[:, :, 1], op=ALU.add), reads=[m8], writes=[gs])
            P.op("dve", lambda e: e.max(out=g8[:], in_=gs[:]), reads=[gs], writes=[g8])
            P.op("dve", lambda e: e.tensor_scalar(out=gm[:], in0=gs[:], scalar1=g8[:, 3:4], scalar2=None, op0=ALU.is_ge), reads=[gs, g8], writes=[gm])
            P.op("dve", lambda e: e.tensor_scalar(out=gn[:], in0=gm[:], scalar1=-1.0, scalar2=BIG, op0=ALU.add, op1=ALU.mult), reads=[gm], writes=[gn])
            g3 = lambda b: b[:].rearrange("p (g s) -> p g s", g=8)
            gb3 = lambda b: b[:].unsqueeze(2).broadcast_to([128, 8, GS])
            P.op("dve", lambda e: e.tensor_tensor(out=g3(mk), in0=g3(bi), in1=gb3(gm), op=ALU.mult), reads=[bi, gm], writes=[mk])
            P.op("dve", lambda e: e.tensor_tensor(out=g3(mk), in0=g3(mk), in1=gb3(gn), op=ALU.add), reads=[mk, gn], writes=[mk])
            P.op("dve", lambda e: e.max(out=t8[:], in_=mk[:]), reads=[mk], writes=[t8])
            P.op("dve", lambda e: e.tensor_scalar(out=sel[:], in0=mk[:], scalar1=t8[:, 7:8], scalar2=None, op0=ALU.is_ge), reads=[mk, t8], writes=[sel])
            P.op("dve", lambda e: e.tensor_copy(out=selb[:], in_=sel[:]), reads=[sel], writes=[selb])
            P.op("dve", lambda e: e.memset(rd[:], 0.0), writes=[rd])
            P.op("dve", lambda e: e.scalar_tensor_tensor(out=gd[:], in0=sel[:], scalar=1.0, in1=sc[:], op0=ALU.mult, op1=ALU.mult, accum_out=rd[:, 0:1]),
                 reads=[sel, sc], writes=[gd, rd])
            P.op("dve", lambda e: e.reciprocal(out=rd[:, 1:2], in_=rd[:, 0:1]), reads=[rd], writes=[rd])
            P.op("dve", lambda e: e.tensor_scalar(out=gd[:], in0=gd[:], scalar1=rd[:, 1:2], scalar2=2.5, op0=ALU.mult, op1=ALU.mult), reads=[gd, rd], writes=[gd])
            yield
            P.op("pe", lambda e: e.matmul(DB[3][:, 0:NE], lhsT=slt[:], rhs=selb[:], start=True, stop=True), reads=[slt, selb], writes=[DB[3]])
            P.op("pe", lambda e: e.matmul(DB[3][:, 512:512 + NE], lhsT=ones[:], rhs=selb[:], start=True, stop=True), reads=[ones, selb], writes=[DB[3]])
            yield
            P.op("dve", lambda e: e.tensor_tensor(out=val[:], in0=DB[3][:, 0:NE], in1=basecap[:], op=ALU.add), reads=[DB[3], basecap], writes=[val])
            P.op("dve", lambda e: e.tensor_tensor(out=okm[:], in0=val[:], in1=elim[:], op=ALU.is_le), reads=[val, elim], writes=[okm])
            P.op("dve", lambda e: e.tensor_tensor(out=okm[:], in0=okm[:], in1=sel[:], op=ALU.mult), reads=[okm, sel], writes=[okm])
            P.op("dve", lambda e: e.tensor_tensor(out=val[:], in0=val[:], in1=okm[:], op=ALU.mult), reads=[val, okm], writes=[val])
            P.op("dve", lambda e: e.tensor_tensor(out=basecap[:], in0=DB[3][:, 512:512 + NE], in1=basecap[:], op=ALU.add), reads=[DB[3], basecap], writes=[basecap])
            P.op("dve", lambda e: e.max(out=d8[:], in_=val[:]), reads=[val], writes=[d8])
            P.op("dve", lambda e: e.tensor_tensor(out=jk[:], in0=gd[:], in1=e4[:], op=ALU.add), reads=[gd, e4], writes=[jk])
            P.op("dve", lambda e: e.tensor_tensor(out=jk[:], in0=jk[:], in1=okm[:], op=ALU.mult), reads=[jk, okm], writes=[jk])
            P.op("dve", lambda e: e.max(out=k8[:], in_=jk[:]), reads=[jk], writes=[k8])
            P.op("dve", lambda e: e.tensor_scalar(out=k8i[:], in0=k8[:], scalar1=0.25, scalar2=-0.3125, op0=ALU.mult, op1=ALU.add), reads=[k8], writes=[k8i])
            P.op("dve", lambda e: e.tensor_copy(out=k8f[:], in_=k8i[:]), reads=[k8i], writes=[k8f])
            P.op("dve", lambda e, i=i: e.scalar_tensor_tensor(out=gate8_b[i][:, :], in0=k8f[:], scalar=-4.0, in1=k8[:], op0=ALU.mult, op1=ALU.add),
                 reads=[k8f, k8], writes=[gate8_b[i]])
            P.op("dve", lambda e: e.tensor_scalar(out=k8f[:], in0=d8[:], scalar1=0.5, scalar2=prow[:, 0:1], op0=ALU.is_lt, op1=ALU.mult), reads=[d8, prow], writes=[k8f])
            P.op("dve", lambda e, i=i: e.scalar_tensor_tensor(out=dest8_b[i][:, :], in0=d8[:], scalar=-1.0, in1=k8f[:], op0=ALU.add, op1=ALU.add), reads=[d8, k8f], writes=[dest8_b[i]])
            for k in range(8):
                P.dma("pool", lambda e, i=i, k=k, ub=ub: e.indirect_dma_start(out=xe.t[:, :], out_offset=bass.IndirectOffsetOnAxis(ap=dest8_b[i][:, k:k + 1], axis=0),
                                                                               in_=ub[:, :], in_offset=None), ub, reads=[ub, dest8_b[i]], writes=[xe])

        def run(g, n=None):
            k = 0
            for _ in g:
                k += 1
                if n is not None and k >= n:
                    return

        run(stage1(0))
        for i in range(NT):
            g1 = stage1(i + 1) if i + 1 < NT else iter(())
            g2 = stage2(i)
            run(g1, 1)
            run(g2, 1)
            run(g1, 1)
            run(g2, 1)
            run(g2)
            run(g1)
        P.barrier()

    with ExitStack() as s3:
        Wf1 = [P.sb("Wf1_%d" % i, [128, 8, FF], F32, s3) for i in range(2)]
        Wf3 = [P.sb("Wf3_%d" % i, [128, 8, FF], F32, s3) for i in range(2)]
        Wf2 = [P.sb("Wf2_%d" % i, [128, 2, D], F32, s3) for i in range(2)]
        W1 = [P.sb("W1_%d" % i, [128, 8, FF], BF16, s3) for i in range(2)]
        W3 = [P.sb("W3_%d" % i, [128, 8, FF], BF16, s3) for i in range(2)]
        W2 = [P.sb("W2_%d" % i, [128, 2, D], BF16, s3) for i in range(2)]
        xr = [P.sb("xr%d" % i, [128, D], BF16, s3) for i in range(2 * NR)]
        XT = [P.sb("XT%d" % i, [128, 8, CAP], BF16, s3) for i in range(2)]
        sl = [P.sb("sl%d" % i, [128, 2 * CAP], F32, s3) for i in range(2)]
        aE = [P.sb("aE%d" % i, [128, 2, CAP], BF16, s3) for i in range(2)]
        yo = [P.sb("yo%d" % i, [128, D], BF16, s3) for i in range(2)]
        DBH = [[Buf("db%d_%d" % (i, hf), DB[i].t[:, hf * 512:(hf + 1) * 512]) for hf in range(2)] for i in range(4)]

        def load_w(e_):
            b = e_ % 2
            P.dma("sp", lambda e: e.dma_start(out=Wf1[b][:], in_=w1.t[e_].rearrange("(p c) f -> p c f", c=8)), Wf1[b], reads=[w1], writes=[Wf1[b]])
            P.dma("sp", lambda e: e.dma_start(out=Wf3[b][:], in_=w3.t[e_].rearrange("(p c) f -> p c f", c=8)), Wf3[b], reads=[w3], writes=[Wf3[b]])
            P.dma("sp", lambda e: e.dma_start(out=Wf2[b][:], in_=w2.t[e_].rearrange("(p c) d -> p c d", c=2)), Wf2[b], reads=[w2], writes=[Wf2[b]])

        def cast_w(e_):
            b = e_ % 2
            pv = lambda t: t[:].rearrange("p k (c m) -> p k c m", c=2)
            sv = lambda t: t[:].rearrange("p k (m c) -> p k c m", c=2)
            P.op("act", lambda e: e.activation(out=pv(W1[b]), in_=sv(Wf1[b]), func=AF.Copy), reads=[Wf1[b]], writes=[W1[b]])
            P.op("dve", lambda e: e.tensor_copy(out=pv(W3[b]), in_=sv(Wf3[b])), reads=[Wf3[b]], writes=[W3[b]])
            P.op("act", lambda e: e.activation(out=W2[b][:, 0, :], in_=Wf2[b][:, 0, :], func=AF.Copy), reads=[Wf2[b]], writes=[W2[b]])
            P.op("dve", lambda e: e.tensor_copy(out=W2[b][:, 1, :], in_=Wf2[b][:, 1, :]), reads=[Wf2[b]], writes=[W2[b]])

        def load_x(e_):
            for r in range(NR):
                xb_ = xr[(e_ % 2) * NR + r]
                row0 = e_ * CAP + r * 128
                rr_ = RROWS[r]
                P.dma("sp", lambda e, xb_=xb_, row0=row0, rr_=rr_: e.dma_start(out=xb_[0:rr_, :], in_=xe.t[row0:row0 + rr_, :]), xb_, reads=[xe], writes=[xb_])

        def trans(e_):
            for r in range(NR):
                xb_ = xr[(e_ % 2) * NR + r]
                bank = DBH[0][r % 2]
                pv = bank.t.bitcast(BF16)
                rr_ = RROWS[r]
                xs = xb_[:].rearrange("s (p c) -> s c p", c=8)
                for c in range(8):
                    P.op("pe", lambda e, c=c, xs=xs, pv=pv, rr_=rr_: e.transpose(out=pv[:, c * 128:c * 128 + rr_], in_=xs[0:rr_, c, :], identity=ident[0:rr_, 0:rr_]),
                         reads=[xb_, ident], writes=[bank])
                xt_ = XT[e_ % 2]
                P.op("act", lambda e, r=r, pv=pv, xt_=xt_, rr_=rr_: e.activation(out=xt_[:, 0:4, r * 128:r * 128 + rr_], in_=pv[:, 0:512].rearrange("p (c t) -> p c t", c=4)[:, :, 0:rr_], func=AF.Copy),
                     reads=[bank], writes=[xt_])
                P.op("dve", lambda e, r=r, pv=pv, xt_=xt_, rr_=rr_: e.tensor_copy(out=xt_[:, 4:8, r * 128:r * 128 + rr_], in_=pv[:, 512:1024].rearrange("p (c t) -> p c t", c=4)[:, :, 0:rr_]),
                     reads=[bank], writes=[xt_])

        def hmm(e_):
            b = e_ % 2
            for wi, Wx in enumerate((W1[b], W3[b])):
                pb = DBH[1 + b][wi]
                for fc in range(2):
                    for k in range(8):
                        P.op("pe", lambda e, Wx=Wx, pb=pb, fc=fc, k=k: e.matmul(pb[:, fc * CAP:(fc + 1) * CAP], lhsT=Wx[:, k, fc * 128:(fc + 1) * 128], rhs=XT[b][:, k, :],
                                                                                start=(k == 0), stop=(k == 7)), reads=[Wx, XT[b]], writes=[pb])

        def act_(e_):
            b = e_ % 2
            P.op("act", lambda e: e.activation(out=sl[b][:], in_=DBH[1 + b][0][:, 0:2 * CAP], func=AF.Silu), reads=[DBH[1 + b][0]], writes=[sl[b]])
            P.op("dve", lambda e: e.tensor_tensor(out=aE[b][:].rearrange("p c t -> p (c t)"), in0=sl[b][:], in1=DBH[1 + b][1][:, 0:2 * CAP], op=ALU.mult),
                 reads=[sl[b], DBH[1 + b][1]], writes=[aE[b]])

        def ymm(e_):
            b = e_ % 2
            for r in range(NR):
                yb = yo[r % 2]
                rr_ = RROWS[r]
                for half in range(2):
                    hs = slice(half * 512, (half + 1) * 512)
                    for fc in range(2):
                        P.op("pe", lambda e, fc=fc, hs=hs, r=r, half=half, rr_=rr_: e.matmul(DBH[3][half][0:rr_, :], lhsT=aE[b][:, fc, r * 128:r * 128 + rr_], rhs=W2[b][:, fc, hs], start=(fc == 0), stop=(fc == 1)),
                             reads=[aE[b], W2[b]], writes=[DBH[3][half]])
                P.op("act", lambda e, yb=yb, rr_=rr_: e.activation(out=yb[0:rr_, 0:512], in_=DBH[3][0][0:rr_, :], func=AF.Copy), reads=[DBH[3][0]], writes=[yb])
                P.op("dve", lambda e, yb=yb, rr_=rr_: e.tensor_copy(out=yb[0:rr_, 512:1024], in_=DBH[3][1][0:rr_, :]), reads=[DBH[3][1]], writes=[yb])
                row0 = e_ * CAP + r * 128
                P.dma("pool", lambda e, yb=yb, row0=row0, rr_=rr_: e.dma_start(out=ye.t[row0:row0 + rr_, :], in_=yb[0:rr_, :]), yb, reads=[yb], writes=[ye])

        P.op("dve", lambda e: e.memset(yo[0][:], 0.0), writes=[yo[0]])
        P.dma("pool", lambda e: e.dma_start(out=ye.t[NE * CAP:NE * CAP + 128, :], in_=yo[0][:]), yo[0], reads=[yo[0]], writes=[ye])
        load_w(0)
        if NE > 1:
            load_w(1)
        load_x(0)
        cast_w(0)
        trans(0)
        for e_ in range(NE):
            hmm(e_)
            if e_ + 1 < NE:
                load_x(e_ + 1)
                cast_w(e_ + 1)
                trans(e_ + 1)
            if e_ + 2 < NE:
                load_w(e_ + 2)
            act_(e_)
            ymm(e_)
        P.barrier()
    if upto == 3:
        es.close()
        return nc
    out_v = out.t.rearrange("(j p) d -> j p d", p=128)
    with ExitStack() as s4:
        yk = [P.sb("yk%d" % i, [128, 8, D], BF16, s4) for i in range(3)]
        ac = [P.sb("ac%d" % i, [128, D], F32, s4) for i in range(3)]
        ada = P.sb("ada4", [128, D], F32, s4)
        P.dma("sp", lambda e: e.dma_start(out=ada[:], in_=adas[:, 5 * D:6 * D]), ada, reads=[adas], writes=[ada])
        G2 = slice(0, D)
        lnp_t = P.sb("lnp2", [128, 2, D], F32, s4)
        P.dma("sp", lambda e: e.dma_start(out=lnp_t[:], in_=lnp[:, 2:4, :]), lnp_t, reads=[lnp], writes=[lnp_t])

        dest8_b = [Buf("dest8_%d" % i_, dest8.t[:, i_, :]) for i_ in range(NT)]
        gate8_b = [Buf("gate8_%d" % i_, gate8.t[:, i_, :]) for i_ in range(NT)]
        rrs = [P.sb("rr%d" % i_, [128, D], F32, s4) for i_ in range(2)]
        ots = [P.sb("ot%d" % i_, [128, D], F32, s4) for i_ in range(2)]
        dgs = [P.sb("dg%d" % i_, [128, 8, 128], BF16, s4) for i_ in range(2)]
        DBH4 = [[Buf("p4db%d_%d" % (i_, hf), DB[i_].t[:, hf * 512:(hf + 1) * 512]) for hf in range(2)] for i_ in range(2)]

        for y0 in yk:
            P.op("dve", lambda e, y0=y0: e.memset(y0[:], 0.0), writes=[y0])
        P.barrier()
        ykk = [[Buf("yk%d_%d" % (i_, k), yk[i_].t[:, k, :]) for k in range(8)] for i_ in range(3)]

        def loads(i):
            P.dma("sp", lambda e: e.dma_start(out=ac[i % 3][:], in_=accs_v[i]), ac[i % 3], reads=[accs], writes=[ac[i % 3]])
            for k in range(8):
                yb_ = ykk[i % 3][k]
                P.dma("pool", lambda e, k=k, yb_=yb_: e.indirect_dma_start(out=yb_[:, :], out_offset=None, in_=ye.t[:, :],
                                                                           in_offset=bass.IndirectOffsetOnAxis(ap=dest8_b[i][:, k:k + 1], axis=0)),
                      yb_, reads=[ye, dest8_b[i]], writes=[yb_])

        def stage1(i):
            y_ = yk[i % 3]
            a_ = ac[i % 3]
            dg = dgs[i % 2]
            r_ = rrs[i % 2]
            P.op("dve", lambda e: e.tensor_tensor(out=dg[:], in0=ident[:].unsqueeze(1).broadcast_to([128, 8, 128]),
                                                  in1=gate8_b[i][:, :].unsqueeze(2).broadcast_to([128, 8, 128]), op=ALU.mult),
                 reads=[ident, gate8_b[i]], writes=[dg])
            for hf in range(2):
                pb = DBH4[i % 2][hf]
                for k in range(8):
                    P.op("pe", lambda e, k=k, hf=hf, pb=pb: e.matmul(pb[:, :], lhsT=dg[:, k, :], rhs=ykk[i % 3][k][:, hf * 512:(hf + 1) * 512], start=(k == 0), stop=(k == 7)),
                         reads=[dg, ykk[i % 3][k]], writes=[pb])
            for hf in range(2):
                pb = DBH4[i % 2][hf]
                hs = slice(hf * 512, (hf + 1) * 512)
                if debug:
                    P.op("act", lambda e, hs=hs, pb=pb: e.activation(out=junk[:, hs], in_=pb[:, :], func=AF.Copy), reads=[pb], writes=[junk])
                P.op("dve", lambda e, hs=hs, pb=pb: e.tensor_tensor(out=r_[:, hs], in0=pb[:, :], in1=ada[:, hs], op=ALU.mult), reads=[pb, ada], writes=[r_])
            if debug:
                P.dma("sp", lambda e: e.dma_start(out=dbg["y_moe"].t[i * 128:(i + 1) * 128, :], in_=junk[:]), junk, reads=[junk], writes=[dbg["y_moe"]])
            P.op("dve", lambda e: e.tensor_tensor(out=r_[:], in0=r_[:], in1=a_[:], op=ALU.add), reads=[r_, a_], writes=[r_])

        def stage2(i):
            r_ = rrs[i % 2]
            ot = ots[i % 2]
            ln_stats(r_)
            P.op("act", lambda e: e.activation(out=ot[:], in_=r_[:], func=AF.Identity, scale=st[:, 4:5], bias=st[:, 5:6]), reads=[r_, st], writes=[ot])
            P.op("dve", lambda e: e.tensor_tensor(out=ot[:], in0=ot[:], in1=lnp_t[:, 0, :], op=ALU.mult), reads=[ot, lnp_t], writes=[ot])
            P.op("dve", lambda e: e.tensor_tensor(out=ot[:], in0=ot[:], in1=lnp_t[:, 1, :], op=ALU.add), reads=[ot, lnp_t], writes=[ot])
            P.dma("sp", lambda e: e.dma_start(out=out_v[i], in_=ot[:]), ot, reads=[ot], writes=[out])

        loads(0)
        if NT > 1:
            loads(1)
        stage1(0)
        for i in range(NT):
            if i + 1 < NT:
                stage1(i + 1)
            if i + 2 < NT:
                loads(i + 2)
            stage2(i)
        P.barrier()
    fin = [out] + list(dbg.values())
    P.finish(fin)
    es.close()
    return nc


def _consts(NE, CAP, first_half):
    cst = np.zeros((128, 5, 128), np.float32)
    j = np.arange(128)[:, None]
    s = np.arange(128)[None, :]
    cst[:, 0] = np.eye(128, dtype=np.float32)
    cst[:, 1] = (j >= s)
    cst[:, 2] = 1.0
    cst[:, 3] = (j < s)
    msk = np.zeros((128, 5, 1024), np.float32)
    rep = lambda m: np.tile(m.astype(np.float32), (1, 8))
    prev = (j > s)
    msk[:, 0] = rep(prev)
    msk[:, 1] = rep(j <= s)
    msk[:, 2] = rep(j < s)
    msk[:, 3] = 0.0 if first_half else rep(prev)
    msk[:, 4] = 0.0 if first_half else 1.0
    vec = np.zeros((128, 4), np.float32)
    p = np.arange(128)
    inv = (10000.0 ** (-(np.arange(32, dtype=np.float32) * 2.0 / 64))).astype(np.float32)
    vec[:, 0] = inv[p % 32]
    vec[:, 1] = np.where((p % 64) < 32, -1.0, 1.0)
    ecap = np.tile((np.arange(NE, dtype=np.float32) * CAP + 1.0)[None, :], (128, 1))
    e4c = np.tile(((np.arange(NE, dtype=np.float32) + 1.0) * 4.0)[None, :], (128, 1))
    elimc = np.tile(((np.arange(NE, dtype=np.float32) + 1.0) * CAP)[None, :], (128, 1))
    prowc = (NE * CAP + 1 + np.arange(128, dtype=np.float32)).reshape(128, 1)
    return cst, msk, vec, ecap, e4c, elimc, prowc


def _win_layout(w_in):
    def sw(w):
        n = w.shape[1] // 64
        w4 = w.reshape(w.shape[0], n, 2, 32)
        return w4[:, :, ::-1, :].reshape(w.shape[0], n * 64)
    qa, ka, va = w_in[:, 0:512], w_in[:, 512:640], w_in[:, 640:768]
    qs, ks, vs = w_in[:, 768:1280], w_in[:, 1280:1792], w_in[:, 1792:2304]
    dup = lambda k: np.concatenate([k[:, 0:64], k[:, 0:64], k[:, 64:128], k[:, 64:128]], axis=1)
    return np.ascontiguousarray(np.concatenate([qa, sw(qa), dup(ka), dup(sw(ka)), qs, ks, vs, va], axis=1))


def make_in_maps(inp, n_cores, NT, HB, NE, CAP, seq):
    rb = lambda v, n=128: np.ascontiguousarray(np.broadcast_to(np.asarray(v, np.float32).reshape(1, -1), (n, np.asarray(v).size)))
    x = np.asarray(inp["x"], np.float32)
    pos = np.asarray(inp["positions"], np.int32)
    halves = seq // (NT * 128)
    win = _win_layout(np.asarray(inp["w_in"], np.float32)[0])
    shared = {
        "w_ada": np.asarray(inp["w_ada"], np.float32)[0],
        "b_ada": rb(inp["b_ada"][0]),
        "w_in": win,
        "w_out": np.asarray(inp["w_out"], np.float32)[0],
        "gmix": rb(np.concatenate([np.asarray(inp["g_swa"])[0], np.asarray(inp["g_sb"])[0]])),
        "sinks": rb(inp["attn_sinks"][0]),
        "lnp": np.ascontiguousarray(np.stack([rb(inp["ln1_g"][0]), rb(inp["ln1_b"][0]), rb(inp["ln2_g"][0]), rb(inp["ln2_b"][0])], axis=1)),
        "w_r": np.asarray(inp["w_router"], np.float32)[0],
        "ebias": rb(inp["e_bias"][0]),
        "w1": np.asarray(inp["w1"], np.float32)[0],
        "w3": np.asarray(inp["w3"], np.float32)[0],
        "w2": np.asarray(inp["w2"], np.float32)[0],
        "ws1": np.asarray(inp["ws1"], np.float32)[0],
        "ws3": np.asarray(inp["ws3"], np.float32)[0],
        "ws2": np.asarray(inp["ws2"], np.float32)[0],
    }
    maps = []
    for c in range(n_cores):
        b, h = c // halves, c % halves
        s0 = h * NT * 128
        lo = s0 - HB * 128
        xh = np.zeros(((HB + NT) * 128, D), np.float32)
        ph = np.zeros(((HB + NT) * 128,), np.int32)
        src_lo = max(lo, 0)
        xh[src_lo - lo:] = x[b, src_lo:s0 + NT * 128]
        ph[src_lo - lo:] = pos[b, src_lo:s0 + NT * 128]
        cst, msk, vec, ecap, e4c, elimc, prowc = _consts(NE, CAP, first_half=(h == 0))
        m = dict(shared)
        m.update({
            "xh": xh,
            "posb": np.ascontiguousarray(np.broadcast_to(ph[None, :], (128, ph.size))),
            "csil": np.ascontiguousarray(np.asarray(inp["c"], np.float32)[b].reshape(8, 128).T),
            "cst": cst, "msk": msk, "vec": vec, "ecap": ecap, "e4c": e4c, "elimc": elimc, "prowc": prowc,
        })
        maps.append(m)
    return maps


NT_FULL, HB_FULL, NE_FULL, CAP_FULL, NKB_FULL = 32, 2, 256, 256, 3


def kernel(**inputs):
    nc = build_program(NT_FULL, HB_FULL, NE_FULL, CAP_FULL, NKB_FULL)
    maps = make_in_maps(inputs, 8, NT_FULL, HB_FULL, NE_FULL, CAP_FULL, 8192)
    res = run_bass_kernel_spmd(nc, maps, core_ids=list(range(8)))
    outs = [np.asarray(r["out"]) for r in res.results]
    full = np.stack(outs, axis=0).reshape(4, 8192, D)
    return full.astype(np.float32)
```
